# Optimizing a Trainium2 kernel written in Bass

```python
import numpy as np
import jax
import jax.numpy as jnp
from jax import lax

D_MODEL = 1024
BATCH = 8
SEQ = 2048
DEPTH = 4

GRID_W = 64
CTX_LEN = 256
N_MIXERS = 3
NORM_EPS = 1e-6
ROPE_THETA = 10000.0
NEG_INF = -1e30

A_HEADS = 16
A_KV_HEADS = 4
A_GROUP = A_HEADS // A_KV_HEADS
A_HEAD_DIM = D_MODEL // A_HEADS
A_WINDOW = 128
A_BLOCK = 128

B_HEADS = 16
B_HEAD_DIM = D_MODEL // B_HEADS
NA_ROWS = 8
NA_COLS = 16
NA_QCOLS = 16
NA_KCOLS = NA_QCOLS + NA_COLS
NA_NCB = GRID_W // NA_QCOLS

C_HEADS = 16
C_NOPE_DIM = 64
C_ROPE_DIM = 32
C_V_DIM = 64
C_Q_LORA = 256
C_KV_LORA = 128
C_BLOCK = 128

N_EXPERTS = 32
TOP_K = 4
D_EXPERT = D_MODEL
SWIGLU_LIMIT = 7.0
SWIGLU_ALPHA = 1.702
MOE_BLOCK = 256

kernel_name = "hybrid_diffusion_swa_natten_mla_moe"

F32 = jnp.float32


def rmsnorm(x, g):
    xf = x.astype(F32)
    y = xf * lax.rsqrt(jnp.mean(xf * xf, axis=-1, keepdims=True) + NORM_EPS)
    return (y * g.astype(F32)).astype(x.dtype)


def modulate(x, shift, scale):
    return x * (1 + scale) + shift


def axial_rope_angles(n_tokens, rot_dim):
    t = jnp.arange(n_tokens)
    row = (t // GRID_W).astype(F32)
    col = (t % GRID_W).astype(F32)
    n_freq = rot_dim // 4
    inv = ROPE_THETA ** (-jnp.arange(n_freq, dtype=F32) / n_freq)
    ang = jnp.concatenate([row[:, None] * inv, col[:, None] * inv], axis=-1)
    return jnp.cos(ang), jnp.sin(ang)


def apply_rope(x, cos, sin):
    shape = (1, cos.shape[0]) + (1,) * (x.ndim - 3) + (cos.shape[1],)
    cs = cos.reshape(shape)
    sn = sin.reshape(shape)
    xf = x.astype(F32).reshape(x.shape[:-1] + (-1, 2))
    x1, x2 = xf[..., 0], xf[..., 1]
    out = jnp.stack([x1 * cs - x2 * sn, x1 * sn + x2 * cs], axis=-1).reshape(x.shape)
    return out.astype(x.dtype)


def joint_softmax(parts, sink=None):
    s = jnp.concatenate([p.astype(F32) for p in parts], axis=-1)
    m = jnp.max(s, axis=-1, keepdims=True)
    if sink is not None:
        m = jnp.maximum(m, sink)
    e = jnp.exp(s - m)
    den = jnp.sum(e, axis=-1, keepdims=True)
    if sink is not None:
        den = den + jnp.exp(sink - m)
    p = e / den
    cuts = [int(v) for v in np.cumsum([q.shape[-1] for q in parts])[:-1]]
    return jnp.split(p, cuts, axis=-1)


def window_gqa_mixer(h, hc, w_in, w_out, sink, cos, sin, need_ctx):
    B, S, _ = h.shape
    C = hc.shape[1]
    qd = A_HEADS * A_HEAD_DIM
    kd = A_KV_HEADS * A_HEAD_DIM
    y = h @ w_in
    q = y[..., :qd].reshape(B, S, A_KV_HEADS, A_GROUP, A_HEAD_DIM)
    k = y[..., qd:qd + kd].reshape(B, S, A_KV_HEADS, A_HEAD_DIM)
    v = y[..., qd + kd:].reshape(B, S, A_KV_HEADS, A_HEAD_DIM)
    q = apply_rope(q, cos, sin)
    k = apply_rope(k, cos, sin)
    yc = hc @ (w_in if need_ctx else w_in[:, qd:])
    kc = yc[..., -2 * kd:-kd].reshape(B, C, A_KV_HEADS, A_HEAD_DIM)
    vc = yc[..., -kd:].reshape(B, C, A_KV_HEADS, A_HEAD_DIM)
    scale = A_HEAD_DIM ** -0.5
    sink_f = sink.astype(F32).reshape(A_KV_HEADS, A_GROUP)[None, :, :, None, None]
    nb = S // A_BLOCK

    def windows(t):
        tp = jnp.pad(t, ((0, 0), (A_BLOCK, A_BLOCK), (0, 0), (0, 0)))
        tp = tp.reshape(B, nb + 2, A_BLOCK, A_KV_HEADS, A_HEAD_DIM)
        w = jnp.concatenate([tp[:, :-2], tp[:, 1:-1], tp[:, 2:]], axis=2)
        return jnp.moveaxis(w, 1, 0)

    qb = jnp.moveaxis(q.reshape(B, nb, A_BLOCK, A_KV_HEADS, A_GROUP, A_HEAD_DIM), 1, 0)
    rel = jnp.arange(3 * A_BLOCK)[None, :] - A_BLOCK - jnp.arange(A_BLOCK)[:, None]
    band = jnp.abs(rel) <= A_WINDOW

    def block(args):
        qi, ki, vi, b = args
        kpos = (b - 1) * A_BLOCK + jnp.arange(3 * A_BLOCK)
        valid = band & ((kpos >= 0) & (kpos < S))[None, :]
        s_loc = jnp.einsum('bqkgd,bjkd->bkgqj', qi, ki).astype(F32) * scale
        s_loc = jnp.where(valid, s_loc, NEG_INF)
        s_ctx = jnp.einsum('bqkgd,bckd->bkgqc', qi, kc).astype(F32) * scale
        p_loc, p_ctx = joint_softmax([s_loc, s_ctx], sink_f)
        return (jnp.einsum('bkgqj,bjkd->bqkgd', p_loc.astype(vi.dtype), vi)
                + jnp.einsum('bkgqc,bckd->bqkgd', p_ctx.astype(vc.dtype), vc))

    o = lax.map(block, (qb, windows(k), windows(v), jnp.arange(nb)))
    out = jnp.moveaxis(o, 0, 1).reshape(B, S, qd) @ w_out
    out_c = None
    if need_ctx:
        qc = yc[..., :qd].reshape(B, C, A_KV_HEADS, A_GROUP, A_HEAD_DIM)
        s = jnp.einsum('bqkgd,bckd->bkgqc', qc, kc).astype(F32) * scale
        (p,) = joint_softmax([s], sink_f)
        oc = jnp.einsum('bkgqc,bckd->bqkgd', p.astype(vc.dtype), vc)
        out_c = oc.reshape(B, C, qd) @ w_out
    return out, out_c


def neighbourhood_mixer(h, hc, w_in, w_out, rpb, need_ctx):
    B, S, _ = h.shape
    C = hc.shape[1]
    rows = S // GRID_W
    kh = min(NA_ROWS, rows)
    wd = B_HEADS * B_HEAD_DIM
    y = h @ w_in
    q = y[..., :wd].reshape(B, S, B_HEADS, B_HEAD_DIM)
    k = y[..., wd:2 * wd].reshape(B, S, B_HEADS, B_HEAD_DIM)
    v = y[..., 2 * wd:].reshape(B, S, B_HEADS, B_HEAD_DIM)
    yc = hc @ (w_in if need_ctx else w_in[:, wd:])
    kc = yc[..., -2 * wd:-wd].reshape(B, C, B_HEADS, B_HEAD_DIM)
    vc = yc[..., -wd:].reshape(B, C, B_HEADS, B_HEAD_DIM)
    scale = B_HEAD_DIM ** -0.5

    qcol = np.arange(NA_NCB)[:, None] * NA_QCOLS + np.arange(NA_QCOLS)[None, :]
    cstart = np.clip(np.arange(NA_NCB) * NA_QCOLS - NA_COLS // 2, 0, GRID_W - NA_KCOLS)
    kcol = cstart[:, None] + np.arange(NA_KCOLS)[None, :]
    wstart = np.clip(qcol - NA_COLS // 2, 0, GRID_W - NA_COLS)
    col_ok = ((kcol[:, None, :] >= wstart[..., None])
              & (kcol[:, None, :] < wstart[..., None] + NA_COLS))
    col_ok = jnp.asarray(col_ok[:, :, None, :])
    dc_idx = jnp.asarray(np.clip(kcol[:, None, :] - qcol[..., None] + NA_COLS - 1, 0, 2 * NA_COLS - 2))
    kcol_j = jnp.asarray(kcol)

    q_rows = jnp.moveaxis(q.reshape(B, rows, NA_NCB, NA_QCOLS, B_HEADS, B_HEAD_DIM), 1, 0)
    k_grid = k.reshape(B, rows, GRID_W, B_HEADS, B_HEAD_DIM)
    v_grid = v.reshape(B, rows, GRID_W, B_HEADS, B_HEAD_DIM)
    rpb_f = rpb.astype(F32)

    def row_block(args):
        qr, r = args
        r0 = jnp.clip(r - kh // 2, 0, rows - kh)
        kr = lax.dynamic_slice_in_dim(k_grid, r0, kh, axis=1)[:, :, kcol_j]
        vr = lax.dynamic_slice_in_dim(v_grid, r0, kh, axis=1)[:, :, kcol_j]
        s_loc = jnp.einsum('bjqhd,brjkhd->bhjqrk', qr, kr).astype(F32) * scale
        dr_idx = r0 + jnp.arange(kh) - r + NA_ROWS - 1
        bias = rpb_f[:, dr_idx[None, None, :, None], dc_idx[:, :, None, :]]
        s_loc = jnp.where(col_ok, s_loc + bias, NEG_INF)
        s_loc = s_loc.reshape(B, B_HEADS, NA_NCB, NA_QCOLS, kh * NA_KCOLS)
        s_ctx = jnp.einsum('bjqhd,bchd->bhjqc', qr, kc).astype(F32) * scale
        p_loc, p_ctx = joint_softmax([s_loc, s_ctx])
        p_loc = p_loc.reshape(B, B_HEADS, NA_NCB, NA_QCOLS, kh, NA_KCOLS)
        return (jnp.einsum('bhjqrk,brjkhd->bjqhd', p_loc.astype(vr.dtype), vr)
                + jnp.einsum('bhjqc,bchd->bjqhd', p_ctx.astype(vc.dtype), vc))

    o = lax.map(row_block, (q_rows, jnp.arange(rows)))
    out = jnp.moveaxis(o, 0, 1).reshape(B, S, wd) @ w_out
    out_c = None
    if need_ctx:
        qc = yc[..., :wd].reshape(B, C, B_HEADS, B_HEAD_DIM)
        s = jnp.einsum('bqhd,bchd->bhqc', qc, kc).astype(F32) * scale
        (p,) = joint_softmax([s])
        oc = jnp.einsum('bhqc,bchd->bqhd', p.astype(vc.dtype), vc)
        out_c = oc.reshape(B, C, wd) @ w_out
    return out, out_c


def mla_mixer(h, hc, w_in, q_norm, kv_norm, w_uq, w_ukv, w_out, cos, sin, need_ctx):
    B, S, _ = h.shape
    C = hc.shape[1]
    H = C_HEADS
    scale = (C_NOPE_DIM + C_ROPE_DIM) ** -0.5

    def kv_path(yk, n):
        ckv = rmsnorm(yk[..., :C_KV_LORA], kv_norm)
        k_rope = yk[..., C_KV_LORA:]
        kv = (ckv @ w_ukv).reshape(B, n, H, C_NOPE_DIM + C_V_DIM)
        return kv[..., :C_NOPE_DIM], k_rope, kv[..., C_NOPE_DIM:]

    def q_path(yq, n):
        cq = rmsnorm(yq, q_norm)
        qq = (cq @ w_uq).reshape(B, n, H, C_NOPE_DIM + C_ROPE_DIM)
        return qq[..., :C_NOPE_DIM], qq[..., C_NOPE_DIM:]

    def attend(qn, qr, kn, kr, vv):
        s = (jnp.einsum('bqhd,bkhd->bhqk', qn, kn)
             + jnp.einsum('bqhd,bkd->bhqk', qr, kr)).astype(F32) * scale
        (p,) = joint_softmax([s])
        return jnp.einsum('bhqk,bkhd->bqhd', p.astype(vv.dtype), vv)

    y = h @ w_in
    qn, qr = q_path(y[..., :C_Q_LORA], S)
    kn, kr, v = kv_path(y[..., C_Q_LORA:], S)
    qr = apply_rope(qr, cos, sin)
    kr = apply_rope(kr, cos, sin)
    yc = hc @ (w_in if need_ctx else w_in[:, C_Q_LORA:])
    kn_c, kr_c, v_c = kv_path(yc[..., -(C_KV_LORA + C_ROPE_DIM):], C)
    kn_all = jnp.concatenate([kn_c, kn], axis=1)
    kr_all = jnp.concatenate([kr_c, kr], axis=1)
    v_all = jnp.concatenate([v_c, v], axis=1)
    nb = S // C_BLOCK
    qn_b = jnp.moveaxis(qn.reshape(B, nb, C_BLOCK, H, C_NOPE_DIM), 1, 0)
    qr_b = jnp.moveaxis(qr.reshape(B, nb, C_BLOCK, H, C_ROPE_DIM), 1, 0)
    o = lax.map(lambda a: attend(a[0], a[1], kn_all, kr_all, v_all), (qn_b, qr_b))
    out = jnp.moveaxis(o, 0, 1).reshape(B, S, H * C_V_DIM) @ w_out
    out_c = None
    if need_ctx:
        qn_c, qr_c = q_path(yc[..., :C_Q_LORA], C)
        oc = attend(qn_c, qr_c, kn_c, kr_c, v_c)
        out_c = oc.reshape(B, C, H * C_V_DIM) @ w_out
    return out, out_c


def moe_ffn(t, router_w, router_b, w_gate, b_gate, w_up, b_up, w_down, b_down):
    n, d = t.shape
    logits = (t @ router_w).astype(F32) + router_b.astype(F32)
    top_val, top_idx = lax.top_k(logits, TOP_K)
    gates = jax.nn.softmax(top_val, axis=-1)
    nk = n * TOP_K
    flat_e = top_idx.reshape(-1).astype(jnp.int32)
    order = jnp.argsort(flat_e)
    sorted_e = flat_e[order]
    counts = jnp.bincount(flat_e, length=N_EXPERTS).astype(jnp.int32)
    padded = (counts + MOE_BLOCK - 1) // MOE_BLOCK * MOE_BLOCK
    pad_end = jnp.cumsum(padded)
    pad_start = pad_end - padded
    grp_start = jnp.cumsum(counts) - counts
    dest = pad_start[sorted_e] + jnp.arange(nk, dtype=jnp.int32) - grp_start[sorted_e]
    n_blocks = -(-nk // MOE_BLOCK) + N_EXPERTS
    slot_token = jnp.full((n_blocks * MOE_BLOCK,), n, jnp.int32).at[dest].set(
        (order // TOP_K).astype(jnp.int32))
    block_expert = jnp.minimum(
        jnp.searchsorted(pad_end, jnp.arange(n_blocks, dtype=jnp.int32) * MOE_BLOCK, side='right'),
        N_EXPERTS - 1)
    t_pad = jnp.concatenate([t, jnp.zeros((1, d), t.dtype)], axis=0)
    x_blocks = t_pad[slot_token].reshape(n_blocks, MOE_BLOCK, d)

    def expert_block(args):
        xb, e = args
        g = xb @ w_gate[e] + b_gate[e]
        u = xb @ w_up[e] + b_up[e]
        g = jnp.minimum(g, SWIGLU_LIMIT)
        u = jnp.clip(u, -SWIGLU_LIMIT, SWIGLU_LIMIT)
        a = g * jax.nn.sigmoid(SWIGLU_ALPHA * g) * (u + 1)
        return a @ w_down[e] + b_down[e]

    y_slots = lax.map(expert_block, (x_blocks, block_expert)).reshape(-1, d)
    slot_of = jnp.zeros((nk,), jnp.int32).at[order].set(dest)
    y = y_slots[slot_of].reshape(n, TOP_K, d)
    return jnp.einsum('nkd,nk->nd', y, gates.astype(y.dtype))


def setup_inputs(seed: int = 0) -> dict:
    key = jax.random.key(seed)
    keys = iter(jax.random.split(key, 32))

    def rnd(shape, scale):
        return jax.random.normal(next(keys), shape, jnp.float32) * scale

    D = D_MODEL
    n_a = len(range(0, DEPTH, N_MIXERS))
    n_b = len(range(1, DEPTH, N_MIXERS))
    n_c = len(range(2, DEPTH, N_MIXERS))
    a_qd = A_HEADS * A_HEAD_DIM
    a_in = a_qd + 2 * A_KV_HEADS * A_HEAD_DIM
    b_w = B_HEADS * B_HEAD_DIM
    c_in = C_Q_LORA + C_KV_LORA + C_ROPE_DIM
    return {
        "x": rnd((BATCH, SEQ, D), 1.0),
        "c": rnd((BATCH, D), 1.0),
        "ctx": rnd((BATCH, CTX_LEN, D), 1.0),
        "c_ctx": rnd((D,), 1.0),
        "ada_w": rnd((DEPTH, D, 6 * D), 0.5 * D ** -0.5),
        "ada_b": rnd((DEPTH, 6 * D), 0.02),
        "norm_mix": 1.0 + rnd((DEPTH, D), 0.05),
        "norm_ffn": 1.0 + rnd((DEPTH, D), 0.05),
        "norm_out": 1.0 + rnd((D,), 0.05),
        "a_w_in": rnd((n_a, D, a_in), D ** -0.5),
        "a_w_out": rnd((n_a, a_qd, D), a_qd ** -0.5),
        "a_sink": rnd((n_a, A_HEADS), 0.5),
        "b_w_in": rnd((n_b, D, 3 * b_w), D ** -0.5),
        "b_w_out": rnd((n_b, b_w, D), b_w ** -0.5),
        "b_rpb": rnd((n_b, B_HEADS, 2 * NA_ROWS - 1, 2 * NA_COLS - 1), 0.2),
        "c_w_in": rnd((n_c, D, c_in), D ** -0.5),
        "c_q_norm": 1.0 + rnd((n_c, C_Q_LORA), 0.05),
        "c_kv_norm": 1.0 + rnd((n_c, C_KV_LORA), 0.05),
        "c_w_uq": rnd((n_c, C_Q_LORA, C_HEADS * (C_NOPE_DIM + C_ROPE_DIM)), C_Q_LORA ** -0.5),
        "c_w_ukv": rnd((n_c, C_KV_LORA, C_HEADS * (C_NOPE_DIM + C_V_DIM)), C_KV_LORA ** -0.5),
        "c_w_out": rnd((n_c, C_HEADS * C_V_DIM, D), (C_HEADS * C_V_DIM) ** -0.5),
        "router_w": rnd((DEPTH, D, N_EXPERTS), D ** -0.5),
        "router_b": rnd((DEPTH, N_EXPERTS), 0.01),
        "moe_w_gate": rnd((DEPTH, N_EXPERTS, D, D_EXPERT), D ** -0.5),
        "moe_b_gate": rnd((DEPTH, N_EXPERTS, D_EXPERT), 0.02),
        "moe_w_up": rnd((DEPTH, N_EXPERTS, D, D_EXPERT), D ** -0.5),
        "moe_b_up": rnd((DEPTH, N_EXPERTS, D_EXPERT), 0.02),
        "moe_w_down": rnd((DEPTH, N_EXPERTS, D_EXPERT, D), D_EXPERT ** -0.5),
        "moe_b_down": rnd((DEPTH, N_EXPERTS, D), 0.02),
    }


def reference(x, c, ctx, c_ctx, ada_w, ada_b, norm_mix, norm_ffn, norm_out,
              a_w_in, a_w_out, a_sink, b_w_in, b_w_out, b_rpb,
              c_w_in, c_q_norm, c_kv_norm, c_w_uq, c_w_ukv, c_w_out,
              router_w, router_b, moe_w_gate, moe_b_gate, moe_w_up, moe_b_up,
              moe_w_down, moe_b_down):
    B, S, D = x.shape
    C = ctx.shape[1]
    cos_a, sin_a = axial_rope_angles(S, A_HEAD_DIM)
    cos_c, sin_c = axial_rope_angles(S, C_ROPE_DIM)
    silu_c = jax.nn.silu(c)
    silu_cc = jax.nn.silu(c_ctx)
    xl, xc = x, ctx
    for l in range(DEPTH):
        last = l == DEPTH - 1
        kind, slot = l % N_MIXERS, l // N_MIXERS
        mod = (silu_c @ ada_w[l] + ada_b[l])[:, None, :]
        sh1, sc1, g1, sh2, sc2, g2 = jnp.split(mod, 6, axis=-1)
        modc = silu_cc @ ada_w[l] + ada_b[l]
        ch1, cs1, cg1, ch2, cs2, cg2 = jnp.split(modc, 6, axis=-1)

        h = modulate(rmsnorm(xl, norm_mix[l]), sh1, sc1)
        hc = modulate(rmsnorm(xc, norm_mix[l]), ch1, cs1)
        if kind == 0:
            y, yc = window_gqa_mixer(h, hc, a_w_in[slot], a_w_out[slot], a_sink[slot],
                                     cos_a, sin_a, not last)
        elif kind == 1:
            y, yc = neighbourhood_mixer(h, hc, b_w_in[slot], b_w_out[slot], b_rpb[slot], not last)
        else:
            y, yc = mla_mixer(h, hc, c_w_in[slot], c_q_norm[slot], c_kv_norm[slot], c_w_uq[slot],
                              c_w_ukv[slot], c_w_out[slot], cos_c, sin_c, not last)
        xl = xl + g1 * y

        h = modulate(rmsnorm(xl, norm_ffn[l]), sh2, sc2)
        moe_args = (router_w[l], router_b[l], moe_w_gate[l], moe_b_gate[l], moe_w_up[l],
                    moe_b_up[l], moe_w_down[l], moe_b_down[l])
        if last:
            f = moe_ffn(h.reshape(B * S, D), *moe_args)
            xl = xl + g2 * f.reshape(B, S, D)
        else:
            xc = xc + cg1 * yc
            hc = modulate(rmsnorm(xc, norm_ffn[l]), ch2, cs2)
            tokens = jnp.concatenate([h.reshape(B * S, D), hc.reshape(B * C, D)], axis=0)
            f = moe_ffn(tokens, *moe_args)
            xl = xl + g2 * f[:B * S].reshape(B, S, D)
            xc = xc + cg2 * f[B * S:].reshape(B, C, D)
    return rmsnorm(xl, norm_out)
```

```python
from contextlib import ExitStack
import numpy as np
import concourse.bass as bass
import concourse.mybir as mybir
from concourse.bass_utils import run_bass_kernel_spmd

F32 = mybir.dt.float32
BF16 = mybir.dt.bfloat16
AF = mybir.ActivationFunctionType
ALU = mybir.AluOpType
AX = mybir.AxisListType

ENG = ["pe", "act", "dve", "pool", "sp"]
N_DMA_SEMS = 12

D = 1024
KC = 8
S = 2048
C = 256
T = S + C
DEPTH = 4
NE = 32
EPS = 1e-6
TT = [(0, 512), (512, 512), (1024, 512), (1536, 512), (2048, 256)]
ARENA_WORDS = 53000


class V:
    __slots__ = ("ap", "keys")

    def __init__(self, ap, keys):
        self.ap = ap
        self.keys = keys


class Buf:
    def __init__(self, name, ap, nsub=1):
        self.name = name
        self.ap = ap
        self.nsub = nsub

    def v(self, idx=None, subs=None):
        ap = self.ap[idx] if idx is not None else self.ap
        if subs is None:
            keys = [(self.name, i) for i in range(self.nsub)]
        elif isinstance(subs, int):
            keys = [(self.name, subs)]
        else:
            keys = [(self.name, i) for i in subs]
        return V(ap, keys)


class Op:
    __slots__ = ("fn", "deps", "dma", "signal", "sem", "val", "sigcount")

    def __init__(self, fn, deps, dma):
        self.fn = fn
        self.deps = deps
        self.dma = dma
        self.signal = False
        self.sem = None
        self.val = 0
        self.sigcount = 0


class Prog:
    def __init__(self):
        self.nc = bass.Bass("TRN2", target_bir_lowering=False)
        self.ops = {e: [] for e in ENG}
        self.res_w = {}
        self.res_r = {}
        self.es = ExitStack()
        self.dma_since_barrier = []
        self.nbuf = 0

    def op(self, eng, fn, reads=(), writes=(), dma=False, extra_deps=()):
        deps = {}

        def add(tok):
            e, i = tok
            if self.ops[e][i].dma:
                deps[("dma", e, i)] = tok
            else:
                if e == "pe" and eng == "pe":
                    return
                k = ("eng", e)
                if k not in deps or deps[k][1] < i:
                    deps[k] = tok

        for v in reads:
            for k in v.keys:
                w = self.res_w.get(k)
                if w is not None:
                    add(w)
                if k[0].startswith("ps"):
                    for r in self.res_r.get(k, ()):
                        if r[0] != eng:
                            add(r)
        for v in writes:
            for k in v.keys:
                w = self.res_w.get(k)
                if w is not None:
                    add(w)
                for r in self.res_r.get(k, ()):
                    add(r)
        for tok in extra_deps:
            add(tok)
        idx = len(self.ops[eng])
        tok = (eng, idx)
        self.ops[eng].append(Op(fn, list(deps.values()), dma))
        if dma:
            self.dma_since_barrier.append(tok)
        for v in reads:
            for k in v.keys:
                self.res_r.setdefault(k, []).append(tok)
        for v in writes:
            for k in v.keys:
                self.res_w[k] = tok
                self.res_r[k] = []
        return tok

    def barrier(self):
        last = [(e, len(self.ops[e]) - 1) for e in ENG if self.ops[e]]
        deps = last + list(self.dma_since_barrier)
        self.dma_since_barrier = []
        for e in ENG:
            self.op(e, lambda eng: eng.nop(), extra_deps=deps)
        self.res_w = {}
        self.res_r = {}

    def mm(self, out, lhsT, rhs, start=True, stop=True):
        return self.op("pe", lambda e: e.matmul(out.ap, lhsT.ap, rhs.ap, start=start, stop=stop),
                       reads=[lhsT, rhs], writes=[out])

    def transpose(self, out, in_, ident):
        return self.op("pe", lambda e: e.transpose(out.ap, in_.ap, ident.ap),
                       reads=[in_, ident], writes=[out])

    def act(self, out, in_, func, bias=None, scale=1.0, accum=None):
        reads = [in_]
        kw = {}
        if isinstance(bias, V):
            reads.append(bias)
            kw["bias"] = bias.ap
        elif bias is not None:
            kw["bias"] = float(bias)
        if isinstance(scale, V):
            reads.append(scale)
            kw["scale"] = scale.ap
        else:
            kw["scale"] = float(scale)
        writes = [out]
        if accum is not None:
            writes.append(accum)
            kw["accum_out"] = accum.ap
        return self.op("act", lambda e: e.activation(out.ap, in_.ap, func, **kw), reads=reads, writes=writes)

    def ts(self, eng, out, in0, s1, s2, op0, op1=None, accum=None):
        reads = [in0]
        a1 = s1
        a2 = s2
        if isinstance(s1, V):
            reads.append(s1)
            a1 = s1.ap
        if isinstance(s2, V):
            reads.append(s2)
            a2 = s2.ap
        kw = {}
        if op1 is not None:
            kw["op1"] = op1
        writes = [out]
        if accum is not None:
            writes.append(accum)
            kw["accum_out"] = accum.ap
        return self.op(eng, lambda e: e.tensor_scalar(out.ap, in0.ap, a1, a2, op0, **kw), reads=reads, writes=writes)

    def tt(self, eng, out, in0, in1, op):
        return self.op(eng, lambda e: e.tensor_tensor(out.ap, in0.ap, in1.ap, op), reads=[in0, in1], writes=[out])

    def stt(self, eng, out, in0, scalar, in1, op0, op1):
        reads = [in0, in1]
        a = scalar
        if isinstance(scalar, V):
            reads.append(scalar)
            a = scalar.ap
        return self.op(eng, lambda e: e.scalar_tensor_tensor(out.ap, in0.ap, a, in1.ap, op0, op1),
                       reads=reads, writes=[out])

    def copy(self, eng, out, in_):
        if eng == "act":
            return self.op(eng, lambda e: e.copy(out.ap, in_.ap), reads=[in_], writes=[out])
        return self.op(eng, lambda e: e.tensor_copy(out.ap, in_.ap), reads=[in_], writes=[out])

    def recip(self, out, in_):
        return self.op("dve", lambda e: e.reciprocal(out.ap, in_.ap), reads=[in_], writes=[out])

    def memset(self, eng, out, val):
        return self.op(eng, lambda e: e.memset(out.ap, val), writes=[out])

    def dma(self, eng, out_ap, in_ap, reads=(), writes=(), **kw):
        return self.op(eng, lambda e: e.dma_start(out_ap, in_ap, **kw), reads=reads, writes=writes, dma=True)

    def emit(self):
        nc = self.nc
        es = self.es
        for e in ENG:
            for op in self.ops[e]:
                for (pe_, pi) in op.deps:
                    p = self.ops[pe_][pi]
                    if not p.dma:
                        p.signal = True
        EPOCH = 8000
        eng_sem = {}
        for e in ENG:
            c = 0
            for op in self.ops[e]:
                if op.dma:
                    continue
                if op.signal:
                    c += 1
                op.sigcount = c
            nep = max(1, (c + EPOCH - 1) // EPOCH)
            eng_sem[e] = [es.enter_context(nc.semaphore(f"s_{e}_{i}")) for i in range(nep)]

        def sig_of(e, sigcount):
            ep = (sigcount - 1) // EPOCH
            return eng_sem[e][ep], sigcount - ep * EPOCH

        for e in ENG:
            dl = [op for op in self.ops[e] if op.dma]
            if not dl:
                continue
            sems = [es.enter_context(nc.semaphore(f"d_{e}_{i}")) for i in range(N_DMA_SEMS)]
            uses = [0] * N_DMA_SEMS
            for j, op in enumerate(dl):
                s = j % N_DMA_SEMS
                uses[s] += 1
                op.sem = sems[s]
                op.val = 16 * uses[s]
        self.stats = {e: (len(self.ops[e]), max([o.sigcount for o in self.ops[e]] + [0])) for e in ENG}

        def run(e, eng):
            seen = {}

            def wait(sem, val):
                k = sem.num
                if seen.get(k, 0) < val:
                    eng.wait_ge(sem, val)
                    seen[k] = val

            for op in self.ops[e]:
                for (pe_, pi) in op.deps:
                    p = self.ops[pe_][pi]
                    if p.dma:
                        wait(p.sem, p.val)
                    else:
                        wait(*sig_of(pe_, p.sigcount))
                if op.dma:
                    if op.val > 16:
                        wait(op.sem, op.val - 16)
                    ins = op.fn(eng)
                    ins.then_inc(op.sem, 16)
                else:
                    ins = op.fn(eng)
                    if op.signal:
                        ins.then_inc(sig_of(e, op.sigcount)[0], 1)

        block = es.enter_context(nc.Block())

        @block.tensor
        def _(eng):
            run("pe", eng)

        @block.scalar
        def _(eng):
            run("act", eng)

        @block.vector
        def _(eng):
            run("dve", eng)

        @block.gpsimd
        def _(eng):
            run("pool", eng)

        @block.sync
        def _(eng):
            run("sp", eng)

        es.close()
        return nc


class MK:
    def __init__(self, layers=(0, 1, 2, 3), do_mixer=True, do_moe=True, n_experts=NE):
        self.layers = list(layers)
        self.do_mixer = do_mixer
        self.do_moe = do_moe
        self.n_experts = n_experts
        self.P = Prog()
        P = self.P
        nc = P.nc
        di = lambda n, s: nc.dram_tensor(n, list(s), F32, kind="ExternalInput").ap()
        self.x = di("x", [S, D])
        self.ctx = di("ctx", [C, D])
        self.c2 = di("c2", [2, D])
        self.ada_w = di("ada_w", [DEPTH, D, 6 * D])
        self.ada_b = di("ada_b", [DEPTH, 6 * D])
        self.norms = di("norms", [9, D])
        self.router_w = di("router_w", [DEPTH, D, NE])
        self.router_b = di("router_b", [DEPTH, NE])
        self.moe_layers = [l for l in self.layers] if do_moe else []
        self.lmap = {l: i for i, l in enumerate(self.moe_layers)}
        nl = max(1, len(self.moe_layers))
        nx = max(1, n_experts) if do_moe else 1
        self.w_gate = di("moe_w_gate", [nl, nx, D, D])
        self.w_up = di("moe_w_up", [nl, nx, D, D])
        self.w_down = di("moe_w_down", [nl, nx, D, D])
        self.b_gate = di("moe_b_gate", [DEPTH, NE, D])
        self.b_up = di("moe_b_up", [DEPTH, NE, D])
        self.b_down = di("moe_b_down", [DEPTH, NE, D])
        self.cst_ident = di("cst_ident", [128, 128])
        if do_mixer:
            self.a_w_in = di("a_w_in", [2, D, 1536])
            self.a_w_in_sw = di("a_w_in_sw", [2, D, 1280])
            self.a_w_out = di("a_w_out", [2, D, D])
            self.a_sink = di("a_sink", [2, 16])
            self.ropeA = di("ropeA", [2, 64, T])
            self.maskA = di("maskA", [2, 128, 128])
            self.b_w_in = di("b_w_in", [D, 3072])
            self.b_w_out = di("b_w_out", [D, D])
            self.b_tm = di("b_tm", [16, 16, 64, 64])
            self.c_w_in = di("c_w_in", [D, 416])
            self.c_w_in_sw = di("c_w_in_sw", [D, 96])
            self.c_norms = di("c_norms", [3, 128])
            self.c_w_uq = di("c_w_uq", [256, 1536])
            self.c_w_uq_sw = di("c_w_uq_sw", [256, 1536])
            self.c_w_ukv = di("c_w_ukv", [128, 2048])
            self.c_w_out = di("c_w_out", [D, D])
            self.ropeC = di("ropeC", [2, 96, T])
        self.out = nc.dram_tensor("out", [S, D], F32, kind="ExternalOutput").ap()
        self.gscr = nc.dram_tensor("gscr", [NE, T], F32, kind="Internal").ap()
        self.gscr_buf = Buf("gscr", self.gscr, 1)

        self.arena = P.es.enter_context(nc.sbuf_tensor("arena", [128, ARENA_WORDS], F32))
        self.ps = []
        for i in range(8):
            h = P.es.enter_context(nc.psum_tensor(f"ps{i}", [128, 512], F32))
            self.ps.append(Buf(f"ps{i}", h[:], 4))
        self.top = 0
        self.xT = self.alloc("xT", [128, KC, T], F32, nsub=KC * 5)
        self.hT = self.alloc("hT", [128, KC, T], BF16, nsub=KC * 5)
        self.ident = self.alloc("ident", [128, 128], F32)
        self.ones32 = self.alloc("ones32", [128, 128], F32)
        self.ones16 = self.alloc("ones16", [128, 64], BF16)
        self.modc = self.alloc("modc", [128, DEPTH, 6, KC, 2], F32, nsub=DEPTH)
        self.normc = self.alloc("normc", [128, KC, 9], F32)
        self.gm = self.alloc("gm", [128, 2, KC, 2], F32, nsub=2)
        self.phase_base = self.top

    def alloc(self, name, shape, dtype, nsub=1):
        n = int(np.prod(shape[1:]))
        words = n if dtype == F32 else (n + 1) // 2
        words = (words + 7) // 8 * 8
        lo = self.top
        self.top += words
        assert self.top <= ARENA_WORDS, (name, self.top)
        ap = self.arena[0:shape[0], lo:lo + words]
        if dtype != F32:
            ap = ap.bitcast(dtype)
        ap = ap[:, 0:n]
        if len(shape) == 3:
            ap = ap.rearrange("p (a b) -> p a b", a=shape[1])
        elif len(shape) == 4:
            ap = ap.rearrange("p (a b c) -> p a b c", a=shape[1], b=shape[2])
        elif len(shape) == 5:
            ap = ap.rearrange("p (a b c d) -> p a b c d", a=shape[1], b=shape[2], c=shape[3])
        self.P.nbuf += 1
        return Buf(f"{name}#{self.P.nbuf}", ap, nsub)

    def new_phase(self):
        self.P.barrier()
        self.top = self.phase_base

    def release(self, mark):
        self.P.barrier()
        self.top = mark

    def xsub(self, kc, tt):
        return kc * 5 + tt

    def psv(self, i, cols=slice(0, 512), parts=slice(0, 128), subs=None):
        return self.ps[i].v((parts, cols), subs=subs)

    def consts(self):
        P = self.P
        P.memset("dve", self.ones32.v(), 1.0)
        P.memset("dve", self.ones16.v(), 1.0)
        P.dma("sp", self.ident.ap, self.cst_ident, writes=[self.ident.v()])

    def rows_to_cols(self, dram_rows_ap, nrows, dst_fn, stage, ps_i):
        P = self.P
        P.dma("sp", stage.ap[0:nrows, :], dram_rows_ap, writes=[stage.v()])
        for kc in range(KC):
            pv = self.ps[ps_i].v((slice(0, 128), slice(kc * 32, kc * 32 + nrows)))
            P.transpose(pv, stage.v((slice(0, nrows), slice(kc * 128, (kc + 1) * 128))),
                        self.ident.v((slice(0, nrows), slice(0, nrows))))
            P.copy("dve", dst_fn(kc), pv)

    def load_x(self):
        P = self.P
        st = [self.alloc(f"xst{i}", [128, D], F32) for i in range(2)]
        n = 0
        for ti in range(T // 128):
            tok0 = ti * 128
            tt = min(tok0 // 512, 4)
            s = st[ti % 2]
            src = self.x[tok0:tok0 + 128, :] if tok0 < S else self.ctx[tok0 - S:tok0 - S + 128, :]
            P.dma("sp", s.ap, src, writes=[s.v()])
            for half in range(2):
                pb = self.ps[n % 4]
                n += 1
                for j in range(4):
                    kc = half * 4 + j
                    P.transpose(pb.v((slice(None), slice(j * 128, (j + 1) * 128)), subs=j),
                                s.v((slice(None), slice(kc * 128, (kc + 1) * 128))), self.ident.v())
                dst = self.xT.v((slice(None), slice(half * 4, half * 4 + 4), slice(tok0, tok0 + 128)),
                                subs=[self.xsub(half * 4 + j, tt) for j in range(4)])
                src_v = V(pb.ap.rearrange("p (a b) -> p a b", a=4), pb.v().keys)
                P.copy("dve" if half == 0 else "act", dst, src_v)

    def load_small(self):
        P = self.P
        stage = self.alloc("sm_stage", [128, D], F32)
        self.rows_to_cols(self.norms, 9, lambda kc: self.normc.v((slice(None), kc, slice(0, 9))), stage, 4)
        sc = self.alloc("siluc", [128, KC, 2], F32)
        self.rows_to_cols(self.c2, 2, lambda kc: sc.v((slice(None), kc, slice(0, 2))), stage, 4)
        sig = self.alloc("silu_sig", [128, KC, 2], F32)
        P.act(sig.v(), sc.v(), AF.Sigmoid)
        P.tt("dve", sc.v(), sc.v(), sig.v(), ALU.mult)
        adab = self.alloc("adab", [128, DEPTH, 48], F32)
        stage2 = self.alloc("sm_stage2", [48, 128], F32)
        for l in range(DEPTH):
            P.dma("sp", stage2.ap, self.ada_b[l].rearrange("(j p) -> j p", p=128), writes=[stage2.v()])
            pv = self.ps[5].v((slice(0, 128), slice(0, 48)))
            P.transpose(pv, stage2.v(), self.ident.v((slice(0, 48), slice(0, 48))))
            P.copy("dve", adab.v((slice(None), l, slice(None))), pv)
        wst = [self.alloc(f"adaw{i}", [128, KC, 512], F32) for i in range(2)]
        n = 0
        for l in range(DEPTH):
            for piece in range(12):
                w = wst[n % 2]
                n += 1
                src = self.ada_w[l].rearrange("(kc p) c -> p kc c", p=128)[:, :, piece * 512:(piece + 1) * 512]
                P.dma("sp", w.ap, src, writes=[w.v()])
                pb = self.ps[6 + (n % 2)]
                for q in range(4):
                    for kc in range(KC):
                        P.mm(pb.v((slice(None), slice(q * 2, q * 2 + 2))),
                             w.v((slice(None), kc, slice(q * 128, (q + 1) * 128))),
                             sc.v((slice(None), kc, slice(0, 2))), start=(kc == 0), stop=(kc == KC - 1))
                j, kc0 = divmod(piece * 4, 8)
                for q in range(4):
                    idx = piece * 4 + q
                    P.ts("dve", self.modc.v((slice(None), l, j, kc0 + q, slice(0, 2)), subs=l),
                         pb.v((slice(None), slice(q * 2, q * 2 + 2))),
                         adab.v((slice(None), l, slice(idx, idx + 1))), None, ALU.add)

    def prep_gm(self, l, which):
        P = self.P
        jsc = 1 if which == 0 else 4
        nidx = l if which == 0 else 4 + l
        for typ in range(2):
            P.ts("dve", self.gm.v((slice(None), which, slice(None), typ), subs=which),
                 self.modc.v((slice(None), l, jsc, slice(None), typ), subs=l), 1.0, None, ALU.add)
            P.tt("dve", self.gm.v((slice(None), which, slice(None), typ), subs=which),
                 self.gm.v((slice(None), which, slice(None), typ), subs=which),
                 self.normc.v((slice(None), slice(None), nidx)), ALU.mult)

    def norm_tile(self, tt, gain_fn, shift_fn, out_fn, tmp, ps_i, tmax=None):
        P = self.P
        t0, n = TT[tt]
        if tmax is not None:
            n = min(n, tmax)
        sq, rstd, tmul = tmp
        pb = self.ps[ps_i]
        for kc in range(KC):
            s = sq[kc % 2]
            P.act(s.v((slice(None), slice(0, n))), self.xT.v((slice(None), kc, slice(t0, t0 + n)), subs=self.xsub(kc, tt)),
                  AF.Square)
            P.mm(pb.v((slice(None), slice(0, n))), self.ones32.v(), s.v((slice(None), slice(0, n))),
                 start=(kc == 0), stop=(kc == KC - 1))
        P.ts("dve", rstd.v((slice(None), slice(0, n))), pb.v((slice(None), slice(0, n))), 1.0 / D, EPS, ALU.mult, ALU.add)
        P.act(rstd.v((slice(None), slice(0, n))), rstd.v((slice(None), slice(0, n))), AF.Sqrt)
        P.recip(rstd.v((slice(None), slice(0, n))), rstd.v((slice(None), slice(0, n))))
        for kc in range(KC):
            tm = tmul[kc % 2]
            P.tt("dve", tm.v((slice(None), slice(0, n))),
                 self.xT.v((slice(None), kc, slice(t0, t0 + n)), subs=self.xsub(kc, tt)),
                 rstd.v((slice(None), slice(0, n))), ALU.mult)
            sh = shift_fn(kc)
            outs = out_fn(kc)
            first = outs[0]
            P.act(first, tm.v((slice(None), slice(0, n))), AF.Identity, bias=sh if sh is not None else 0.0,
                  scale=gain_fn(kc))
            for o in outs[1:]:
                P.copy("pool", o, first)

    def ffn_phase(self, l, last):
        P = self.P
        ntt = 4 if last else 5
        Tl = S if last else T
        self.new_phase()
        self.prep_gm(l, 1)
        h32 = self.alloc("h32", [128, KC, 512], F32, nsub=KC)
        sq = [self.alloc(f"sq{i}", [128, 512], F32) for i in range(2)]
        rstd = self.alloc("rstd", [128, 512], F32)
        tmul = [self.alloc(f"tmul{i}", [128, 512], F32) for i in range(2)]
        rw = self.alloc("rw", [128, KC, NE], F32)
        rb = self.alloc("rb", [128, NE], F32)
        gatesT = self.alloc("gatesT", [NE, T], F32, nsub=5)
        lg = [self.alloc(f"lg{i}", [128, NE], F32) for i in range(2)]
        ex = [self.alloc(f"ex{i}", [128, NE], F32) for i in range(2)]
        mk = [self.alloc(f"mk{i}", [128, NE], F32) for i in range(2)]
        top8 = [self.alloc(f"top8{i}", [128, 8], F32) for i in range(2)]
        sm = [self.alloc(f"sm{i}", [128, 4], F32) for i in range(2)]
        P.dma("sp", rw.ap, self.router_w[l].rearrange("(kc p) e -> p kc e", p=128), writes=[rw.v()])
        P.dma("sp", rb.ap, self.router_b[l:l + 1, :].to_broadcast([128, NE]), writes=[rb.v()])
        it = 0
        for tt in range(ntt):
            t0, n = TT[tt]
            typ = 0 if tt < 4 else 1
            self.norm_tile(
                tt,
                gain_fn=lambda kc: self.gm.v((slice(None), 1, kc, slice(typ, typ + 1)), subs=1),
                shift_fn=lambda kc: self.modc.v((slice(None), l, 3, kc, slice(typ, typ + 1)), subs=l),
                out_fn=lambda kc: [h32.v((slice(None), kc, slice(0, n)), subs=kc),
                                   self.hT.v((slice(None), kc, slice(t0, t0 + n)), subs=self.xsub(kc, tt))],
                tmp=(sq, rstd, tmul), ps_i=0 + (tt % 2))
            for sub in range(n // 128):
                i2 = it % 2
                it += 1
                pl = self.ps[2 + i2]
                for kc in range(KC):
                    P.mm(pl.v((slice(None), slice(0, NE)), subs=0),
                         h32.v((slice(None), kc, slice(sub * 128, (sub + 1) * 128)), subs=kc),
                         rw.v((slice(None), kc, slice(None))), start=(kc == 0), stop=(kc == KC - 1))
                L = lg[i2]
                P.tt("dve", L.v(), pl.v((slice(None), slice(0, NE)), subs=0), rb.v(), ALU.add)
                P.op("dve", lambda e, o=top8[i2], i=L: e.max(o.ap, i.ap), reads=[L.v()], writes=[top8[i2].v()])
                P.ts("dve", mk[i2].v(), L.v(), top8[i2].v((slice(None), slice(3, 4))), None, ALU.is_ge)
                P.ts("dve", sm[i2].v((slice(None), slice(0, 1))), top8[i2].v((slice(None), slice(0, 1))), -1.0, None, ALU.mult)
                P.act(ex[i2].v(), L.v(), AF.Exp, bias=sm[i2].v((slice(None), slice(0, 1))))
                P.tt("dve", ex[i2].v(), ex[i2].v(), mk[i2].v(), ALU.mult)
                P.op("dve", lambda e, o=sm[i2], i=ex[i2]: e.reduce_sum(o.ap[:, 1:2], i.ap, axis=AX.X),
                     reads=[ex[i2].v()], writes=[sm[i2].v()])
                P.recip(sm[i2].v((slice(None), slice(2, 3))), sm[i2].v((slice(None), slice(1, 2))))
                P.ts("dve", ex[i2].v(), ex[i2].v(), sm[i2].v((slice(None), slice(2, 3))), None, ALU.mult)
                pg = self.ps[4 + i2]
                P.transpose(pg.v((slice(0, NE), slice(0, 128)), subs=0), ex[i2].v(), self.ident.v())
                tok = t0 + sub * 128
                P.copy("act", gatesT.v((slice(None), slice(tok, tok + 128)), subs=tt),
                       pg.v((slice(0, NE), slice(0, 128)), subs=0))
        P.dma("sp", self.gscr[:, 0:Tl], gatesT.ap[:, 0:Tl], reads=[gatesT.v()], writes=[self.gscr_buf.v()])

        self.new_phase()
        bgc = self.alloc("bgc", [128, KC, NE], F32)
        buc = self.alloc("buc", [128, KC, NE], F32)
        mark = self.top
        bstage = self.alloc("bstage", [NE, D], F32)
        bd = self.alloc("bd", [NE, D], F32)
        gT32 = self.alloc("gT32", [NE, T], F32)
        self.rows_to_cols(self.b_gate[l], NE, lambda kc: bgc.v((slice(None), kc, slice(None))), bstage, 7)
        self.rows_to_cols(self.b_up[l], NE, lambda kc: buc.v((slice(None), kc, slice(None))), bstage, 7)
        P.dma("sp", bd.ap, self.b_down[l], writes=[bd.v()])
        P.dma("sp", gT32.ap[:, 0:Tl], self.gscr[:, 0:Tl], reads=[self.gscr_buf.v()], writes=[gT32.v()])
        g2 = lambda kc, typ: self.modc.v((slice(None), l, 5, kc, slice(typ, typ + 1)), subs=l)
        for tt in range(ntt):
            t0, n = TT[tt]
            typ = 0 if tt < 4 else 1
            for dc in range(KC):
                pb = self.ps[dc % 2]
                P.mm(pb.v((slice(None), slice(0, n))), bd.v((slice(None), slice(dc * 128, (dc + 1) * 128))),
                     gT32.v((slice(None), slice(t0, t0 + n))))
                xv = self.xT.v((slice(None), dc, slice(t0, t0 + n)), subs=self.xsub(dc, tt))
                P.stt("dve", xv, pb.v((slice(None), slice(0, n))), g2(dc, typ), xv, ALU.mult, ALU.add)
        self.release(mark)
        NU = 2
        wg = [self.alloc(f"wg{i}", [128, KC, 512], BF16) for i in range(NU)]
        wu = [self.alloc(f"wu{i}", [128, KC, 512], BF16) for i in range(NU)]
        wd = [self.alloc(f"wd{i}", [128, 4, D], BF16) for i in range(NU)]
        G = [self.alloc(f"G{i}", [128, T], BF16) for i in range(2)]
        aT = [self.alloc(f"aT{i}", [128, 4, 512], BF16, nsub=4) for i in range(2)]
        gcb = [self.alloc(f"gcb{i}", [128, 512], F32) for i in range(2)]
        sgb = [self.alloc(f"sgb{i}", [128, 512], BF16) for i in range(2)]
        u1b = [self.alloc(f"u1b{i}", [128, 512], F32) for i in range(2)]
        un = 0
        an = 0
        tn = 0
        for e in range(self.n_experts):
            Ge = G[e % 2]
            P.dma("pool", Ge.ap[:, 0:Tl], self.gscr[e:e + 1, 0:Tl].to_broadcast([128, Tl]),
                  reads=[self.gscr_buf.v()], writes=[Ge.v()])
            for half in range(2):
                u = un % NU
                un += 1
                fsl = slice(half * 512, (half + 1) * 512)
                P.dma("pool", wg[u].ap, self.w_gate[self.lmap[l], e].rearrange("(kc p) f -> p kc f", p=128)[:, :, fsl],
                      writes=[wg[u].v()])
                P.dma("pool", wu[u].ap, self.w_up[self.lmap[l], e].rearrange("(kc p) f -> p kc f", p=128)[:, :, fsl],
                      writes=[wu[u].v()])
                P.dma("pool", wd[u].ap, self.w_down[self.lmap[l], e].rearrange("(fc p) d -> p fc d", p=128)[:, half * 4:half * 4 + 4, :],
                      writes=[wd[u].v()])
                for tt in range(ntt):
                    t0, n = TT[tt]
                    typ = 0 if tt < 4 else 1
                    A = aT[an % 2]
                    an += 1
                    for fc in range(4):
                        fcg = half * 4 + fc
                        pg_ = self.ps[2 + (tn % 2)]
                        pu_ = self.ps[4 + (tn % 2)]
                        gc, sg, u1 = gcb[tn % 2], sgb[tn % 2], u1b[tn % 2]
                        tn += 1
                        for kc in range(KC):
                            P.mm(pg_.v((slice(None), slice(0, n))), wg[u].v((slice(None), kc, slice(fc * 128, (fc + 1) * 128))),
                                 self.hT.v((slice(None), kc, slice(t0, t0 + n)), subs=self.xsub(kc, tt)),
                                 start=(kc == 0), stop=(kc == KC - 1))
                        for kc in range(KC):
                            P.mm(pu_.v((slice(None), slice(0, n))), wu[u].v((slice(None), kc, slice(fc * 128, (fc + 1) * 128))),
                                 self.hT.v((slice(None), kc, slice(t0, t0 + n)), subs=self.xsub(kc, tt)),
                                 start=(kc == 0), stop=(kc == KC - 1))
                        gv = gc.v((slice(None), slice(0, n)))
                        sv = sg.v((slice(None), slice(0, n)))
                        uv = u1.v((slice(None), slice(0, n)))
                        P.ts("dve", gv, pg_.v((slice(None), slice(0, n))), bgc.v((slice(None), fcg, slice(e, e + 1))), 7.0,
                             ALU.add, ALU.min)
                        P.act(sv, gv, AF.Sigmoid, scale=1.702)
                        P.ts("dve", uv, pu_.v((slice(None), slice(0, n))), buc.v((slice(None), fcg, slice(e, e + 1))), -7.0,
                             ALU.add, ALU.max)
                        P.ts("pool", uv, uv, 7.0, 1.0, ALU.min, ALU.add)
                        P.tt("pool", gv, gv, sv, ALU.mult)
                        P.tt("pool", uv, uv, Ge.v((slice(None), slice(t0, t0 + n))), ALU.mult)
                        P.tt("dve", A.v((slice(None), fc, slice(0, n)), subs=fc), gv, uv, ALU.mult)
                    for dc in range(KC):
                        py = self.ps[dc % 2]
                        for fc in range(4):
                            P.mm(py.v((slice(None), slice(0, n))), wd[u].v((slice(None), fc, slice(dc * 128, (dc + 1) * 128))),
                                 A.v((slice(None), fc, slice(0, n)), subs=fc), start=(fc == 0), stop=(fc == 3))
                        xv = self.xT.v((slice(None), dc, slice(t0, t0 + n)), subs=self.xsub(dc, tt))
                        P.stt("dve", xv, py.v((slice(None), slice(0, n))), g2(dc, typ), xv, ALU.mult, ALU.add)

    def norm1(self, l):
        P = self.P
        self.new_phase()
        self.prep_gm(l, 0)
        sq = [self.alloc(f"n1sq{i}", [128, 512], F32) for i in range(2)]
        rstd = self.alloc("n1rstd", [128, 512], F32)
        tmul = [self.alloc(f"n1tmul{i}", [128, 512], F32) for i in range(2)]
        for tt in range(5):
            t0, n = TT[tt]
            typ = 0 if tt < 4 else 1
            self.norm_tile(
                tt,
                gain_fn=lambda kc: self.gm.v((slice(None), 0, kc, slice(typ, typ + 1)), subs=0),
                shift_fn=lambda kc: self.modc.v((slice(None), l, 0, kc, slice(typ, typ + 1)), subs=l),
                out_fn=lambda kc: [self.hT.v((slice(None), kc, slice(t0, t0 + n)), subs=self.xsub(kc, tt))],
                tmp=(sq, rstd, tmul), ps_i=0 + (tt % 2))

    def hTv(self, kc, tt, lo=None, n=None):
        t0, nn = TT[tt]
        if lo is None:
            lo, n = t0, nn
        return self.hT.v((slice(None), kc, slice(lo, lo + n)), subs=self.xsub(kc, tt))

    def attn_setup(self):
        self.Pt = [self.alloc(f"Pt{i}", [128, 512], BF16) for i in range(2)]
        self.St = [self.alloc(f"St{i}", [128, 512], F32) for i in range(2)]
        self.rd = [self.alloc(f"rd{i}", [64, 512], F32) for i in range(2)]
        self.a_n = 0
        self.s_n = 0

    def attend(self, qparts, chunks, QW, out_v, sink_v, scale):
        P = self.P
        G = 512 // QW
        ob = self.ps[2 + self.a_n % 2]
        db = self.ps[4 + self.a_n % 2]
        rd = self.rd[self.a_n % 2]
        self.a_n += 1
        ng = (len(chunks) + G - 1) // G
        sl64 = slice(0, 64)
        for gi in range(ng):
            cs = chunks[gi * G:(gi + 1) * G]
            sb = self.ps[self.s_n % 2]
            pt = self.Pt[self.s_n % 2]
            st = self.St[self.s_n % 2]
            self.s_n += 1
            for j, ch in enumerate(cs):
                cols = slice(j * QW, (j + 1) * QW)
                for pi, (kv, qv) in enumerate(zip(ch["k"], qparts)):
                    P.mm(sb.v((slice(None), cols)), kv, qv, start=(pi == 0), stop=(pi == len(qparts) - 1))
            j = 0
            while j < len(cs):
                if cs[j]["mask"] is None:
                    j2 = j
                    while j2 < len(cs) and cs[j2]["mask"] is None:
                        j2 += 1
                    cols = slice(j * QW, j2 * QW)
                    P.act(pt.v((slice(None), cols)), sb.v((slice(None), cols)), AF.Exp, scale=scale)
                    j = j2
                else:
                    cols = slice(j * QW, (j + 1) * QW)
                    P.stt("dve", st.v((slice(None), cols)), sb.v((slice(None), cols)), scale, cs[j]["mask"],
                          ALU.mult, ALU.add)
                    P.act(pt.v((slice(None), cols)), st.v((slice(None), cols)), AF.Exp)
                    j += 1
            for j, ch in enumerate(cs):
                cols = slice(j * QW, (j + 1) * QW)
                first = (gi == 0 and j == 0)
                lastc = (gi == ng - 1 and j == len(cs) - 1)
                P.mm(ob.v((sl64, slice(0, QW))), ch["v"], pt.v((slice(None), cols)), start=first, stop=lastc)
                P.mm(db.v((sl64, slice(0, QW))), self.ones16.v(), pt.v((slice(None), cols)), start=first, stop=lastc)
        rv = rd.v((sl64, slice(0, QW)))
        if sink_v is not None:
            P.ts("dve", rv, db.v((sl64, slice(0, QW))), sink_v, None, ALU.add)
            P.recip(rv, rv)
        else:
            P.recip(rv, db.v((sl64, slice(0, QW))))
        P.tt("dve", out_v, ob.v((sl64, slice(0, QW))), rv, ALU.mult)

    def out_proj(self, l, wo, OTb, nh, ntt):
        P = self.P
        for tt in range(ntt):
            t0, n = TT[tt]
            typ = 0 if tt < 4 else 1
            for dc in range(KC):
                pb = self.ps[6 + dc % 2]
                for hh in range(nh):
                    P.mm(pb.v((slice(None), slice(0, n))), wo.v((slice(None), hh, slice(dc * 128, (dc + 1) * 128))),
                         OTb.v((slice(None), hh, slice(t0, t0 + n)), subs=hh), start=(hh == 0), stop=(hh == nh - 1))
                xv = self.xT.v((slice(None), dc, slice(t0, t0 + n)), subs=self.xsub(dc, tt))
                P.stt("dve", xv, pb.v((slice(None), slice(0, n))),
                      self.modc.v((slice(None), l, 2, dc, slice(typ, typ + 1)), subs=l), xv, ALU.mult, ALU.add)

    def rope_proj(self, dst_v, wA_fn, wB_fn, rhs_fn, nkc, n, t0, cosT, sinT, prow, M, rt):
        P = self.P
        pa, pb = self.ps[6], self.ps[7]
        for kc in range(nkc):
            P.mm(pa.v((slice(0, M), slice(0, n))), wA_fn(kc), rhs_fn(kc), start=(kc == 0), stop=(kc == nkc - 1))
        for kc in range(nkc):
            P.mm(pb.v((slice(0, M), slice(0, n))), wB_fn(kc), rhs_fn(kc), start=(kc == 0), stop=(kc == nkc - 1))
        t1, t2 = rt
        P.tt("dve", t1.v((prow, slice(0, n))), pa.v((prow, slice(0, n))), cosT.v((prow, slice(t0, t0 + n))), ALU.mult)
        P.tt("dve", t2.v((prow, slice(0, n))), pb.v((prow, slice(0, n))), sinT.v((prow, slice(t0, t0 + n))), ALU.mult)
        P.tt("pool", dst_v, t1.v((prow, slice(0, n))), t2.v((prow, slice(0, n))), ALU.add)
        return pa

    def mixer_a(self, l, last):
        P = self.P
        slot = l // 3
        self.new_phase()
        nttq = 4 if last else 5
        scale = 64 ** -0.5
        cosT = self.alloc("cosA", [64, T], F32)
        sinT = self.alloc("sinA", [64, T], F32)
        P.dma("sp", cosT.ap, self.ropeA[0], writes=[cosT.v()])
        P.dma("sp", sinT.ap, self.ropeA[1], writes=[sinT.v()])
        mprev = self.alloc("mprev", [128, 128], F32)
        mnext = self.alloc("mnext", [128, 128], F32)
        P.dma("sp", mprev.ap, self.maskA[0], writes=[mprev.v()])
        P.dma("sp", mnext.ap, self.maskA[1], writes=[mnext.v()])
        sinkx = self.alloc("sinkx", [64, 16], F32)
        P.dma("sp", sinkx.ap, self.a_sink[slot:slot + 1, :].to_broadcast([64, 16]), writes=[sinkx.v()])
        P.act(sinkx.v(), sinkx.v(), AF.Exp)
        self.attn_setup()
        KTb = self.alloc("KT", [64, T], BF16)
        Vb = self.alloc("V", [128, 18, 64], BF16)
        QTb = self.alloc("QT", [64, 2, T], BF16, nsub=2)
        OTb = self.alloc("OT", [64, 2, T], BF16, nsub=2)
        wk = self.alloc("wk", [128, KC, 128], BF16)
        wv = self.alloc("wv", [128, KC, 64], BF16)
        wq = self.alloc("wq", [128, KC, 256], BF16)
        wo = self.alloc("wo", [64, 2, D], BF16)
        rt = [self.alloc(f"rt{i}", [64, 512], F32) for i in range(2)]
        win = self.a_w_in[slot].rearrange("(kc p) c -> p kc c", p=128)
        wsw = self.a_w_in_sw[slot].rearrange("(kc p) c -> p kc c", p=128)
        sl64 = slice(0, 64)
        for g in range(4):
            kc0 = 1024 + g * 64
            P.dma("pool", wk.ap[:, :, 0:64], win[:, :, kc0:kc0 + 64], writes=[wk.v()])
            P.dma("pool", wk.ap[:, :, 64:128], wsw[:, :, kc0:kc0 + 64], writes=[wk.v()])
            P.dma("pool", wv.ap, win[:, :, 1280 + g * 64:1280 + (g + 1) * 64], writes=[wv.v()])
            for tt in range(5):
                t0, n = TT[tt]
                self.rope_proj(KTb.v((sl64, slice(t0, t0 + n))),
                               lambda kc: wk.v((slice(None), kc, slice(0, 64))),
                               lambda kc: wk.v((slice(None), kc, slice(64, 128))),
                               lambda kc: self.hTv(kc, tt), KC, n, t0, cosT, sinT, sl64, 64, rt)
            for tci in range(18):
                tt = min(tci // 4, 4)
                pb = self.ps[6 + tci % 2]
                for kc in range(KC):
                    P.mm(pb.v((slice(None), slice(0, 64))), self.hTv(kc, tt, tci * 128, 128), wv.v((slice(None), kc, slice(None))),
                         start=(kc == 0), stop=(kc == KC - 1))
                P.copy("act", Vb.v((slice(None), tci, slice(None))), pb.v((slice(None), slice(0, 64))))
            kch = lambda ci, m: dict(k=[KTb.v((sl64, slice(ci * 128, (ci + 1) * 128)))],
                                     v=Vb.v((slice(None), ci, slice(None))), mask=m)
            for half in range(2):
                h0 = g * 4 + half * 2
                P.dma("pool", wq.ap[:, :, 0:128], win[:, :, h0 * 64:(h0 + 2) * 64], writes=[wq.v()])
                P.dma("pool", wq.ap[:, :, 128:256], wsw[:, :, h0 * 64:(h0 + 2) * 64], writes=[wq.v()])
                P.dma("pool", wo.ap, self.a_w_out[slot][h0 * 64:(h0 + 2) * 64, :].rearrange("(h d) n -> d h n", d=64),
                      writes=[wo.v()])
                for hh in range(2):
                    for tt in range(nttq):
                        t0, n = TT[tt]
                        self.rope_proj(QTb.v((sl64, hh, slice(t0, t0 + n)), subs=hh),
                                       lambda kc: wq.v((slice(None), kc, slice(hh * 64, (hh + 1) * 64))),
                                       lambda kc: wq.v((slice(None), kc, slice(128 + hh * 64, 128 + (hh + 1) * 64))),
                                       lambda kc: self.hTv(kc, tt), KC, n, t0, cosT, sinT, sl64, 64, rt)
                for hh in range(2):
                    h = h0 + hh
                    sv = sinkx.v((slice(None), slice(h, h + 1)))
                    for qb in range(16):
                        chunks = [kch(qb, None), kch(16, None), kch(17, None)]
                        if qb > 0:
                            chunks.append(kch(qb - 1, mprev.v()))
                        if qb < 15:
                            chunks.append(kch(qb + 1, mnext.v()))
                        qs = slice(qb * 128, (qb + 1) * 128)
                        self.attend([QTb.v((sl64, hh, qs), subs=hh)], chunks, 128, OTb.v((sl64, hh, qs), subs=hh), sv, scale)
                    if not last:
                        for qb in (16, 17):
                            qs = slice(qb * 128, (qb + 1) * 128)
                            self.attend([QTb.v((sl64, hh, qs), subs=hh)], [kch(16, None), kch(17, None)], 128,
                                        OTb.v((sl64, hh, qs), subs=hh), sv, scale)
                self.out_proj(l, wo, OTb, 2, nttq)

    def mixer_b(self, l):
        P = self.P
        self.new_phase()
        scale = 64 ** -0.5
        NEG = -30000.0
        self.attn_setup()
        QTb = self.alloc("QT", [64, 2, T], BF16, nsub=2)
        KTb = self.alloc("KT", [64, 2, T], BF16, nsub=2)
        Vb = self.alloc("V", [128, 18, 128], BF16)
        OTb = self.alloc("OT", [64, 2, T], BF16, nsub=2)
        wqkv = self.alloc("wqkv", [128, KC, 384], BF16)
        wo = self.alloc("wo", [64, 2, D], BF16)
        bm = [[self.alloc(f"bm{a_}_{b_}", [128, 128], F32) for b_ in range(5)] for a_ in range(5)]
        win = self.b_w_in.rearrange("(kc p) c -> p kc c", p=128)
        sl64 = slice(0, 64)
        reps = [0, 1, 2, 14, 15]
        for hp in range(8):
            h0 = hp * 2
            for j in range(3):
                P.dma("pool", wqkv.ap[:, :, j * 128:(j + 1) * 128], win[:, :, j * 1024 + h0 * 64:j * 1024 + (h0 + 2) * 64],
                      writes=[wqkv.v()])
            P.dma("pool", wo.ap, self.b_w_out[h0 * 64:(h0 + 2) * 64, :].rearrange("(h d) n -> d h n", d=64), writes=[wo.v()])
            n_ = 0
            for hh in range(2):
                for j, dstb in ((0, QTb), (1, KTb)):
                    for tt in range(5):
                        t0, n = TT[tt]
                        pb = self.ps[6 + n_ % 2]
                        n_ += 1
                        for kc in range(KC):
                            P.mm(pb.v((sl64, slice(0, n))), wqkv.v((slice(None), kc, slice(j * 128 + hh * 64, j * 128 + (hh + 1) * 64))),
                                 self.hTv(kc, tt), start=(kc == 0), stop=(kc == KC - 1))
                        P.copy("act", dstb.v((sl64, hh, slice(t0, t0 + n)), subs=hh), pb.v((sl64, slice(0, n))))
            for tci in range(18):
                tt = min(tci // 4, 4)
                pb = self.ps[6 + tci % 2]
                for kc in range(KC):
                    P.mm(pb.v((slice(None), slice(0, 128))), self.hTv(kc, tt, tci * 128, 128), wqkv.v((slice(None), kc, slice(256, 384))),
                         start=(kc == 0), stop=(kc == KC - 1))
                P.copy("act", Vb.v((slice(None), tci, slice(None))), pb.v((slice(None), slice(0, 128))))
            for hh in range(2):
                h = h0 + hh
                import os
                for cls, rp in enumerate(reps):
                    cs = min(max(2 * rp - 4, 0), 22)
                    if os.environ.get("B_MODE", "dma") in ("nolocal", "nomask", "nofill"):
                        break
                    for c in range(5):
                        for a in range(2):
                            for b in range(2):
                                kr = cs + 2 * c + a
                                qr = 2 * rp + b
                                r0 = min(max(qr - 4, 0), 24)
                                dst = bm[cls][c].v((slice(a * 64, (a + 1) * 64), slice(b * 64, (b + 1) * 64)))
                                dr = (kr - qr + 7) if (r0 <= kr < r0 + 8) else 15
                                P.dma("sp", dst.ap, self.b_tm[h, dr], writes=[dst])
                kch = lambda ci, m: dict(k=[KTb.v((sl64, hh, slice(ci * 128, (ci + 1) * 128)), subs=hh)],
                                         v=Vb.v((slice(None), ci, slice(hh * 64, (hh + 1) * 64))), mask=m)
                for rp in range(16):
                    cls = {0: 0, 1: 1, 14: 3, 15: 4}.get(rp, 2)
                    cs = min(max(2 * rp - 4, 0), 22)
                    chunks = [kch(16, None), kch(17, None)]
                    import os
                    for c in range(5):
                        if os.environ.get("B_MODE", "dma") == "nolocal":
                            break
                        if os.environ.get("B_MODE", "dma") == "nomask":
                            chunks.append(kch(cs // 2 + c, None))
                            continue
                        chunks.append(kch(cs // 2 + c, bm[cls][c].v()))
                    qs = slice(rp * 128, (rp + 1) * 128)
                    self.attend([QTb.v((sl64, hh, qs), subs=hh)], chunks, 128, OTb.v((sl64, hh, qs), subs=hh), None, scale)
                for qb in (16, 17):
                    qs = slice(qb * 128, (qb + 1) * 128)
                    self.attend([QTb.v((sl64, hh, qs), subs=hh)], [kch(16, None), kch(17, None)], 128,
                                OTb.v((sl64, hh, qs), subs=hh), None, scale)
            self.out_proj(l, wo, OTb, 2, 5)

    def mixer_c(self, l):
        P = self.P
        self.new_phase()
        scale = 96 ** -0.5
        cosT = self.alloc("cosC", [96, T], F32)
        sinT = self.alloc("sinC", [96, T], F32)
        P.dma("sp", cosT.ap, self.ropeC[0], writes=[cosT.v()])
        P.dma("sp", sinT.ap, self.ropeC[1], writes=[sinT.v()])
        cqT = self.alloc("cqT", [128, 2, T], BF16)
        ckvT = self.alloc("ckvT", [128, T], BF16)
        krT = self.alloc("krT", [96, T], BF16)
        ncol = self.alloc("cnorm", [128, 3], F32)
        rt = [self.alloc(f"rt{i}", [96, 512], F32) for i in range(2)]
        mark = self.top
        w1 = self.alloc("w1", [128, KC, 416], BF16)
        w1s = self.alloc("w1s", [128, KC, 96], BF16)
        P.dma("pool", w1.ap, self.c_w_in.rearrange("(kc p) c -> p kc c", p=128), writes=[w1.v()])
        P.dma("pool", w1s.ap, self.c_w_in_sw.rearrange("(kc p) c -> p kc c", p=128), writes=[w1s.v()])
        nst = self.alloc("nst", [3, 128], F32)
        P.dma("sp", nst.ap, self.c_norms, writes=[nst.v()])
        pv = self.ps[5].v((slice(None), slice(0, 3)))
        P.transpose(pv, nst.v(), self.ident.v((slice(0, 3), slice(0, 3))))
        P.copy("dve", ncol.v(), pv)
        c32 = self.alloc("c32", [128, 3, 512], F32, nsub=3)
        sq = [self.alloc(f"csq{i}", [128, 512], F32) for i in range(2)]
        rs = [self.alloc(f"crs{i}", [128, 512], F32) for i in range(2)]
        r96 = slice(64, 96)
        for tt in range(5):
            t0, n = TT[tt]
            sn = slice(0, n)
            for s3 in range(3):
                pb = self.ps[6 + s3 % 2]
                for kc in range(KC):
                    P.mm(pb.v((slice(None), sn)), w1.v((slice(None), kc, slice(s3 * 128, (s3 + 1) * 128))), self.hTv(kc, tt),
                         start=(kc == 0), stop=(kc == KC - 1))
                P.copy("act", c32.v((slice(None), s3, sn), subs=s3), pb.v((slice(None), sn)))
            for which, slots, nf in ((0, (0, 1), 256.0), (1, (2,), 128.0)):
                pr = self.ps[0 + which]
                for i, s3 in enumerate(slots):
                    P.act(sq[i % 2].v((slice(None), sn)), c32.v((slice(None), s3, sn), subs=s3), AF.Square)
                    P.mm(pr.v((slice(None), sn)), self.ones32.v(), sq[i % 2].v((slice(None), sn)),
                         start=(i == 0), stop=(i == len(slots) - 1))
                r = rs[which]
                P.ts("dve", r.v((slice(None), sn)), pr.v((slice(None), sn)), 1.0 / nf, EPS, ALU.mult, ALU.add)
                P.act(r.v((slice(None), sn)), r.v((slice(None), sn)), AF.Sqrt)
                P.recip(r.v((slice(None), sn)), r.v((slice(None), sn)))
                for s3 in slots:
                    P.tt("dve", c32.v((slice(None), s3, sn), subs=s3), c32.v((slice(None), s3, sn), subs=s3),
                         r.v((slice(None), sn)), ALU.mult)
                    dst = cqT.v((slice(None), s3, slice(t0, t0 + n))) if which == 0 else ckvT.v((slice(None), slice(t0, t0 + n)))
                    P.act(dst, c32.v((slice(None), s3, sn), subs=s3), AF.Identity, scale=ncol.v((slice(None), slice(s3, s3 + 1))))
            self.rope_proj(krT.v((r96, slice(t0, t0 + n))),
                           lambda kc: w1.v((slice(None), kc, slice(320, 416))),
                           lambda kc: w1s.v((slice(None), kc, slice(0, 96))),
                           lambda kc: self.hTv(kc, tt), KC, n, t0, cosT, sinT, r96, 96, rt)
        self.release(mark)
        self.attn_setup()
        q96 = self.alloc("q96", [96, 2, T], BF16, nsub=2)
        k96 = self.alloc("k96", [96, 2, T], BF16, nsub=2)
        Vb = self.alloc("V", [128, 18, 128], BF16)
        OTb = self.alloc("OT", [64, 2, T], BF16, nsub=2)
        wuq = self.alloc("wuq", [128, 2, 192], BF16)
        wuqs = self.alloc("wuqs", [128, 2, 192], BF16)
        wukv = self.alloc("wukv", [128, 256], BF16)
        wo = self.alloc("wo", [64, 2, D], BF16)
        uq = self.c_w_uq.rearrange("(kc p) c -> p kc c", p=128)
        uqs = self.c_w_uq_sw.rearrange("(kc p) c -> p kc c", p=128)
        sl64 = slice(0, 64)
        for hp in range(8):
            h0 = hp * 2
            P.dma("pool", wuq.ap, uq[:, :, h0 * 96:(h0 + 2) * 96], writes=[wuq.v()])
            P.dma("pool", wuqs.ap, uqs[:, :, h0 * 96:(h0 + 2) * 96], writes=[wuqs.v()])
            P.dma("pool", wukv.ap, self.c_w_ukv[:, h0 * 128:(h0 + 2) * 128], writes=[wukv.v()])
            P.dma("pool", wo.ap, self.c_w_out[h0 * 64:(h0 + 2) * 64, :].rearrange("(h d) n -> d h n", d=64), writes=[wo.v()])
            for hh in range(2):
                for tt in range(5):
                    t0, n = TT[tt]
                    pa = self.rope_proj(q96.v((r96, hh, slice(t0, t0 + n)), subs=hh),
                                        lambda kc: wuq.v((slice(None), kc, slice(hh * 96, (hh + 1) * 96))),
                                        lambda kc: wuqs.v((slice(None), kc, slice(hh * 96, (hh + 1) * 96))),
                                        lambda kc: cqT.v((slice(None), kc, slice(t0, t0 + n))), 2, n, t0, cosT, sinT, r96, 96, rt)
                    P.copy("act", q96.v((sl64, hh, slice(t0, t0 + n)), subs=hh), pa.v((sl64, slice(0, n))))
                    pk = self.ps[6 + 0]
                    P.mm(pk.v((sl64, slice(0, n))), wukv.v((slice(None), slice(hh * 128, hh * 128 + 64))),
                         ckvT.v((slice(None), slice(t0, t0 + n))))
                    P.copy("act", k96.v((sl64, hh, slice(t0, t0 + n)), subs=hh), pk.v((sl64, slice(0, n))))
                P.copy("pool", k96.v((r96, hh, slice(None)), subs=hh), krT.v((r96, slice(None))))
            for tci in range(18):
                pb = self.ps[6 + tci % 2]
                for hh in range(2):
                    P.mm(pb.v((slice(None), slice(hh * 64, (hh + 1) * 64))), ckvT.v((slice(None), slice(tci * 128, (tci + 1) * 128))),
                         wukv.v((slice(None), slice(hh * 128 + 64, (hh + 1) * 128))))
                P.copy("act", Vb.v((slice(None), tci, slice(None))), pb.v((slice(None), slice(0, 128))))
            r0_96 = slice(0, 96)
            for hh in range(2):
                kch = lambda ci: dict(k=[k96.v((r0_96, hh, slice(ci * 128, (ci + 1) * 128)), subs=hh)],
                                      v=Vb.v((slice(None), ci, slice(hh * 64, (hh + 1) * 64))), mask=None)
                for qt in range(4):
                    t0, n = TT[qt]
                    self.attend([q96.v((r0_96, hh, slice(t0, t0 + n)), subs=hh)], [kch(ci) for ci in range(18)], 512,
                                OTb.v((sl64, hh, slice(t0, t0 + n)), subs=hh), None, scale)
                t0, n = TT[4]
                self.attend([q96.v((r0_96, hh, slice(t0, t0 + n)), subs=hh)], [kch(16), kch(17)], 256,
                            OTb.v((sl64, hh, slice(t0, t0 + n)), subs=hh), None, scale)
            self.out_proj(l, wo, OTb, 2, 5)

    def epilogue(self, do_norm=True):
        P = self.P
        self.new_phase()
        sq = [self.alloc(f"esq{i}", [128, 512], F32) for i in range(2)]
        rstd = self.alloc("erstd", [128, 512], F32)
        tmul = [self.alloc(f"etmul{i}", [128, 512], F32) for i in range(2)]
        hn = self.alloc("hn", [128, KC, 512], F32, nsub=KC)
        ost = [self.alloc(f"ost{i}", [128, D], F32) for i in range(2)]
        outbuf = Buf("outdram", self.out, 16)
        n = 0
        for tt in range(4):
            t0, _ = TT[tt]
            if do_norm:
                self.norm_tile(tt, gain_fn=lambda kc: self.normc.v((slice(None), kc, slice(8, 9))),
                               shift_fn=lambda kc: None,
                               out_fn=lambda kc: [hn.v((slice(None), kc, slice(None)), subs=kc)],
                               tmp=(sq, rstd, tmul), ps_i=0 + (tt % 2))
            else:
                for kc in range(KC):
                    P.copy("pool", hn.v((slice(None), kc, slice(None)), subs=kc),
                           self.xT.v((slice(None), kc, slice(t0, t0 + 512)), subs=self.xsub(kc, tt)))
            for sub in range(4):
                o = ost[n % 2]
                for half in range(2):
                    pb = self.ps[2 + (n * 2 + half) % 4]
                    for j in range(4):
                        kc = half * 4 + j
                        P.transpose(pb.v((slice(None), slice(j * 128, (j + 1) * 128)), subs=j),
                                    hn.v((slice(None), kc, slice(sub * 128, (sub + 1) * 128)), subs=kc), self.ident.v())
                    P.copy("dve" if half == 0 else "act", o.v((slice(None), slice(half * 512, (half + 1) * 512))), pb.v())
                tok = t0 + sub * 128
                P.dma("sp", self.out[tok:tok + 128, :], o.ap, reads=[o.v()], writes=[outbuf.v(subs=tok // 128)])
                n += 1
        P.op("sp", lambda e: e.nop(), reads=[outbuf.v()])

    def build(self, final_norm=True):
        self.consts()
        self.load_small()
        self.load_x()
        for l in self.layers:
            last = (l == DEPTH - 1)
            if self.do_mixer:
                self.norm1(l)
                kind = l % 3
                if kind == 0:
                    self.mixer_a(l, last)
                elif kind == 1:
                    self.mixer_b(l)
                else:
                    self.mixer_c(l)
            if self.do_moe:
                self.ffn_phase(l, last)
        self.epilogue(final_norm)
        return self.P.emit()


def rope_tables(rot_dim, nrows, row_off):
    f32 = np.float32
    t = np.arange(S)
    row = (t // 64).astype(f32)
    col = (t % 64).astype(f32)
    nf = rot_dim // 4
    inv = (f32(10000.0) ** (-np.arange(nf, dtype=f32) / f32(nf))).astype(f32)
    ang = np.concatenate([row[:, None] * inv, col[:, None] * inv], axis=-1).astype(f32)
    cos = np.cos(ang).astype(f32)
    sin = np.sin(ang).astype(f32)
    out = np.zeros((2, nrows, T), f32)
    out[0] = 1.0
    for d in range(rot_dim):
        i = d // 2
        out[0, row_off + d, :S] = cos[:, i]
        out[1, row_off + d, :S] = -sin[:, i] if d % 2 == 0 else sin[:, i]
    return out


def mixer_host_inputs(inp):
    f = lambda a: np.ascontiguousarray(np.asarray(a, dtype=np.float32))
    NEG = np.float32(-30000.0)
    a_w_in = f(inp["a_w_in"])
    perm = np.arange(1280) ^ 1
    a_sw = a_w_in[:, :, perm]
    j = np.arange(128)[:, None]
    i = np.arange(128)[None, :]
    maskA = np.stack([np.where(j >= i, 0.0, NEG), np.where(j <= i, 0.0, NEG)]).astype(np.float32)
    rpb = f(inp["b_rpb"])[0]
    kc = np.arange(64)[:, None]
    qc = np.arange(64)[None, :]
    ws = np.clip(qc - 8, 0, 48)
    ok = (kc >= ws) & (kc < ws + 16)
    idx = np.clip(kc - qc + 15, 0, 30)
    tm = np.full((16, 16, 64, 64), NEG, np.float32)
    tm[:, :15] = np.where(ok[None, None], rpb[:, :, idx], NEG)
    c_w_in = f(inp["c_w_in"])[0]
    c_sw = c_w_in[:, 320:416].copy()
    c_sw[:, 64:96] = c_w_in[:, 384:416][:, np.arange(32) ^ 1]
    uq = f(inp["c_w_uq"])[0]
    uqs = uq.copy()
    for h in range(16):
        uqs[:, h * 96 + 64:h * 96 + 96] = uq[:, h * 96 + 64:h * 96 + 96][:, np.arange(32) ^ 1]
    c_norms = np.stack([inp["c_q_norm"][0][:128], inp["c_q_norm"][0][128:], inp["c_kv_norm"][0]])
    return {
        "a_w_in": a_w_in, "a_w_in_sw": f(a_sw), "a_w_out": f(inp["a_w_out"]), "a_sink": f(inp["a_sink"]),
        "ropeA": rope_tables(64, 64, 0), "maskA": maskA,
        "b_w_in": f(inp["b_w_in"])[0], "b_w_out": f(inp["b_w_out"])[0], "b_tm": f(tm),
        "c_w_in": c_w_in, "c_w_in_sw": f(c_sw), "c_norms": f(c_norms),
        "c_w_uq": uq, "c_w_uq_sw": f(uqs), "c_w_ukv": f(inp["c_w_ukv"])[0], "c_w_out": f(inp["c_w_out"])[0],
        "ropeC": rope_tables(32, 96, 64),
    }


def make_in_maps(inp, ncores=8, mk=None):
    f = lambda a: np.ascontiguousarray(np.asarray(a, dtype=np.float32))
    ml = mk.moe_layers if (mk is not None and mk.moe_layers) else [0]
    nx = max(1, mk.n_experts) if (mk is not None and mk.do_moe) else 1
    if mk is None:
        ml, nx = list(range(DEPTH)), NE
    msl = lambda a: f(np.asarray(a)[ml][:, :nx])
    norms = f(np.concatenate([inp["norm_mix"], inp["norm_ffn"], inp["norm_out"][None, :]], axis=0))
    shared = {
        "cst_ident": np.eye(128, dtype=np.float32),
        "ada_w": f(inp["ada_w"]), "ada_b": f(inp["ada_b"]), "norms": norms,
        "router_w": f(inp["router_w"]), "router_b": f(inp["router_b"]),
        "moe_w_gate": msl(inp["moe_w_gate"]), "moe_w_up": msl(inp["moe_w_up"]), "moe_w_down": msl(inp["moe_w_down"]),
        "moe_b_gate": f(inp["moe_b_gate"]), "moe_b_up": f(inp["moe_b_up"]), "moe_b_down": f(inp["moe_b_down"]),
    }
    if mk is None or mk.do_mixer:
        shared.update(mixer_host_inputs(inp))
    maps = []
    for b in range(ncores):
        m = dict(shared)
        m["x"] = f(inp["x"][b])
        m["ctx"] = f(inp["ctx"][b])
        m["c2"] = f(np.stack([inp["c"][b], inp["c_ctx"]], axis=0))
        maps.append(m)
    return maps


def kernel(**inputs):
    mk = MK()
    nc = mk.build()
    in_maps = make_in_maps(inputs, 8, mk)
    res = run_bass_kernel_spmd(nc, in_maps, core_ids=list(range(8)))
    return np.stack([np.asarray(r["out"], dtype=np.float32) for r in res.results], axis=0)
```

```python
from contextlib import ExitStack
import numpy as np
import concourse.bass as bass
import concourse.mybir as mybir
from concourse.bass_utils import run_bass_kernel_spmd

F32 = mybir.dt.float32
BF16 = mybir.dt.bfloat16
AF = mybir.ActivationFunctionType
ALU = mybir.AluOpType
AX = mybir.AxisListType

ENG = ["pe", "act", "dve", "pool", "sp"]
N_DMA_SEMS = 12

D = 1024
KC = 8
S = 2048
C = 256
T = S + C
DEPTH = 4
NE = 32
EPS = 1e-6
TT = [(0, 512), (512, 512), (1024, 512), (1536, 512), (2048, 256)]
ARENA_WORDS = 53000


class V:
    __slots__ = ("ap", "keys")

    def __init__(self, ap, keys):
        self.ap = ap
        self.keys = keys


class Buf:
    def __init__(self, name, ap, nsub=1):
        self.name = name
        self.ap = ap
        self.nsub = nsub

    def v(self, idx=None, subs=None):
        ap = self.ap[idx] if idx is not None else self.ap
        if subs is None:
            keys = [(self.name, i) for i in range(self.nsub)]
        elif isinstance(subs, int):
            keys = [(self.name, subs)]
        else:
            keys = [(self.name, i) for i in subs]
        return V(ap, keys)


class Op:
    __slots__ = ("fn", "deps", "dma", "signal", "sem", "val", "sigcount")

    def __init__(self, fn, deps, dma):
        self.fn = fn
        self.deps = deps
        self.dma = dma
        self.signal = False
        self.sem = None
        self.val = 0
        self.sigcount = 0


class Prog:
    def __init__(self):
        self.nc = bass.Bass("TRN2", target_bir_lowering=False)
        self.ops = {e: [] for e in ENG}
        self.res_w = {}
        self.res_r = {}
        self.es = ExitStack()
        self.dma_since_barrier = []
        self.nbuf = 0

    def op(self, eng, fn, reads=(), writes=(), dma=False, extra_deps=()):
        deps = {}

        def add(tok):
            e, i = tok
            if self.ops[e][i].dma:
                deps[("dma", e, i)] = tok
            else:
                if e == "pe" and eng == "pe":
                    return
                k = ("eng", e)
                if k not in deps or deps[k][1] < i:
                    deps[k] = tok

        for v in reads:
            for k in v.keys:
                w = self.res_w.get(k)
                if w is not None:
                    add(w)
                if k[0].startswith("ps"):
                    for r in self.res_r.get(k, ()):
                        if r[0] != eng:
                            add(r)
        for v in writes:
            for k in v.keys:
                w = self.res_w.get(k)
                if w is not None:
                    add(w)
                for r in self.res_r.get(k, ()):
                    add(r)
        for tok in extra_deps:
            add(tok)
        idx = len(self.ops[eng])
        tok = (eng, idx)
        self.ops[eng].append(Op(fn, list(deps.values()), dma))
        if dma:
            self.dma_since_barrier.append(tok)
        for v in reads:
            for k in v.keys:
                self.res_r.setdefault(k, []).append(tok)
        for v in writes:
            for k in v.keys:
                self.res_w[k] = tok
                self.res_r[k] = []
        return tok

    def barrier(self):
        last = [(e, len(self.ops[e]) - 1) for e in ENG if self.ops[e]]
        deps = last + list(self.dma_since_barrier)
        self.dma_since_barrier = []
        for e in ENG:
            self.op(e, lambda eng: eng.nop(), extra_deps=deps)
        self.res_w = {}
        self.res_r = {}

    def mm(self, out, lhsT, rhs, start=True, stop=True):
        return self.op("pe", lambda e: e.matmul(out.ap, lhsT.ap, rhs.ap, start=start, stop=stop),
                       reads=[lhsT, rhs], writes=[out])

    def transpose(self, out, in_, ident):
        return self.op("pe", lambda e: e.transpose(out.ap, in_.ap, ident.ap),
                       reads=[in_, ident], writes=[out])

    def act(self, out, in_, func, bias=None, scale=1.0, accum=None):
        reads = [in_]
        kw = {}
        if isinstance(bias, V):
            reads.append(bias)
            kw["bias"] = bias.ap
        elif bias is not None:
            kw["bias"] = float(bias)
        if isinstance(scale, V):
            reads.append(scale)
            kw["scale"] = scale.ap
        else:
            kw["scale"] = float(scale)
        writes = [out]
        if accum is not None:
            writes.append(accum)
            kw["accum_out"] = accum.ap
        return self.op("act", lambda e: e.activation(out.ap, in_.ap, func, **kw), reads=reads, writes=writes)

    def ts(self, eng, out, in0, s1, s2, op0, op1=None, accum=None):
        reads = [in0]
        a1 = s1
        a2 = s2
        if isinstance(s1, V):
            reads.append(s1)
            a1 = s1.ap
        if isinstance(s2, V):
            reads.append(s2)
            a2 = s2.ap
        kw = {}
        if op1 is not None:
            kw["op1"] = op1
        writes = [out]
        if accum is not None:
            writes.append(accum)
            kw["accum_out"] = accum.ap
        return self.op(eng, lambda e: e.tensor_scalar(out.ap, in0.ap, a1, a2, op0, **kw), reads=reads, writes=writes)

    def tt(self, eng, out, in0, in1, op):
        return self.op(eng, lambda e: e.tensor_tensor(out.ap, in0.ap, in1.ap, op), reads=[in0, in1], writes=[out])

    def stt(self, eng, out, in0, scalar, in1, op0, op1):
        reads = [in0, in1]
        a = scalar
        if isinstance(scalar, V):
            reads.append(scalar)
            a = scalar.ap
        return self.op(eng, lambda e: e.scalar_tensor_tensor(out.ap, in0.ap, a, in1.ap, op0, op1),
                       reads=reads, writes=[out])

    def copy(self, eng, out, in_):
        if eng == "act":
            return self.op(eng, lambda e: e.copy(out.ap, in_.ap), reads=[in_], writes=[out])
        return self.op(eng, lambda e: e.tensor_copy(out.ap, in_.ap), reads=[in_], writes=[out])

    def recip(self, out, in_):
        return self.op("dve", lambda e: e.reciprocal(out.ap, in_.ap), reads=[in_], writes=[out])

    def memset(self, eng, out, val):
        return self.op(eng, lambda e: e.memset(out.ap, val), writes=[out])

    def dma(self, eng, out_ap, in_ap, reads=(), writes=(), **kw):
        return self.op(eng, lambda e: e.dma_start(out_ap, in_ap, **kw), reads=reads, writes=writes, dma=True)

    def emit(self):
        nc = self.nc
        es = self.es
        for e in ENG:
            for op in self.ops[e]:
                for (pe_, pi) in op.deps:
                    p = self.ops[pe_][pi]
                    if not p.dma:
                        p.signal = True
        EPOCH = 8000
        eng_sem = {}
        for e in ENG:
            c = 0
            for op in self.ops[e]:
                if op.dma:
                    continue
                if op.signal:
                    c += 1
                op.sigcount = c
            nep = max(1, (c + EPOCH - 1) // EPOCH)
            eng_sem[e] = [es.enter_context(nc.semaphore(f"s_{e}_{i}")) for i in range(nep)]

        def sig_of(e, sigcount):
            ep = (sigcount - 1) // EPOCH
            return eng_sem[e][ep], sigcount - ep * EPOCH

        for e in ENG:
            dl = [op for op in self.ops[e] if op.dma]
            if not dl:
                continue
            sems = [es.enter_context(nc.semaphore(f"d_{e}_{i}")) for i in range(N_DMA_SEMS)]
            uses = [0] * N_DMA_SEMS
            for j, op in enumerate(dl):
                s = j % N_DMA_SEMS
                uses[s] += 1
                op.sem = sems[s]
                op.val = 16 * uses[s]
        self.stats = {e: (len(self.ops[e]), max([o.sigcount for o in self.ops[e]] + [0])) for e in ENG}

        def run(e, eng):
            seen = {}

            def wait(sem, val):
                k = sem.num
                if seen.get(k, 0) < val:
                    eng.wait_ge(sem, val)
                    seen[k] = val

            for op in self.ops[e]:
                for (pe_, pi) in op.deps:
                    p = self.ops[pe_][pi]
                    if p.dma:
                        wait(p.sem, p.val)
                    else:
                        wait(*sig_of(pe_, p.sigcount))
                if op.dma:
                    if op.val > 16:
                        wait(op.sem, op.val - 16)
                    ins = op.fn(eng)
                    ins.then_inc(op.sem, 16)
                else:
                    ins = op.fn(eng)
                    if op.signal:
                        ins.then_inc(sig_of(e, op.sigcount)[0], 1)

        block = es.enter_context(nc.Block())

        @block.tensor
        def _(eng):
            run("pe", eng)

        @block.scalar
        def _(eng):
            run("act", eng)

        @block.vector
        def _(eng):
            run("dve", eng)

        @block.gpsimd
        def _(eng):
            run("pool", eng)

        @block.sync
        def _(eng):
            run("sp", eng)

        es.close()
        return nc


class MK:
    def __init__(self, layers=(0, 1, 2, 3), do_mixer=True, do_moe=True, n_experts=NE):
        self.layers = list(layers)
        self.do_mixer = do_mixer
        self.do_moe = do_moe
        self.n_experts = n_experts
        self.P = Prog()
        P = self.P
        nc = P.nc
        di = lambda n, s: nc.dram_tensor(n, list(s), F32, kind="ExternalInput").ap()
        self.x = di("x", [S, D])
        self.ctx = di("ctx", [C, D])
        self.c2 = di("c2", [2, D])
        self.ada_w = di("ada_w", [DEPTH, D, 6 * D])
        self.ada_b = di("ada_b", [DEPTH, 6 * D])
        self.norms = di("norms", [9, D])
        self.router_w = di("router_w", [DEPTH, D, NE])
        self.router_b = di("router_b", [DEPTH, NE])
        self.moe_layers = [l for l in self.layers] if do_moe else []
        self.lmap = {l: i for i, l in enumerate(self.moe_layers)}
        nl = max(1, len(self.moe_layers))
        nx = max(1, n_experts) if do_moe else 1
        self.w_gate = di("moe_w_gate", [nl, nx, D, D])
        self.w_up = di("moe_w_up", [nl, nx, D, D])
        self.w_down = di("moe_w_down", [nl, nx, D, D])
        self.b_gate = di("moe_b_gate", [DEPTH, NE, D])
        self.b_up = di("moe_b_up", [DEPTH, NE, D])
        self.b_down = di("moe_b_down", [DEPTH, NE, D])
        self.cst_ident = di("cst_ident", [128, 128])
        if do_mixer:
            self.a_w_in = di("a_w_in", [2, D, 1536])
            self.a_w_in_sw = di("a_w_in_sw", [2, D, 1280])
            self.a_w_out = di("a_w_out", [2, D, D])
            self.a_sink = di("a_sink", [2, 16])
            self.ropeA = di("ropeA", [2, 64, T])
            self.maskA = di("maskA", [2, 128, 128])
            self.b_w_in = di("b_w_in", [D, 3072])
            self.b_w_out = di("b_w_out", [D, D])
            self.b_tm = di("b_tm", [16, 16, 64, 64])
            self.c_w_in = di("c_w_in", [D, 416])
            self.c_w_in_sw = di("c_w_in_sw", [D, 96])
            self.c_norms = di("c_norms", [3, 128])
            self.c_w_uq = di("c_w_uq", [256, 1536])
            self.c_w_uq_sw = di("c_w_uq_sw", [256, 1536])
            self.c_w_ukv = di("c_w_ukv", [128, 2048])
            self.c_w_out = di("c_w_out", [D, D])
            self.ropeC = di("ropeC", [2, 96, T])
        self.out = nc.dram_tensor("out", [S, D], F32, kind="ExternalOutput").ap()
        self.gscr = nc.dram_tensor("gscr", [NE, T], F32, kind="Internal").ap()
        self.gscr_buf = Buf("gscr", self.gscr, 1)

        self.arena = P.es.enter_context(nc.sbuf_tensor("arena", [128, ARENA_WORDS], F32))
        self.ps = []
        for i in range(8):
            h = P.es.enter_context(nc.psum_tensor(f"ps{i}", [128, 512], F32))
            self.ps.append(Buf(f"ps{i}", h[:], 4))
        self.top = 0
        self.xT = self.alloc("xT", [128, KC, T], F32, nsub=KC * 5)
        self.hT = self.alloc("hT", [128, KC, T], BF16, nsub=KC * 5)
        self.ident = self.alloc("ident", [128, 128], F32)
        self.ones32 = self.alloc("ones32", [128, 128], F32)
        self.ones16 = self.alloc("ones16", [128, 64], BF16)
        self.modc = self.alloc("modc", [128, DEPTH, 6, KC, 2], F32, nsub=DEPTH)
        self.normc = self.alloc("normc", [128, KC, 9], F32)
        self.gm = self.alloc("gm", [128, 2, KC, 2], F32, nsub=2)
        self.phase_base = self.top

    def alloc(self, name, shape, dtype, nsub=1):
        n = int(np.prod(shape[1:]))
        words = n if dtype == F32 else (n + 1) // 2
        words = (words + 7) // 8 * 8
        lo = self.top
        self.top += words
        assert self.top <= ARENA_WORDS, (name, self.top)
        ap = self.arena[0:shape[0], lo:lo + words]
        if dtype != F32:
            ap = ap.bitcast(dtype)
        ap = ap[:, 0:n]
        if len(shape) == 3:
            ap = ap.rearrange("p (a b) -> p a b", a=shape[1])
        elif len(shape) == 4:
            ap = ap.rearrange("p (a b c) -> p a b c", a=shape[1], b=shape[2])
        elif len(shape) == 5:
            ap = ap.rearrange("p (a b c d) -> p a b c d", a=shape[1], b=shape[2], c=shape[3])
        self.P.nbuf += 1
        return Buf(f"{name}#{self.P.nbuf}", ap, nsub)

    def new_phase(self):
        self.P.barrier()
        self.top = self.phase_base

    def release(self, mark):
        self.P.barrier()
        self.top = mark

    def xsub(self, kc, tt):
        return kc * 5 + tt

    def psv(self, i, cols=slice(0, 512), parts=slice(0, 128), subs=None):
        return self.ps[i].v((parts, cols), subs=subs)

    def consts(self):
        P = self.P
        P.memset("dve", self.ones32.v(), 1.0)
        P.memset("dve", self.ones16.v(), 1.0)
        P.dma("sp", self.ident.ap, self.cst_ident, writes=[self.ident.v()])

    def rows_to_cols(self, dram_rows_ap, nrows, dst_fn, stage, ps_i):
        P = self.P
        P.dma("sp", stage.ap[0:nrows, :], dram_rows_ap, writes=[stage.v()])
        for kc in range(KC):
            pv = self.ps[ps_i].v((slice(0, 128), slice(kc * 32, kc * 32 + nrows)))
            P.transpose(pv, stage.v((slice(0, nrows), slice(kc * 128, (kc + 1) * 128))),
                        self.ident.v((slice(0, nrows), slice(0, nrows))))
            P.copy("dve", dst_fn(kc), pv)

    def load_x(self):
        P = self.P
        st = [self.alloc(f"xst{i}", [128, D], F32) for i in range(2)]
        n = 0
        for ti in range(T // 128):
            tok0 = ti * 128
            tt = min(tok0 // 512, 4)
            s = st[ti % 2]
            src = self.x[tok0:tok0 + 128, :] if tok0 < S else self.ctx[tok0 - S:tok0 - S + 128, :]
            P.dma("sp", s.ap, src, writes=[s.v()])
            for half in range(2):
                pb = self.ps[n % 4]
                n += 1
                for j in range(4):
                    kc = half * 4 + j
                    P.transpose(pb.v((slice(None), slice(j * 128, (j + 1) * 128)), subs=j),
                                s.v((slice(None), slice(kc * 128, (kc + 1) * 128))), self.ident.v())
                dst = self.xT.v((slice(None), slice(half * 4, half * 4 + 4), slice(tok0, tok0 + 128)),
                                subs=[self.xsub(half * 4 + j, tt) for j in range(4)])
                src_v = V(pb.ap.rearrange("p (a b) -> p a b", a=4), pb.v().keys)
                P.copy("dve" if half == 0 else "act", dst, src_v)

    def load_small(self):
        P = self.P
        stage = self.alloc("sm_stage", [128, D], F32)
        self.rows_to_cols(self.norms, 9, lambda kc: self.normc.v((slice(None), kc, slice(0, 9))), stage, 4)
        sc = self.alloc("siluc", [128, KC, 2], F32)
        self.rows_to_cols(self.c2, 2, lambda kc: sc.v((slice(None), kc, slice(0, 2))), stage, 4)
        sig = self.alloc("silu_sig", [128, KC, 2], F32)
        P.act(sig.v(), sc.v(), AF.Sigmoid)
        P.tt("dve", sc.v(), sc.v(), sig.v(), ALU.mult)
        adab = self.alloc("adab", [128, DEPTH, 48], F32)
        stage2 = self.alloc("sm_stage2", [48, 128], F32)
        for l in range(DEPTH):
            P.dma("sp", stage2.ap, self.ada_b[l].rearrange("(j p) -> j p", p=128), writes=[stage2.v()])
            pv = self.ps[5].v((slice(0, 128), slice(0, 48)))
            P.transpose(pv, stage2.v(), self.ident.v((slice(0, 48), slice(0, 48))))
            P.copy("dve", adab.v((slice(None), l, slice(None))), pv)
        wst = [self.alloc(f"adaw{i}", [128, KC, 512], F32) for i in range(2)]
        n = 0
        for l in range(DEPTH):
            for piece in range(12):
                w = wst[n % 2]
                n += 1
                src = self.ada_w[l].rearrange("(kc p) c -> p kc c", p=128)[:, :, piece * 512:(piece + 1) * 512]
                P.dma("sp", w.ap, src, writes=[w.v()])
                pb = self.ps[6 + (n % 2)]
                for q in range(4):
                    for kc in range(KC):
                        P.mm(pb.v((slice(None), slice(q * 2, q * 2 + 2))),
                             w.v((slice(None), kc, slice(q * 128, (q + 1) * 128))),
                             sc.v((slice(None), kc, slice(0, 2))), start=(kc == 0), stop=(kc == KC - 1))
                j, kc0 = divmod(piece * 4, 8)
                for q in range(4):
                    idx = piece * 4 + q
                    P.ts("dve", self.modc.v((slice(None), l, j, kc0 + q, slice(0, 2)), subs=l),
                         pb.v((slice(None), slice(q * 2, q * 2 + 2))),
                         adab.v((slice(None), l, slice(idx, idx + 1))), None, ALU.add)

    def prep_gm(self, l, which):
        P = self.P
        jsc = 1 if which == 0 else 4
        nidx = l if which == 0 else 4 + l
        for typ in range(2):
            P.ts("dve", self.gm.v((slice(None), which, slice(None), typ), subs=which),
                 self.modc.v((slice(None), l, jsc, slice(None), typ), subs=l), 1.0, None, ALU.add)
            P.tt("dve", self.gm.v((slice(None), which, slice(None), typ), subs=which),
                 self.gm.v((slice(None), which, slice(None), typ), subs=which),
                 self.normc.v((slice(None), slice(None), nidx)), ALU.mult)

    def norm_tile(self, tt, gain_fn, shift_fn, out_fn, tmp, ps_i, tmax=None):
        P = self.P
        t0, n = TT[tt]
        if tmax is not None:
            n = min(n, tmax)
        sq, rstd, tmul = tmp
        pb = self.ps[ps_i]
        for kc in range(KC):
            s = sq[kc % 2]
            P.act(s.v((slice(None), slice(0, n))), self.xT.v((slice(None), kc, slice(t0, t0 + n)), subs=self.xsub(kc, tt)),
                  AF.Square)
            P.mm(pb.v((slice(None), slice(0, n))), self.ones32.v(), s.v((slice(None), slice(0, n))),
                 start=(kc == 0), stop=(kc == KC - 1))
        P.ts("dve", rstd.v((slice(None), slice(0, n))), pb.v((slice(None), slice(0, n))), 1.0 / D, EPS, ALU.mult, ALU.add)
        P.act(rstd.v((slice(None), slice(0, n))), rstd.v((slice(None), slice(0, n))), AF.Sqrt)
        P.recip(rstd.v((slice(None), slice(0, n))), rstd.v((slice(None), slice(0, n))))
        for kc in range(KC):
            tm = tmul[kc % 2]
            P.tt("dve", tm.v((slice(None), slice(0, n))),
                 self.xT.v((slice(None), kc, slice(t0, t0 + n)), subs=self.xsub(kc, tt)),
                 rstd.v((slice(None), slice(0, n))), ALU.mult)
            sh = shift_fn(kc)
            outs = out_fn(kc)
            first = outs[0]
            P.act(first, tm.v((slice(None), slice(0, n))), AF.Identity, bias=sh if sh is not None else 0.0,
                  scale=gain_fn(kc))
            for o in outs[1:]:
                P.copy("pool", o, first)

    def ffn_phase(self, l, last):
        P = self.P
        ntt = 4 if last else 5
        Tl = S if last else T
        self.new_phase()
        self.prep_gm(l, 1)
        h32 = self.alloc("h32", [128, KC, 512], F32, nsub=KC)
        sq = [self.alloc(f"sq{i}", [128, 512], F32) for i in range(2)]
        rstd = self.alloc("rstd", [128, 512], F32)
        tmul = [self.alloc(f"tmul{i}", [128, 512], F32) for i in range(2)]
        rw = self.alloc("rw", [128, KC, NE], F32)
        rb = self.alloc("rb", [128, NE], F32)
        gatesT = self.alloc("gatesT", [NE, T], F32, nsub=5)
        lg = [self.alloc(f"lg{i}", [128, NE], F32) for i in range(2)]
        ex = [self.alloc(f"ex{i}", [128, NE], F32) for i in range(2)]
        mk = [self.alloc(f"mk{i}", [128, NE], F32) for i in range(2)]
        top8 = [self.alloc(f"top8{i}", [128, 8], F32) for i in range(2)]
        sm = [self.alloc(f"sm{i}", [128, 4], F32) for i in range(2)]
        P.dma("sp", rw.ap, self.router_w[l].rearrange("(kc p) e -> p kc e", p=128), writes=[rw.v()])
        P.dma("sp", rb.ap, self.router_b[l:l + 1, :].to_broadcast([128, NE]), writes=[rb.v()])
        it = 0
        for tt in range(ntt):
            t0, n = TT[tt]
            typ = 0 if tt < 4 else 1
            self.norm_tile(
                tt,
                gain_fn=lambda kc: self.gm.v((slice(None), 1, kc, slice(typ, typ + 1)), subs=1),
                shift_fn=lambda kc: self.modc.v((slice(None), l, 3, kc, slice(typ, typ + 1)), subs=l),
                out_fn=lambda kc: [h32.v((slice(None), kc, slice(0, n)), subs=kc),
                                   self.hT.v((slice(None), kc, slice(t0, t0 + n)), subs=self.xsub(kc, tt))],
                tmp=(sq, rstd, tmul), ps_i=0 + (tt % 2))
            for sub in range(n // 128):
                i2 = it % 2
                it += 1
                pl = self.ps[2 + i2]
                for kc in range(KC):
                    P.mm(pl.v((slice(None), slice(0, NE)), subs=0),
                         h32.v((slice(None), kc, slice(sub * 128, (sub + 1) * 128)), subs=kc),
                         rw.v((slice(None), kc, slice(None))), start=(kc == 0), stop=(kc == KC - 1))
                L = lg[i2]
                P.tt("dve", L.v(), pl.v((slice(None), slice(0, NE)), subs=0), rb.v(), ALU.add)
                P.op("dve", lambda e, o=top8[i2], i=L: e.max(o.ap, i.ap), reads=[L.v()], writes=[top8[i2].v()])
                P.ts("dve", mk[i2].v(), L.v(), top8[i2].v((slice(None), slice(3, 4))), None, ALU.is_ge)
                P.ts("dve", sm[i2].v((slice(None), slice(0, 1))), top8[i2].v((slice(None), slice(0, 1))), -1.0, None, ALU.mult)
                P.act(ex[i2].v(), L.v(), AF.Exp, bias=sm[i2].v((slice(None), slice(0, 1))))
                P.tt("dve", ex[i2].v(), ex[i2].v(), mk[i2].v(), ALU.mult)
                P.op("dve", lambda e, o=sm[i2], i=ex[i2]: e.reduce_sum(o.ap[:, 1:2], i.ap, axis=AX.X),
                     reads=[ex[i2].v()], writes=[sm[i2].v()])
                P.recip(sm[i2].v((slice(None), slice(2, 3))), sm[i2].v((slice(None), slice(1, 2))))
                P.ts("dve", ex[i2].v(), ex[i2].v(), sm[i2].v((slice(None), slice(2, 3))), None, ALU.mult)
                pg = self.ps[4 + i2]
                P.transpose(pg.v((slice(0, NE), slice(0, 128)), subs=0), ex[i2].v(), self.ident.v())
                tok = t0 + sub * 128
                P.copy("act", gatesT.v((slice(None), slice(tok, tok + 128)), subs=tt),
                       pg.v((slice(0, NE), slice(0, 128)), subs=0))
        P.dma("sp", self.gscr[:, 0:Tl], gatesT.ap[:, 0:Tl], reads=[gatesT.v()], writes=[self.gscr_buf.v()])

        self.new_phase()
        bgc = self.alloc("bgc", [128, KC, NE], F32)
        buc = self.alloc("buc", [128, KC, NE], F32)
        mark = self.top
        bstage = self.alloc("bstage", [NE, D], F32)
        bd = self.alloc("bd", [NE, D], F32)
        gT32 = self.alloc("gT32", [NE, T], F32)
        self.rows_to_cols(self.b_gate[l], NE, lambda kc: bgc.v((slice(None), kc, slice(None))), bstage, 7)
        self.rows_to_cols(self.b_up[l], NE, lambda kc: buc.v((slice(None), kc, slice(None))), bstage, 7)
        P.dma("sp", bd.ap, self.b_down[l], writes=[bd.v()])
        P.dma("sp", gT32.ap[:, 0:Tl], self.gscr[:, 0:Tl], reads=[self.gscr_buf.v()], writes=[gT32.v()])
        g2 = lambda kc, typ: self.modc.v((slice(None), l, 5, kc, slice(typ, typ + 1)), subs=l)
        for tt in range(ntt):
            t0, n = TT[tt]
            typ = 0 if tt < 4 else 1
            for dc in range(KC):
                pb = self.ps[dc % 2]
                P.mm(pb.v((slice(None), slice(0, n))), bd.v((slice(None), slice(dc * 128, (dc + 1) * 128))),
                     gT32.v((slice(None), slice(t0, t0 + n))))
                xv = self.xT.v((slice(None), dc, slice(t0, t0 + n)), subs=self.xsub(dc, tt))
                P.stt("dve", xv, pb.v((slice(None), slice(0, n))), g2(dc, typ), xv, ALU.mult, ALU.add)
        self.release(mark)
        NU = 2
        ALPHA = 1.702
        C7 = float(ALPHA * 7.0 / (1.0 + np.exp(-7.0 * ALPHA)))
        wg = [self.alloc(f"wg{i}", [128, KC, 512], BF16) for i in range(NU)]
        wu = [self.alloc(f"wu{i}", [128, KC, 512], BF16) for i in range(NU)]
        wd = [self.alloc(f"wd{i}", [128, 4, D], BF16) for i in range(NU)]
        G = [self.alloc(f"G{i}", [128, T], BF16) for i in range(2)]
        aT = [self.alloc(f"aT{i}", [128, 4, 512], BF16, nsub=4) for i in range(2)]
        silb = [self.alloc(f"silb{i}", [128, 512], F32) for i in range(2)]
        uvb = [self.alloc(f"uvb{i}", [128, 512], F32) for i in range(2)]
        g2a = self.alloc("g2a", [128, KC, 2], F32)
        P.ts("dve", bgc.v(), bgc.v(), ALPHA, None, ALU.mult)
        P.ts("dve", buc.v(), buc.v(), 1.0, None, ALU.add)
        P.ts("dve", g2a.v(), self.modc.v((slice(None), l, 5, slice(None), slice(None)), subs=l), 1.0 / ALPHA, None, ALU.mult)
        items = [(e, half, tt) for e in range(self.n_experts) for half in range(2) for tt in range(ntt)]
        st_ = {"tn": 0}

        def gu(i):
            e, half, tt = items[i]
            u = (e * 2 + half) % NU
            Ge = G[e % 2]
            if tt == 0:
                if half == 0:
                    P.dma("pool", Ge.ap[:, 0:Tl], self.gscr[e:e + 1, 0:Tl].to_broadcast([128, Tl]),
                          reads=[self.gscr_buf.v()], writes=[Ge.v()])
                fsl = slice(half * 512, (half + 1) * 512)
                P.dma("pool", wg[u].ap, self.w_gate[self.lmap[l], e].rearrange("(kc p) f -> p kc f", p=128)[:, :, fsl],
                      writes=[wg[u].v()])
                P.dma("pool", wu[u].ap, self.w_up[self.lmap[l], e].rearrange("(kc p) f -> p kc f", p=128)[:, :, fsl],
                      writes=[wu[u].v()])
                P.dma("pool", wd[u].ap, self.w_down[self.lmap[l], e].rearrange("(fc p) d -> p fc d", p=128)[:, half * 4:half * 4 + 4, :],
                      writes=[wd[u].v()])
            t0, n = TT[tt]
            A = aT[i % 2]
            sn = slice(0, n)
            for fc in range(4):
                fcg = half * 4 + fc
                tn = st_["tn"]
                st_["tn"] += 1
                pg_ = self.ps[2 + (tn % 2)]
                pu_ = self.ps[4 + (tn % 2)]
                sil, uvt = silb[tn % 2], uvb[tn % 2]
                for kc in range(KC):
                    P.mm(pg_.v((slice(None), sn)), wg[u].v((slice(None), kc, slice(fc * 128, (fc + 1) * 128))),
                         self.hT.v((slice(None), kc, slice(t0, t0 + n)), subs=self.xsub(kc, tt)),
                         start=(kc == 0), stop=(kc == KC - 1))
                for kc in range(KC):
                    P.mm(pu_.v((slice(None), sn)), wu[u].v((slice(None), kc, slice(fc * 128, (fc + 1) * 128))),
                         self.hT.v((slice(None), kc, slice(t0, t0 + n)), subs=self.xsub(kc, tt)),
                         start=(kc == 0), stop=(kc == KC - 1))
                sv = sil.v((slice(None), sn))
                uv = uvt.v((slice(None), sn))
                P.act(sv, pg_.v((slice(None), sn)), AF.Silu, bias=bgc.v((slice(None), fcg, slice(e, e + 1))), scale=ALPHA)
                P.ts("dve", uv, pu_.v((slice(None), sn)), buc.v((slice(None), fcg, slice(e, e + 1))), -6.0, ALU.add, ALU.max)
                P.stt("dve", uv, uv, 8.0, Ge.v((slice(None), slice(t0, t0 + n))), ALU.min, ALU.mult)
                P.stt("dve", A.v((slice(None), fc, sn), subs=fc), sv, C7, uv, ALU.min, ALU.mult)

        def down(i):
            e, half, tt = items[i]
            u = (e * 2 + half) % NU
            t0, n = TT[tt]
            typ = 0 if tt < 4 else 1
            A = aT[i % 2]
            sn = slice(0, n)
            for dc in range(KC):
                py = self.ps[dc % 2]
                for fc in range(4):
                    P.mm(py.v((slice(None), sn)), wd[u].v((slice(None), fc, slice(dc * 128, (dc + 1) * 128))),
                         A.v((slice(None), fc, sn), subs=fc), start=(fc == 0), stop=(fc == 3))
                xv = self.xT.v((slice(None), dc, slice(t0, t0 + n)), subs=self.xsub(dc, tt))
                P.stt("dve", xv, py.v((slice(None), sn)), g2a.v((slice(None), dc, slice(typ, typ + 1))), xv, ALU.mult, ALU.add)

        if items:
            gu(0)
        for i in range(len(items)):
            if i + 1 < len(items):
                gu(i + 1)
            down(i)

    def norm1(self, l):
        P = self.P
        self.new_phase()
        self.prep_gm(l, 0)
        sq = [self.alloc(f"n1sq{i}", [128, 512], F32) for i in range(2)]
        rstd = self.alloc("n1rstd", [128, 512], F32)
        tmul = [self.alloc(f"n1tmul{i}", [128, 512], F32) for i in range(2)]
        for tt in range(5):
            t0, n = TT[tt]
            typ = 0 if tt < 4 else 1
            self.norm_tile(
                tt,
                gain_fn=lambda kc: self.gm.v((slice(None), 0, kc, slice(typ, typ + 1)), subs=0),
                shift_fn=lambda kc: self.modc.v((slice(None), l, 0, kc, slice(typ, typ + 1)), subs=l),
                out_fn=lambda kc: [self.hT.v((slice(None), kc, slice(t0, t0 + n)), subs=self.xsub(kc, tt))],
                tmp=(sq, rstd, tmul), ps_i=0 + (tt % 2))

    def hTv(self, kc, tt, lo=None, n=None):
        t0, nn = TT[tt]
        if lo is None:
            lo, n = t0, nn
        return self.hT.v((slice(None), kc, slice(lo, lo + n)), subs=self.xsub(kc, tt))

    def attn_setup(self):
        self.Pt = [self.alloc(f"Pt{i}", [128, 512], BF16) for i in range(2)]
        self.St = [self.alloc(f"St{i}", [128, 512], F32) for i in range(2)]
        self.rd = [self.alloc(f"rd{i}", [64, 512], F32) for i in range(2)]
        self.a_n = 0
        self.s_n = 0

    def attend(self, qparts, chunks, QW, out_v, sink_v, scale):
        P = self.P
        G = 512 // QW
        ob = self.ps[2 + self.a_n % 2]
        db = self.ps[4 + self.a_n % 2]
        rd = self.rd[self.a_n % 2]
        self.a_n += 1
        ng = (len(chunks) + G - 1) // G
        sl64 = slice(0, 64)
        for gi in range(ng):
            cs = chunks[gi * G:(gi + 1) * G]
            sb = self.ps[self.s_n % 2]
            pt = self.Pt[self.s_n % 2]
            st = self.St[self.s_n % 2]
            self.s_n += 1
            for j, ch in enumerate(cs):
                cols = slice(j * QW, (j + 1) * QW)
                for pi, (kv, qv) in enumerate(zip(ch["k"], qparts)):
                    P.mm(sb.v((slice(None), cols)), kv, qv, start=(pi == 0), stop=(pi == len(qparts) - 1))
            j = 0
            while j < len(cs):
                if cs[j]["mask"] is None:
                    j2 = j
                    while j2 < len(cs) and cs[j2]["mask"] is None:
                        j2 += 1
                    cols = slice(j * QW, j2 * QW)
                    P.act(pt.v((slice(None), cols)), sb.v((slice(None), cols)), AF.Exp, scale=scale)
                    j = j2
                else:
                    cols = slice(j * QW, (j + 1) * QW)
                    P.stt("dve", st.v((slice(None), cols)), sb.v((slice(None), cols)), scale, cs[j]["mask"],
                          ALU.mult, ALU.add)
                    P.act(pt.v((slice(None), cols)), st.v((slice(None), cols)), AF.Exp)
                    j += 1
            for j, ch in enumerate(cs):
                cols = slice(j * QW, (j + 1) * QW)
                first = (gi == 0 and j == 0)
                lastc = (gi == ng - 1 and j == len(cs) - 1)
                P.mm(ob.v((sl64, slice(0, QW))), ch["v"], pt.v((slice(None), cols)), start=first, stop=lastc)
                P.mm(db.v((sl64, slice(0, QW))), self.ones16.v(), pt.v((slice(None), cols)), start=first, stop=lastc)
        rv = rd.v((sl64, slice(0, QW)))
        if sink_v is not None:
            P.ts("dve", rv, db.v((sl64, slice(0, QW))), sink_v, None, ALU.add)
            P.recip(rv, rv)
        else:
            P.recip(rv, db.v((sl64, slice(0, QW))))
        P.tt("dve", out_v, ob.v((sl64, slice(0, QW))), rv, ALU.mult)

    def out_proj(self, l, wo, OTb, nh, ntt):
        P = self.P
        for tt in range(ntt):
            t0, n = TT[tt]
            typ = 0 if tt < 4 else 1
            for dc in range(KC):
                pb = self.ps[6 + dc % 2]
                for hh in range(nh):
                    P.mm(pb.v((slice(None), slice(0, n))), wo.v((slice(None), hh, slice(dc * 128, (dc + 1) * 128))),
                         OTb.v((slice(None), hh, slice(t0, t0 + n)), subs=hh), start=(hh == 0), stop=(hh == nh - 1))
                xv = self.xT.v((slice(None), dc, slice(t0, t0 + n)), subs=self.xsub(dc, tt))
                P.stt("dve", xv, pb.v((slice(None), slice(0, n))),
                      self.modc.v((slice(None), l, 2, dc, slice(typ, typ + 1)), subs=l), xv, ALU.mult, ALU.add)

    def rope_proj(self, dst_v, wA_fn, wB_fn, rhs_fn, nkc, n, t0, cosT, sinT, prow, M, rt):
        P = self.P
        pa, pb = self.ps[6], self.ps[7]
        for kc in range(nkc):
            P.mm(pa.v((slice(0, M), slice(0, n))), wA_fn(kc), rhs_fn(kc), start=(kc == 0), stop=(kc == nkc - 1))
        for kc in range(nkc):
            P.mm(pb.v((slice(0, M), slice(0, n))), wB_fn(kc), rhs_fn(kc), start=(kc == 0), stop=(kc == nkc - 1))
        t1, t2 = rt
        P.tt("dve", t1.v((prow, slice(0, n))), pa.v((prow, slice(0, n))), cosT.v((prow, slice(t0, t0 + n))), ALU.mult)
        P.tt("dve", t2.v((prow, slice(0, n))), pb.v((prow, slice(0, n))), sinT.v((prow, slice(t0, t0 + n))), ALU.mult)
        P.tt("pool", dst_v, t1.v((prow, slice(0, n))), t2.v((prow, slice(0, n))), ALU.add)
        return pa

    def mixer_a(self, l, last):
        P = self.P
        slot = l // 3
        self.new_phase()
        nttq = 4 if last else 5
        scale = 64 ** -0.5
        cosT = self.alloc("cosA", [64, T], F32)
        sinT = self.alloc("sinA", [64, T], F32)
        P.dma("sp", cosT.ap, self.ropeA[0], writes=[cosT.v()])
        P.dma("sp", sinT.ap, self.ropeA[1], writes=[sinT.v()])
        mprev = self.alloc("mprev", [128, 128], F32)
        mnext = self.alloc("mnext", [128, 128], F32)
        P.dma("sp", mprev.ap, self.maskA[0], writes=[mprev.v()])
        P.dma("sp", mnext.ap, self.maskA[1], writes=[mnext.v()])
        sinkx = self.alloc("sinkx", [64, 16], F32)
        P.dma("sp", sinkx.ap, self.a_sink[slot:slot + 1, :].to_broadcast([64, 16]), writes=[sinkx.v()])
        P.act(sinkx.v(), sinkx.v(), AF.Exp)
        self.attn_setup()
        KTb = self.alloc("KT", [64, T], BF16)
        Vb = self.alloc("V", [128, 18, 64], BF16)
        QTb = self.alloc("QT", [64, 2, T], BF16, nsub=2)
        OTb = self.alloc("OT", [64, 2, T], BF16, nsub=2)
        wk = self.alloc("wk", [128, KC, 128], BF16)
        wv = self.alloc("wv", [128, KC, 64], BF16)
        wq = self.alloc("wq", [128, KC, 256], BF16)
        wo = self.alloc("wo", [64, 2, D], BF16)
        rt = [self.alloc(f"rt{i}", [64, 512], F32) for i in range(2)]
        win = self.a_w_in[slot].rearrange("(kc p) c -> p kc c", p=128)
        wsw = self.a_w_in_sw[slot].rearrange("(kc p) c -> p kc c", p=128)
        sl64 = slice(0, 64)
        for g in range(4):
            kc0 = 1024 + g * 64
            P.dma("pool", wk.ap[:, :, 0:64], win[:, :, kc0:kc0 + 64], writes=[wk.v()])
            P.dma("pool", wk.ap[:, :, 64:128], wsw[:, :, kc0:kc0 + 64], writes=[wk.v()])
            P.dma("pool", wv.ap, win[:, :, 1280 + g * 64:1280 + (g + 1) * 64], writes=[wv.v()])
            for tt in range(5):
                t0, n = TT[tt]
                self.rope_proj(KTb.v((sl64, slice(t0, t0 + n))),
                               lambda kc: wk.v((slice(None), kc, slice(0, 64))),
                               lambda kc: wk.v((slice(None), kc, slice(64, 128))),
                               lambda kc: self.hTv(kc, tt), KC, n, t0, cosT, sinT, sl64, 64, rt)
            for tci in range(18):
                tt = min(tci // 4, 4)
                pb = self.ps[6 + tci % 2]
                for kc in range(KC):
                    P.mm(pb.v((slice(None), slice(0, 64))), self.hTv(kc, tt, tci * 128, 128), wv.v((slice(None), kc, slice(None))),
                         start=(kc == 0), stop=(kc == KC - 1))
                P.copy("act", Vb.v((slice(None), tci, slice(None))), pb.v((slice(None), slice(0, 64))))
            kch = lambda ci, m: dict(k=[KTb.v((sl64, slice(ci * 128, (ci + 1) * 128)))],
                                     v=Vb.v((slice(None), ci, slice(None))), mask=m)
            for half in range(2):
                h0 = g * 4 + half * 2
                P.dma("pool", wq.ap[:, :, 0:128], win[:, :, h0 * 64:(h0 + 2) * 64], writes=[wq.v()])
                P.dma("pool", wq.ap[:, :, 128:256], wsw[:, :, h0 * 64:(h0 + 2) * 64], writes=[wq.v()])
                P.dma("pool", wo.ap, self.a_w_out[slot][h0 * 64:(h0 + 2) * 64, :].rearrange("(h d) n -> d h n", d=64),
                      writes=[wo.v()])
                for hh in range(2):
                    for tt in range(nttq):
                        t0, n = TT[tt]
                        self.rope_proj(QTb.v((sl64, hh, slice(t0, t0 + n)), subs=hh),
                                       lambda kc: wq.v((slice(None), kc, slice(hh * 64, (hh + 1) * 64))),
                                       lambda kc: wq.v((slice(None), kc, slice(128 + hh * 64, 128 + (hh + 1) * 64))),
                                       lambda kc: self.hTv(kc, tt), KC, n, t0, cosT, sinT, sl64, 64, rt)
                for hh in range(2):
                    h = h0 + hh
                    sv = sinkx.v((slice(None), slice(h, h + 1)))
                    for qb in range(16):
                        chunks = [kch(qb, None), kch(16, None), kch(17, None)]
                        if qb > 0:
                            chunks.append(kch(qb - 1, mprev.v()))
                        if qb < 15:
                            chunks.append(kch(qb + 1, mnext.v()))
                        qs = slice(qb * 128, (qb + 1) * 128)
                        self.attend([QTb.v((sl64, hh, qs), subs=hh)], chunks, 128, OTb.v((sl64, hh, qs), subs=hh), sv, scale)
                    if not last:
                        for qb in (16, 17):
                            qs = slice(qb * 128, (qb + 1) * 128)
                            self.attend([QTb.v((sl64, hh, qs), subs=hh)], [kch(16, None), kch(17, None)], 128,
                                        OTb.v((sl64, hh, qs), subs=hh), sv, scale)
                self.out_proj(l, wo, OTb, 2, nttq)

    def mixer_b(self, l):
        P = self.P
        self.new_phase()
        scale = 64 ** -0.5
        NEG = -30000.0
        self.attn_setup()
        QTb = self.alloc("QT", [64, 2, T], BF16, nsub=2)
        KTb = self.alloc("KT", [64, 2, T], BF16, nsub=2)
        Vb = self.alloc("V", [128, 18, 128], BF16)
        OTb = self.alloc("OT", [64, 2, T], BF16, nsub=2)
        wqkv = self.alloc("wqkv", [128, KC, 384], BF16)
        wo = self.alloc("wo", [64, 2, D], BF16)
        bm = [[self.alloc(f"bm{a_}_{b_}", [128, 128], F32) for b_ in range(5)] for a_ in range(5)]
        win = self.b_w_in.rearrange("(kc p) c -> p kc c", p=128)
        sl64 = slice(0, 64)
        reps = [0, 1, 2, 14, 15]
        for hp in range(8):
            h0 = hp * 2
            for j in range(3):
                P.dma("pool", wqkv.ap[:, :, j * 128:(j + 1) * 128], win[:, :, j * 1024 + h0 * 64:j * 1024 + (h0 + 2) * 64],
                      writes=[wqkv.v()])
            P.dma("pool", wo.ap, self.b_w_out[h0 * 64:(h0 + 2) * 64, :].rearrange("(h d) n -> d h n", d=64), writes=[wo.v()])
            n_ = 0
            for hh in range(2):
                for j, dstb in ((0, QTb), (1, KTb)):
                    for tt in range(5):
                        t0, n = TT[tt]
                        pb = self.ps[6 + n_ % 2]
                        n_ += 1
                        for kc in range(KC):
                            P.mm(pb.v((sl64, slice(0, n))), wqkv.v((slice(None), kc, slice(j * 128 + hh * 64, j * 128 + (hh + 1) * 64))),
                                 self.hTv(kc, tt), start=(kc == 0), stop=(kc == KC - 1))
                        P.copy("act", dstb.v((sl64, hh, slice(t0, t0 + n)), subs=hh), pb.v((sl64, slice(0, n))))
            for tci in range(18):
                tt = min(tci // 4, 4)
                pb = self.ps[6 + tci % 2]
                for kc in range(KC):
                    P.mm(pb.v((slice(None), slice(0, 128))), self.hTv(kc, tt, tci * 128, 128), wqkv.v((slice(None), kc, slice(256, 384))),
                         start=(kc == 0), stop=(kc == KC - 1))
                P.copy("act", Vb.v((slice(None), tci, slice(None))), pb.v((slice(None), slice(0, 128))))
            for hh in range(2):
                h = h0 + hh
                import os
                for cls, rp in enumerate(reps):
                    cs = min(max(2 * rp - 4, 0), 22)
                    if os.environ.get("B_MODE", "dma") in ("nolocal", "nomask", "nofill"):
                        break
                    for c in range(5):
                        for a in range(2):
                            for b in range(2):
                                kr = cs + 2 * c + a
                                qr = 2 * rp + b
                                r0 = min(max(qr - 4, 0), 24)
                                dst = bm[cls][c].v((slice(a * 64, (a + 1) * 64), slice(b * 64, (b + 1) * 64)))
                                dr = (kr - qr + 7) if (r0 <= kr < r0 + 8) else 15
                                P.dma("sp", dst.ap, self.b_tm[h, dr], writes=[dst])
                kch = lambda ci, m: dict(k=[KTb.v((sl64, hh, slice(ci * 128, (ci + 1) * 128)), subs=hh)],
                                         v=Vb.v((slice(None), ci, slice(hh * 64, (hh + 1) * 64))), mask=m)
                for rp in range(16):
                    cls = {0: 0, 1: 1, 14: 3, 15: 4}.get(rp, 2)
                    cs = min(max(2 * rp - 4, 0), 22)
                    chunks = [kch(16, None), kch(17, None)]
                    import os
                    for c in range(5):
                        if os.environ.get("B_MODE", "dma") == "nolocal":
                            break
                        if os.environ.get("B_MODE", "dma") == "nomask":
                            chunks.append(kch(cs // 2 + c, None))
                            continue
                        chunks.append(kch(cs // 2 + c, bm[cls][c].v()))
                    qs = slice(rp * 128, (rp + 1) * 128)
                    self.attend([QTb.v((sl64, hh, qs), subs=hh)], chunks, 128, OTb.v((sl64, hh, qs), subs=hh), None, scale)
                for qb in (16, 17):
                    qs = slice(qb * 128, (qb + 1) * 128)
                    self.attend([QTb.v((sl64, hh, qs), subs=hh)], [kch(16, None), kch(17, None)], 128,
                                OTb.v((sl64, hh, qs), subs=hh), None, scale)
            self.out_proj(l, wo, OTb, 2, 5)

    def mixer_c(self, l):
        P = self.P
        self.new_phase()
        scale = 96 ** -0.5
        cosT = self.alloc("cosC", [96, T], F32)
        sinT = self.alloc("sinC", [96, T], F32)
        P.dma("sp", cosT.ap, self.ropeC[0], writes=[cosT.v()])
        P.dma("sp", sinT.ap, self.ropeC[1], writes=[sinT.v()])
        cqT = self.alloc("cqT", [128, 2, T], BF16)
        ckvT = self.alloc("ckvT", [128, T], BF16)
        krT = self.alloc("krT", [96, T], BF16)
        ncol = self.alloc("cnorm", [128, 3], F32)
        rt = [self.alloc(f"rt{i}", [96, 512], F32) for i in range(2)]
        mark = self.top
        w1 = self.alloc("w1", [128, KC, 416], BF16)
        w1s = self.alloc("w1s", [128, KC, 96], BF16)
        P.dma("pool", w1.ap, self.c_w_in.rearrange("(kc p) c -> p kc c", p=128), writes=[w1.v()])
        P.dma("pool", w1s.ap, self.c_w_in_sw.rearrange("(kc p) c -> p kc c", p=128), writes=[w1s.v()])
        nst = self.alloc("nst", [3, 128], F32)
        P.dma("sp", nst.ap, self.c_norms, writes=[nst.v()])
        pv = self.ps[5].v((slice(None), slice(0, 3)))
        P.transpose(pv, nst.v(), self.ident.v((slice(0, 3), slice(0, 3))))
        P.copy("dve", ncol.v(), pv)
        c32 = self.alloc("c32", [128, 3, 512], F32, nsub=3)
        sq = [self.alloc(f"csq{i}", [128, 512], F32) for i in range(2)]
        rs = [self.alloc(f"crs{i}", [128, 512], F32) for i in range(2)]
        r96 = slice(64, 96)
        for tt in range(5):
            t0, n = TT[tt]
            sn = slice(0, n)
            for s3 in range(3):
                pb = self.ps[6 + s3 % 2]
                for kc in range(KC):
                    P.mm(pb.v((slice(None), sn)), w1.v((slice(None), kc, slice(s3 * 128, (s3 + 1) * 128))), self.hTv(kc, tt),
                         start=(kc == 0), stop=(kc == KC - 1))
                P.copy("act", c32.v((slice(None), s3, sn), subs=s3), pb.v((slice(None), sn)))
            for which, slots, nf in ((0, (0, 1), 256.0), (1, (2,), 128.0)):
                pr = self.ps[0 + which]
                for i, s3 in enumerate(slots):
                    P.act(sq[i % 2].v((slice(None), sn)), c32.v((slice(None), s3, sn), subs=s3), AF.Square)
                    P.mm(pr.v((slice(None), sn)), self.ones32.v(), sq[i % 2].v((slice(None), sn)),
                         start=(i == 0), stop=(i == len(slots) - 1))
                r = rs[which]
                P.ts("dve", r.v((slice(None), sn)), pr.v((slice(None), sn)), 1.0 / nf, EPS, ALU.mult, ALU.add)
                P.act(r.v((slice(None), sn)), r.v((slice(None), sn)), AF.Sqrt)
                P.recip(r.v((slice(None), sn)), r.v((slice(None), sn)))
                for s3 in slots:
                    P.tt("dve", c32.v((slice(None), s3, sn), subs=s3), c32.v((slice(None), s3, sn), subs=s3),
                         r.v((slice(None), sn)), ALU.mult)
                    dst = cqT.v((slice(None), s3, slice(t0, t0 + n))) if which == 0 else ckvT.v((slice(None), slice(t0, t0 + n)))
                    P.act(dst, c32.v((slice(None), s3, sn), subs=s3), AF.Identity, scale=ncol.v((slice(None), slice(s3, s3 + 1))))
            self.rope_proj(krT.v((r96, slice(t0, t0 + n))),
                           lambda kc: w1.v((slice(None), kc, slice(320, 416))),
                           lambda kc: w1s.v((slice(None), kc, slice(0, 96))),
                           lambda kc: self.hTv(kc, tt), KC, n, t0, cosT, sinT, r96, 96, rt)
        self.release(mark)
        self.attn_setup()
        q96 = self.alloc("q96", [96, 2, T], BF16, nsub=2)
        k96 = self.alloc("k96", [96, 2, T], BF16, nsub=2)
        Vb = self.alloc("V", [128, 18, 128], BF16)
        OTb = self.alloc("OT", [64, 2, T], BF16, nsub=2)
        wuq = self.alloc("wuq", [128, 2, 192], BF16)
        wuqs = self.alloc("wuqs", [128, 2, 192], BF16)
        wukv = self.alloc("wukv", [128, 256], BF16)
        wo = self.alloc("wo", [64, 2, D], BF16)
        uq = self.c_w_uq.rearrange("(kc p) c -> p kc c", p=128)
        uqs = self.c_w_uq_sw.rearrange("(kc p) c -> p kc c", p=128)
        sl64 = slice(0, 64)
        for hp in range(8):
            h0 = hp * 2
            P.dma("pool", wuq.ap, uq[:, :, h0 * 96:(h0 + 2) * 96], writes=[wuq.v()])
            P.dma("pool", wuqs.ap, uqs[:, :, h0 * 96:(h0 + 2) * 96], writes=[wuqs.v()])
            P.dma("pool", wukv.ap, self.c_w_ukv[:, h0 * 128:(h0 + 2) * 128], writes=[wukv.v()])
            P.dma("pool", wo.ap, self.c_w_out[h0 * 64:(h0 + 2) * 64, :].rearrange("(h d) n -> d h n", d=64), writes=[wo.v()])
            for hh in range(2):
                for tt in range(5):
                    t0, n = TT[tt]
                    pa = self.rope_proj(q96.v((r96, hh, slice(t0, t0 + n)), subs=hh),
                                        lambda kc: wuq.v((slice(None), kc, slice(hh * 96, (hh + 1) * 96))),
                                        lambda kc: wuqs.v((slice(None), kc, slice(hh * 96, (hh + 1) * 96))),
                                        lambda kc: cqT.v((slice(None), kc, slice(t0, t0 + n))), 2, n, t0, cosT, sinT, r96, 96, rt)
                    P.copy("act", q96.v((sl64, hh, slice(t0, t0 + n)), subs=hh), pa.v((sl64, slice(0, n))))
                    pk = self.ps[6 + 0]
                    P.mm(pk.v((sl64, slice(0, n))), wukv.v((slice(None), slice(hh * 128, hh * 128 + 64))),
                         ckvT.v((slice(None), slice(t0, t0 + n))))
                    P.copy("act", k96.v((sl64, hh, slice(t0, t0 + n)), subs=hh), pk.v((sl64, slice(0, n))))
                P.copy("pool", k96.v((r96, hh, slice(None)), subs=hh), krT.v((r96, slice(None))))
            for tci in range(18):
                pb = self.ps[6 + tci % 2]
                for hh in range(2):
                    P.mm(pb.v((slice(None), slice(hh * 64, (hh + 1) * 64))), ckvT.v((slice(None), slice(tci * 128, (tci + 1) * 128))),
                         wukv.v((slice(None), slice(hh * 128 + 64, (hh + 1) * 128))))
                P.copy("act", Vb.v((slice(None), tci, slice(None))), pb.v((slice(None), slice(0, 128))))
            r0_96 = slice(0, 96)
            for hh in range(2):
                kch = lambda ci: dict(k=[k96.v((r0_96, hh, slice(ci * 128, (ci + 1) * 128)), subs=hh)],
                                      v=Vb.v((slice(None), ci, slice(hh * 64, (hh + 1) * 64))), mask=None)
                for qt in range(4):
                    t0, n = TT[qt]
                    self.attend([q96.v((r0_96, hh, slice(t0, t0 + n)), subs=hh)], [kch(ci) for ci in range(18)], 512,
                                OTb.v((sl64, hh, slice(t0, t0 + n)), subs=hh), None, scale)
                t0, n = TT[4]
                self.attend([q96.v((r0_96, hh, slice(t0, t0 + n)), subs=hh)], [kch(16), kch(17)], 256,
                            OTb.v((sl64, hh, slice(t0, t0 + n)), subs=hh), None, scale)
            self.out_proj(l, wo, OTb, 2, 5)

    def epilogue(self, do_norm=True):
        P = self.P
        self.new_phase()
        sq = [self.alloc(f"esq{i}", [128, 512], F32) for i in range(2)]
        rstd = self.alloc("erstd", [128, 512], F32)
        tmul = [self.alloc(f"etmul{i}", [128, 512], F32) for i in range(2)]
        hn = self.alloc("hn", [128, KC, 512], F32, nsub=KC)
        ost = [self.alloc(f"ost{i}", [128, D], F32) for i in range(2)]
        outbuf = Buf("outdram", self.out, 16)
        n = 0
        for tt in range(4):
            t0, _ = TT[tt]
            if do_norm:
                self.norm_tile(tt, gain_fn=lambda kc: self.normc.v((slice(None), kc, slice(8, 9))),
                               shift_fn=lambda kc: None,
                               out_fn=lambda kc: [hn.v((slice(None), kc, slice(None)), subs=kc)],
                               tmp=(sq, rstd, tmul), ps_i=0 + (tt % 2))
            else:
                for kc in range(KC):
                    P.copy("pool", hn.v((slice(None), kc, slice(None)), subs=kc),
                           self.xT.v((slice(None), kc, slice(t0, t0 + 512)), subs=self.xsub(kc, tt)))
            for sub in range(4):
                o = ost[n % 2]
                for half in range(2):
                    pb = self.ps[2 + (n * 2 + half) % 4]
                    for j in range(4):
                        kc = half * 4 + j
                        P.transpose(pb.v((slice(None), slice(j * 128, (j + 1) * 128)), subs=j),
                                    hn.v((slice(None), kc, slice(sub * 128, (sub + 1) * 128)), subs=kc), self.ident.v())
                    P.copy("dve" if half == 0 else "act", o.v((slice(None), slice(half * 512, (half + 1) * 512))), pb.v())
                tok = t0 + sub * 128
                P.dma("sp", self.out[tok:tok + 128, :], o.ap, reads=[o.v()], writes=[outbuf.v(subs=tok // 128)])
                n += 1
        P.op("sp", lambda e: e.nop(), reads=[outbuf.v()])

    def build(self, final_norm=True):
        self.consts()
        self.load_small()
        self.load_x()
        for l in self.layers:
            last = (l == DEPTH - 1)
            if self.do_mixer:
                self.norm1(l)
                kind = l % 3
                if kind == 0:
                    self.mixer_a(l, last)
                elif kind == 1:
                    self.mixer_b(l)
                else:
                    self.mixer_c(l)
            if self.do_moe:
                self.ffn_phase(l, last)
        self.epilogue(final_norm)
        return self.P.emit()


def rope_tables(rot_dim, nrows, row_off):
    f32 = np.float32
    t = np.arange(S)
    row = (t // 64).astype(f32)
    col = (t % 64).astype(f32)
    nf = rot_dim // 4
    inv = (f32(10000.0) ** (-np.arange(nf, dtype=f32) / f32(nf))).astype(f32)
    ang = np.concatenate([row[:, None] * inv, col[:, None] * inv], axis=-1).astype(f32)
    cos = np.cos(ang).astype(f32)
    sin = np.sin(ang).astype(f32)
    out = np.zeros((2, nrows, T), f32)
    out[0] = 1.0
    for d in range(rot_dim):
        i = d // 2
        out[0, row_off + d, :S] = cos[:, i]
        out[1, row_off + d, :S] = -sin[:, i] if d % 2 == 0 else sin[:, i]
    return out


def mixer_host_inputs(inp):
    f = lambda a: np.ascontiguousarray(np.asarray(a, dtype=np.float32))
    NEG = np.float32(-30000.0)
    a_w_in = f(inp["a_w_in"])
    perm = np.arange(1280) ^ 1
    a_sw = a_w_in[:, :, perm]
    j = np.arange(128)[:, None]
    i = np.arange(128)[None, :]
    maskA = np.stack([np.where(j >= i, 0.0, NEG), np.where(j <= i, 0.0, NEG)]).astype(np.float32)
    rpb = f(inp["b_rpb"])[0]
    kc = np.arange(64)[:, None]
    qc = np.arange(64)[None, :]
    ws = np.clip(qc - 8, 0, 48)
    ok = (kc >= ws) & (kc < ws + 16)
    idx = np.clip(kc - qc + 15, 0, 30)
    tm = np.full((16, 16, 64, 64), NEG, np.float32)
    tm[:, :15] = np.where(ok[None, None], rpb[:, :, idx], NEG)
    c_w_in = f(inp["c_w_in"])[0]
    c_sw = c_w_in[:, 320:416].copy()
    c_sw[:, 64:96] = c_w_in[:, 384:416][:, np.arange(32) ^ 1]
    uq = f(inp["c_w_uq"])[0]
    uqs = uq.copy()
    for h in range(16):
        uqs[:, h * 96 + 64:h * 96 + 96] = uq[:, h * 96 + 64:h * 96 + 96][:, np.arange(32) ^ 1]
    c_norms = np.stack([inp["c_q_norm"][0][:128], inp["c_q_norm"][0][128:], inp["c_kv_norm"][0]])
    return {
        "a_w_in": a_w_in, "a_w_in_sw": f(a_sw), "a_w_out": f(inp["a_w_out"]), "a_sink": f(inp["a_sink"]),
        "ropeA": rope_tables(64, 64, 0), "maskA": maskA,
        "b_w_in": f(inp["b_w_in"])[0], "b_w_out": f(inp["b_w_out"])[0], "b_tm": f(tm),
        "c_w_in": c_w_in, "c_w_in_sw": f(c_sw), "c_norms": f(c_norms),
        "c_w_uq": uq, "c_w_uq_sw": f(uqs), "c_w_ukv": f(inp["c_w_ukv"])[0], "c_w_out": f(inp["c_w_out"])[0],
        "ropeC": rope_tables(32, 96, 64),
    }


def make_in_maps(inp, ncores=8, mk=None):
    f = lambda a: np.ascontiguousarray(np.asarray(a, dtype=np.float32))
    ml = mk.moe_layers if (mk is not None and mk.moe_layers) else [0]
    nx = max(1, mk.n_experts) if (mk is not None and mk.do_moe) else 1
    if mk is None:
        ml, nx = list(range(DEPTH)), NE
    msl = lambda a: f(np.asarray(a)[ml][:, :nx])
    norms = f(np.concatenate([inp["norm_mix"], inp["norm_ffn"], inp["norm_out"][None, :]], axis=0))
    shared = {
        "cst_ident": np.eye(128, dtype=np.float32),
        "ada_w": f(inp["ada_w"]), "ada_b": f(inp["ada_b"]), "norms": norms,
        "router_w": f(inp["router_w"]), "router_b": f(inp["router_b"]),
        "moe_w_gate": msl(inp["moe_w_gate"]), "moe_w_up": msl(inp["moe_w_up"]), "moe_w_down": msl(inp["moe_w_down"]),
        "moe_b_gate": f(inp["moe_b_gate"]), "moe_b_up": f(inp["moe_b_up"]), "moe_b_down": f(inp["moe_b_down"]),
    }
    if mk is None or mk.do_mixer:
        shared.update(mixer_host_inputs(inp))
    maps = []
    for b in range(ncores):
        m = dict(shared)
        m["x"] = f(inp["x"][b])
        m["ctx"] = f(inp["ctx"][b])
        m["c2"] = f(np.stack([inp["c"][b], inp["c_ctx"]], axis=0))
        maps.append(m)
    return maps


def kernel(**inputs):
    mk = MK()
    nc = mk.build()
    in_maps = make_in_maps(inputs, 8, mk)
    res = run_bass_kernel_spmd(nc, in_maps, core_ids=list(range(8)))
    return np.stack([np.asarray(r["out"], dtype=np.float32) for r in res.results], axis=0)
```

```python
from contextlib import ExitStack
import numpy as np
import concourse.bass as bass
import concourse.mybir as mybir
from concourse.bass_utils import run_bass_kernel_spmd

F32 = mybir.dt.float32
BF16 = mybir.dt.bfloat16
AF = mybir.ActivationFunctionType
ALU = mybir.AluOpType
AX = mybir.AxisListType

ENG = ["pe", "act", "dve", "pool", "sp"]
N_DMA_SEMS = 12

D = 1024
KC = 8
S = 2048
C = 256
T = S + C
DEPTH = 4
NE = 32
EPS = 1e-6
TT = [(0, 512), (512, 512), (1024, 512), (1536, 512), (2048, 256)]
ARENA_WORDS = 53000


class V:
    __slots__ = ("ap", "keys")

    def __init__(self, ap, keys):
        self.ap = ap
        self.keys = keys


class Buf:
    def __init__(self, name, ap, nsub=1):
        self.name = name
        self.ap = ap
        self.nsub = nsub

    def v(self, idx=None, subs=None):
        ap = self.ap[idx] if idx is not None else self.ap
        if subs is None:
            keys = [(self.name, i) for i in range(self.nsub)]
        elif isinstance(subs, int):
            keys = [(self.name, subs)]
        else:
            keys = [(self.name, i) for i in subs]
        return V(ap, keys)


class Op:
    __slots__ = ("fn", "deps", "dma", "signal", "sem", "val", "sigcount")

    def __init__(self, fn, deps, dma):
        self.fn = fn
        self.deps = deps
        self.dma = dma
        self.signal = False
        self.sem = None
        self.val = 0
        self.sigcount = 0


class Prog:
    def __init__(self):
        self.nc = bass.Bass("TRN2", target_bir_lowering=False)
        self.ops = {e: [] for e in ENG}
        self.res_w = {}
        self.res_r = {}
        self.es = ExitStack()
        self.dma_since_barrier = []
        self.nbuf = 0

    def op(self, eng, fn, reads=(), writes=(), dma=False, extra_deps=()):
        deps = {}

        def add(tok):
            e, i = tok
            if self.ops[e][i].dma:
                deps[("dma", e, i)] = tok
            else:
                if e == "pe" and eng == "pe":
                    return
                k = ("eng", e)
                if k not in deps or deps[k][1] < i:
                    deps[k] = tok

        for v in reads:
            for k in v.keys:
                w = self.res_w.get(k)
                if w is not None:
                    add(w)
                if k[0].startswith("ps"):
                    for r in self.res_r.get(k, ()):
                        if r[0] != eng:
                            add(r)
        for v in writes:
            for k in v.keys:
                w = self.res_w.get(k)
                if w is not None:
                    add(w)
                for r in self.res_r.get(k, ()):
                    add(r)
        for tok in extra_deps:
            add(tok)
        idx = len(self.ops[eng])
        tok = (eng, idx)
        self.ops[eng].append(Op(fn, list(deps.values()), dma))
        if dma:
            self.dma_since_barrier.append(tok)
        for v in reads:
            for k in v.keys:
                self.res_r.setdefault(k, []).append(tok)
        for v in writes:
            for k in v.keys:
                self.res_w[k] = tok
                self.res_r[k] = []
        return tok

    def barrier(self):
        last = [(e, len(self.ops[e]) - 1) for e in ENG if self.ops[e]]
        deps = last + list(self.dma_since_barrier)
        self.dma_since_barrier = []
        for e in ENG:
            self.op(e, lambda eng: eng.nop(), extra_deps=deps)
        self.res_w = {}
        self.res_r = {}

    def mm(self, out, lhsT, rhs, start=True, stop=True):
        return self.op("pe", lambda e: e.matmul(out.ap, lhsT.ap, rhs.ap, start=start, stop=stop),
                       reads=[lhsT, rhs], writes=[out])

    def transpose(self, out, in_, ident):
        return self.op("pe", lambda e: e.transpose(out.ap, in_.ap, ident.ap),
                       reads=[in_, ident], writes=[out])

    def act(self, out, in_, func, bias=None, scale=1.0, accum=None):
        reads = [in_]
        kw = {}
        if isinstance(bias, V):
            reads.append(bias)
            kw["bias"] = bias.ap
        elif bias is not None:
            kw["bias"] = float(bias)
        if isinstance(scale, V):
            reads.append(scale)
            kw["scale"] = scale.ap
        else:
            kw["scale"] = float(scale)
        writes = [out]
        if accum is not None:
            writes.append(accum)
            kw["accum_out"] = accum.ap
        return self.op("act", lambda e: e.activation(out.ap, in_.ap, func, **kw), reads=reads, writes=writes)

    def ts(self, eng, out, in0, s1, s2, op0, op1=None, accum=None):
        reads = [in0]
        a1 = s1
        a2 = s2
        if isinstance(s1, V):
            reads.append(s1)
            a1 = s1.ap
        if isinstance(s2, V):
            reads.append(s2)
            a2 = s2.ap
        kw = {}
        if op1 is not None:
            kw["op1"] = op1
        writes = [out]
        if accum is not None:
            writes.append(accum)
            kw["accum_out"] = accum.ap
        return self.op(eng, lambda e: e.tensor_scalar(out.ap, in0.ap, a1, a2, op0, **kw), reads=reads, writes=writes)

    def tt(self, eng, out, in0, in1, op):
        return self.op(eng, lambda e: e.tensor_tensor(out.ap, in0.ap, in1.ap, op), reads=[in0, in1], writes=[out])

    def stt(self, eng, out, in0, scalar, in1, op0, op1):
        reads = [in0, in1]
        a = scalar
        if isinstance(scalar, V):
            reads.append(scalar)
            a = scalar.ap
        return self.op(eng, lambda e: e.scalar_tensor_tensor(out.ap, in0.ap, a, in1.ap, op0, op1),
                       reads=reads, writes=[out])

    def copy(self, eng, out, in_):
        if eng == "act":
            return self.op(eng, lambda e: e.copy(out.ap, in_.ap), reads=[in_], writes=[out])
        return self.op(eng, lambda e: e.tensor_copy(out.ap, in_.ap), reads=[in_], writes=[out])

    def recip(self, out, in_):
        return self.op("dve", lambda e: e.reciprocal(out.ap, in_.ap), reads=[in_], writes=[out])

    def memset(self, eng, out, val):
        return self.op(eng, lambda e: e.memset(out.ap, val), writes=[out])

    def dma(self, eng, out_ap, in_ap, reads=(), writes=(), **kw):
        return self.op(eng, lambda e: e.dma_start(out_ap, in_ap, **kw), reads=reads, writes=writes, dma=True)

    def emit(self):
        nc = self.nc
        es = self.es
        for e in ENG:
            for op in self.ops[e]:
                for (pe_, pi) in op.deps:
                    p = self.ops[pe_][pi]
                    if not p.dma:
                        p.signal = True
        EPOCH = 8000
        eng_sem = {}
        for e in ENG:
            c = 0
            for op in self.ops[e]:
                if op.dma:
                    continue
                if op.signal:
                    c += 1
                op.sigcount = c
            nep = max(1, (c + EPOCH - 1) // EPOCH)
            eng_sem[e] = [es.enter_context(nc.semaphore(f"s_{e}_{i}")) for i in range(nep)]

        def sig_of(e, sigcount):
            ep = (sigcount - 1) // EPOCH
            return eng_sem[e][ep], sigcount - ep * EPOCH

        for e in ENG:
            dl = [op for op in self.ops[e] if op.dma]
            if not dl:
                continue
            sems = [es.enter_context(nc.semaphore(f"d_{e}_{i}")) for i in range(N_DMA_SEMS)]
            uses = [0] * N_DMA_SEMS
            for j, op in enumerate(dl):
                s = j % N_DMA_SEMS
                uses[s] += 1
                op.sem = sems[s]
                op.val = 16 * uses[s]
        self.stats = {e: (len(self.ops[e]), max([o.sigcount for o in self.ops[e]] + [0])) for e in ENG}

        def run(e, eng):
            seen = {}

            def wait(sem, val):
                k = sem.num
                if seen.get(k, 0) < val:
                    eng.wait_ge(sem, val)
                    seen[k] = val

            for op in self.ops[e]:
                for (pe_, pi) in op.deps:
                    p = self.ops[pe_][pi]
                    if p.dma:
                        wait(p.sem, p.val)
                    else:
                        wait(*sig_of(pe_, p.sigcount))
                if op.dma:
                    if op.val > 16:
                        wait(op.sem, op.val - 16)
                    ins = op.fn(eng)
                    ins.then_inc(op.sem, 16)
                else:
                    ins = op.fn(eng)
                    if op.signal:
                        ins.then_inc(sig_of(e, op.sigcount)[0], 1)

        block = es.enter_context(nc.Block())

        @block.tensor
        def _(eng):
            run("pe", eng)

        @block.scalar
        def _(eng):
            run("act", eng)

        @block.vector
        def _(eng):
            run("dve", eng)

        @block.gpsimd
        def _(eng):
            run("pool", eng)

        @block.sync
        def _(eng):
            run("sp", eng)

        es.close()
        return nc


class MK:
    def __init__(self, layers=(0, 1, 2, 3), do_mixer=True, do_moe=True, n_experts=NE):
        self.layers = list(layers)
        self.do_mixer = do_mixer
        self.do_moe = do_moe
        self.n_experts = n_experts
        self.P = Prog()
        P = self.P
        nc = P.nc
        di = lambda n, s: nc.dram_tensor(n, list(s), F32, kind="ExternalInput").ap()
        self.x = di("x", [S, D])
        self.ctx = di("ctx", [C, D])
        self.c2 = di("c2", [2, D])
        self.ada_w = di("ada_w", [DEPTH, D, 6 * D])
        self.ada_b = di("ada_b", [DEPTH, 6 * D])
        self.norms = di("norms", [9, D])
        self.router_w = di("router_w", [DEPTH, D, NE])
        self.router_b = di("router_b", [DEPTH, NE])
        self.moe_layers = [l for l in self.layers] if do_moe else []
        self.lmap = {l: i for i, l in enumerate(self.moe_layers)}
        nl = max(1, len(self.moe_layers))
        nx = max(1, n_experts) if do_moe else 1
        self.w_gate = di("moe_w_gate", [nl, nx, D, D])
        self.w_up = di("moe_w_up", [nl, nx, D, D])
        self.w_down = di("moe_w_down", [nl, nx, D, D])
        self.b_gate = di("moe_b_gate", [DEPTH, NE, D])
        self.b_up = di("moe_b_up", [DEPTH, NE, D])
        self.b_down = di("moe_b_down", [DEPTH, NE, D])
        self.cst_ident = di("cst_ident", [128, 128])
        if do_mixer:
            self.a_w_in = di("a_w_in", [2, D, 1536])
            self.a_w_in_sw = di("a_w_in_sw", [2, D, 1280])
            self.a_w_out = di("a_w_out", [2, D, D])
            self.a_sink = di("a_sink", [2, 16])
            self.ropeA = di("ropeA", [2, 64, T])
            self.maskA = di("maskA", [2, 128, 128])
            self.b_w_in = di("b_w_in", [D, 3072])
            self.b_w_out = di("b_w_out", [D, D])
            self.b_tm = di("b_tm", [16, 16, 64, 64])
            self.c_w_in = di("c_w_in", [D, 416])
            self.c_w_in_sw = di("c_w_in_sw", [D, 96])
            self.c_norms = di("c_norms", [3, 128])
            self.c_w_uq = di("c_w_uq", [256, 1536])
            self.c_w_uq_sw = di("c_w_uq_sw", [256, 1536])
            self.c_w_ukv = di("c_w_ukv", [128, 2048])
            self.c_w_out = di("c_w_out", [D, D])
            self.ropeC = di("ropeC", [2, 96, T])
        self.out = nc.dram_tensor("out", [S, D], F32, kind="ExternalOutput").ap()
        self.gscr = nc.dram_tensor("gscr", [NE, T], F32, kind="Internal").ap()
        self.gscr_buf = Buf("gscr", self.gscr, 1)

        self.arena = P.es.enter_context(nc.sbuf_tensor("arena", [128, ARENA_WORDS], F32))
        self.ps = []
        for i in range(8):
            h = P.es.enter_context(nc.psum_tensor(f"ps{i}", [128, 512], F32))
            self.ps.append(Buf(f"ps{i}", h[:], 4))
        self.top = 0
        self.xT = self.alloc("xT", [128, KC, T], F32, nsub=KC * 5)
        self.hT = self.alloc("hT", [128, KC, T], BF16, nsub=KC * 5)
        self.ident = self.alloc("ident", [128, 128], F32)
        self.ones32 = self.alloc("ones32", [128, 128], F32)
        self.ones16 = self.alloc("ones16", [128, 64], BF16)
        self.modc = self.alloc("modc", [128, DEPTH, 6, KC, 2], F32, nsub=DEPTH)
        self.normc = self.alloc("normc", [128, KC, 9], F32)
        self.gm = self.alloc("gm", [128, 2, KC, 2], F32, nsub=2)
        self.phase_base = self.top

    def alloc(self, name, shape, dtype, nsub=1):
        n = int(np.prod(shape[1:]))
        words = n if dtype == F32 else (n + 1) // 2
        words = (words + 7) // 8 * 8
        lo = self.top
        self.top += words
        assert self.top <= ARENA_WORDS, (name, self.top)
        ap = self.arena[0:shape[0], lo:lo + words]
        if dtype != F32:
            ap = ap.bitcast(dtype)
        ap = ap[:, 0:n]
        if len(shape) == 3:
            ap = ap.rearrange("p (a b) -> p a b", a=shape[1])
        elif len(shape) == 4:
            ap = ap.rearrange("p (a b c) -> p a b c", a=shape[1], b=shape[2])
        elif len(shape) == 5:
            ap = ap.rearrange("p (a b c d) -> p a b c d", a=shape[1], b=shape[2], c=shape[3])
        self.P.nbuf += 1
        return Buf(f"{name}#{self.P.nbuf}", ap, nsub)

    def new_phase(self):
        self.P.barrier()
        self.top = self.phase_base

    def release(self, mark):
        self.P.barrier()
        self.top = mark

    def xsub(self, kc, tt):
        return kc * 5 + tt

    def psv(self, i, cols=slice(0, 512), parts=slice(0, 128), subs=None):
        return self.ps[i].v((parts, cols), subs=subs)

    def consts(self):
        P = self.P
        P.memset("dve", self.ones32.v(), 1.0)
        P.memset("dve", self.ones16.v(), 1.0)
        P.dma("sp", self.ident.ap, self.cst_ident, writes=[self.ident.v()])

    def rows_to_cols(self, dram_rows_ap, nrows, dst_fn, stage, ps_i):
        P = self.P
        P.dma("sp", stage.ap[0:nrows, :], dram_rows_ap, writes=[stage.v()])
        for kc in range(KC):
            pv = self.ps[ps_i].v((slice(0, 128), slice(kc * 32, kc * 32 + nrows)))
            P.transpose(pv, stage.v((slice(0, nrows), slice(kc * 128, (kc + 1) * 128))),
                        self.ident.v((slice(0, nrows), slice(0, nrows))))
            P.copy("dve", dst_fn(kc), pv)

    def load_x(self):
        P = self.P
        st = [self.alloc(f"xst{i}", [128, D], F32) for i in range(2)]
        n = 0
        for ti in range(T // 128):
            tok0 = ti * 128
            tt = min(tok0 // 512, 4)
            s = st[ti % 2]
            src = self.x[tok0:tok0 + 128, :] if tok0 < S else self.ctx[tok0 - S:tok0 - S + 128, :]
            P.dma("sp", s.ap, src, writes=[s.v()])
            for half in range(2):
                pb = self.ps[n % 4]
                n += 1
                for j in range(4):
                    kc = half * 4 + j
                    P.transpose(pb.v((slice(None), slice(j * 128, (j + 1) * 128)), subs=j),
                                s.v((slice(None), slice(kc * 128, (kc + 1) * 128))), self.ident.v())
                dst = self.xT.v((slice(None), slice(half * 4, half * 4 + 4), slice(tok0, tok0 + 128)),
                                subs=[self.xsub(half * 4 + j, tt) for j in range(4)])
                src_v = V(pb.ap.rearrange("p (a b) -> p a b", a=4), pb.v().keys)
                P.copy("dve" if half == 0 else "act", dst, src_v)

    def load_small(self):
        P = self.P
        stage = self.alloc("sm_stage", [128, D], F32)
        self.rows_to_cols(self.norms, 9, lambda kc: self.normc.v((slice(None), kc, slice(0, 9))), stage, 4)
        sc = self.alloc("siluc", [128, KC, 2], F32)
        self.rows_to_cols(self.c2, 2, lambda kc: sc.v((slice(None), kc, slice(0, 2))), stage, 4)
        sig = self.alloc("silu_sig", [128, KC, 2], F32)
        P.act(sig.v(), sc.v(), AF.Sigmoid)
        P.tt("dve", sc.v(), sc.v(), sig.v(), ALU.mult)
        adab = self.alloc("adab", [128, DEPTH, 48], F32)
        stage2 = self.alloc("sm_stage2", [48, 128], F32)
        for l in range(DEPTH):
            P.dma("sp", stage2.ap, self.ada_b[l].rearrange("(j p) -> j p", p=128), writes=[stage2.v()])
            pv = self.ps[5].v((slice(0, 128), slice(0, 48)))
            P.transpose(pv, stage2.v(), self.ident.v((slice(0, 48), slice(0, 48))))
            P.copy("dve", adab.v((slice(None), l, slice(None))), pv)
        wst = [self.alloc(f"adaw{i}", [128, KC, 512], F32) for i in range(2)]
        n = 0
        for l in range(DEPTH):
            for piece in range(12):
                w = wst[n % 2]
                n += 1
                src = self.ada_w[l].rearrange("(kc p) c -> p kc c", p=128)[:, :, piece * 512:(piece + 1) * 512]
                P.dma("sp", w.ap, src, writes=[w.v()])
                pb = self.ps[6 + (n % 2)]
                for q in range(4):
                    for kc in range(KC):
                        P.mm(pb.v((slice(None), slice(q * 2, q * 2 + 2))),
                             w.v((slice(None), kc, slice(q * 128, (q + 1) * 128))),
                             sc.v((slice(None), kc, slice(0, 2))), start=(kc == 0), stop=(kc == KC - 1))
                j, kc0 = divmod(piece * 4, 8)
                for q in range(4):
                    idx = piece * 4 + q
                    P.ts("dve", self.modc.v((slice(None), l, j, kc0 + q, slice(0, 2)), subs=l),
                         pb.v((slice(None), slice(q * 2, q * 2 + 2))),
                         adab.v((slice(None), l, slice(idx, idx + 1))), None, ALU.add)

    def prep_gm(self, l, which):
        P = self.P
        jsc = 1 if which == 0 else 4
        nidx = l if which == 0 else 4 + l
        for typ in range(2):
            P.ts("dve", self.gm.v((slice(None), which, slice(None), typ), subs=which),
                 self.modc.v((slice(None), l, jsc, slice(None), typ), subs=l), 1.0, None, ALU.add)
            P.tt("dve", self.gm.v((slice(None), which, slice(None), typ), subs=which),
                 self.gm.v((slice(None), which, slice(None), typ), subs=which),
                 self.normc.v((slice(None), slice(None), nidx)), ALU.mult)

    def norm_tile(self, tt, gain_fn, shift_fn, out_fn, tmp, ps_i, tmax=None):
        P = self.P
        t0, n = TT[tt]
        if tmax is not None:
            n = min(n, tmax)
        sq, rstd, tmul = tmp
        pb = self.ps[ps_i]
        for kc in range(KC):
            s = sq[kc % 2]
            P.act(s.v((slice(None), slice(0, n))), self.xT.v((slice(None), kc, slice(t0, t0 + n)), subs=self.xsub(kc, tt)),
                  AF.Square)
            P.mm(pb.v((slice(None), slice(0, n))), self.ones32.v(), s.v((slice(None), slice(0, n))),
                 start=(kc == 0), stop=(kc == KC - 1))
        P.ts("dve", rstd.v((slice(None), slice(0, n))), pb.v((slice(None), slice(0, n))), 1.0 / D, EPS, ALU.mult, ALU.add)
        P.act(rstd.v((slice(None), slice(0, n))), rstd.v((slice(None), slice(0, n))), AF.Sqrt)
        P.recip(rstd.v((slice(None), slice(0, n))), rstd.v((slice(None), slice(0, n))))
        for kc in range(KC):
            tm = tmul[kc % 2]
            P.tt("dve", tm.v((slice(None), slice(0, n))),
                 self.xT.v((slice(None), kc, slice(t0, t0 + n)), subs=self.xsub(kc, tt)),
                 rstd.v((slice(None), slice(0, n))), ALU.mult)
            sh = shift_fn(kc)
            outs = out_fn(kc)
            first = outs[0]
            P.act(first, tm.v((slice(None), slice(0, n))), AF.Identity, bias=sh if sh is not None else 0.0,
                  scale=gain_fn(kc))
            for o in outs[1:]:
                P.copy("pool", o, first)

    def ffn_phase(self, l, last):
        P = self.P
        ntt = 4 if last else 5
        Tl = S if last else T
        self.new_phase()
        self.prep_gm(l, 1)
        h32 = self.alloc("h32", [128, KC, 512], F32, nsub=KC)
        sq = [self.alloc(f"sq{i}", [128, 512], F32) for i in range(2)]
        rstd = self.alloc("rstd", [128, 512], F32)
        tmul = [self.alloc(f"tmul{i}", [128, 512], F32) for i in range(2)]
        rw = self.alloc("rw", [128, KC, NE], F32)
        rb = self.alloc("rb", [128, NE], F32)
        gatesT = self.alloc("gatesT", [NE, T], F32, nsub=5)
        lg = [self.alloc(f"lg{i}", [128, NE], F32) for i in range(2)]
        ex = [self.alloc(f"ex{i}", [128, NE], F32) for i in range(2)]
        mk = [self.alloc(f"mk{i}", [128, NE], F32) for i in range(2)]
        top8 = [self.alloc(f"top8{i}", [128, 8], F32) for i in range(2)]
        sm = [self.alloc(f"sm{i}", [128, 4], F32) for i in range(2)]
        P.dma("sp", rw.ap, self.router_w[l].rearrange("(kc p) e -> p kc e", p=128), writes=[rw.v()])
        P.dma("sp", rb.ap, self.router_b[l:l + 1, :].to_broadcast([128, NE]), writes=[rb.v()])
        it = 0
        for tt in range(ntt):
            t0, n = TT[tt]
            typ = 0 if tt < 4 else 1
            self.norm_tile(
                tt,
                gain_fn=lambda kc: self.gm.v((slice(None), 1, kc, slice(typ, typ + 1)), subs=1),
                shift_fn=lambda kc: self.modc.v((slice(None), l, 3, kc, slice(typ, typ + 1)), subs=l),
                out_fn=lambda kc: [h32.v((slice(None), kc, slice(0, n)), subs=kc),
                                   self.hT.v((slice(None), kc, slice(t0, t0 + n)), subs=self.xsub(kc, tt))],
                tmp=(sq, rstd, tmul), ps_i=0 + (tt % 2))
            for sub in range(n // 128):
                i2 = it % 2
                it += 1
                pl = self.ps[2 + i2]
                for kc in range(KC):
                    P.mm(pl.v((slice(None), slice(0, NE)), subs=0),
                         h32.v((slice(None), kc, slice(sub * 128, (sub + 1) * 128)), subs=kc),
                         rw.v((slice(None), kc, slice(None))), start=(kc == 0), stop=(kc == KC - 1))
                L = lg[i2]
                P.tt("dve", L.v(), pl.v((slice(None), slice(0, NE)), subs=0), rb.v(), ALU.add)
                P.op("dve", lambda e, o=top8[i2], i=L: e.max(o.ap, i.ap), reads=[L.v()], writes=[top8[i2].v()])
                P.ts("dve", mk[i2].v(), L.v(), top8[i2].v((slice(None), slice(3, 4))), None, ALU.is_ge)
                P.ts("dve", sm[i2].v((slice(None), slice(0, 1))), top8[i2].v((slice(None), slice(0, 1))), -1.0, None, ALU.mult)
                P.act(ex[i2].v(), L.v(), AF.Exp, bias=sm[i2].v((slice(None), slice(0, 1))))
                P.tt("dve", ex[i2].v(), ex[i2].v(), mk[i2].v(), ALU.mult)
                P.op("dve", lambda e, o=sm[i2], i=ex[i2]: e.reduce_sum(o.ap[:, 1:2], i.ap, axis=AX.X),
                     reads=[ex[i2].v()], writes=[sm[i2].v()])
                P.recip(sm[i2].v((slice(None), slice(2, 3))), sm[i2].v((slice(None), slice(1, 2))))
                P.ts("dve", ex[i2].v(), ex[i2].v(), sm[i2].v((slice(None), slice(2, 3))), None, ALU.mult)
                pg = self.ps[4 + i2]
                P.transpose(pg.v((slice(0, NE), slice(0, 128)), subs=0), ex[i2].v(), self.ident.v())
                tok = t0 + sub * 128
                P.copy("act", gatesT.v((slice(None), slice(tok, tok + 128)), subs=tt),
                       pg.v((slice(0, NE), slice(0, 128)), subs=0))
        P.dma("sp", self.gscr[:, 0:Tl], gatesT.ap[:, 0:Tl], reads=[gatesT.v()], writes=[self.gscr_buf.v()])

        self.new_phase()
        bgc = self.alloc("bgc", [128, KC, NE], F32)
        buc = self.alloc("buc", [128, KC, NE], F32)
        mark = self.top
        bstage = self.alloc("bstage", [NE, D], F32)
        bd = self.alloc("bd", [NE, D], F32)
        gT32 = self.alloc("gT32", [NE, T], F32)
        self.rows_to_cols(self.b_gate[l], NE, lambda kc: bgc.v((slice(None), kc, slice(None))), bstage, 7)
        self.rows_to_cols(self.b_up[l], NE, lambda kc: buc.v((slice(None), kc, slice(None))), bstage, 7)
        P.dma("sp", bd.ap, self.b_down[l], writes=[bd.v()])
        P.dma("sp", gT32.ap[:, 0:Tl], self.gscr[:, 0:Tl], reads=[self.gscr_buf.v()], writes=[gT32.v()])
        g2 = lambda kc, typ: self.modc.v((slice(None), l, 5, kc, slice(typ, typ + 1)), subs=l)
        for tt in range(ntt):
            t0, n = TT[tt]
            typ = 0 if tt < 4 else 1
            for dc in range(KC):
                pb = self.ps[dc % 2]
                P.mm(pb.v((slice(None), slice(0, n))), bd.v((slice(None), slice(dc * 128, (dc + 1) * 128))),
                     gT32.v((slice(None), slice(t0, t0 + n))))
                xv = self.xT.v((slice(None), dc, slice(t0, t0 + n)), subs=self.xsub(dc, tt))
                P.stt("dve", xv, pb.v((slice(None), slice(0, n))), g2(dc, typ), xv, ALU.mult, ALU.add)
        self.release(mark)
        NU = 2
        ALPHA = 1.702
        C7 = float(ALPHA * 7.0 / (1.0 + np.exp(-7.0 * ALPHA)))
        wg = [self.alloc(f"wg{i}", [128, KC, 512], BF16) for i in range(NU)]
        wu = [self.alloc(f"wu{i}", [128, KC, 512], BF16) for i in range(NU)]
        wd = [self.alloc(f"wd{i}", [128, 4, D], BF16) for i in range(NU)]
        G = [self.alloc(f"G{i}", [128, T], BF16) for i in range(2)]
        aT = [self.alloc(f"aT{i}", [128, 4, 512], BF16, nsub=4) for i in range(2)]
        silb = [self.alloc(f"silb{i}", [128, 512], F32) for i in range(2)]
        uvb = [self.alloc(f"uvb{i}", [128, 512], F32) for i in range(2)]
        g2a = self.alloc("g2a", [128, KC, 2], F32)
        P.ts("dve", bgc.v(), bgc.v(), ALPHA, None, ALU.mult)
        P.ts("dve", buc.v(), buc.v(), 1.0, None, ALU.add)
        P.ts("dve", g2a.v(), self.modc.v((slice(None), l, 5, slice(None), slice(None)), subs=l), 1.0 / ALPHA, None, ALU.mult)
        items = [(e, half, tt) for e in range(self.n_experts) for half in range(2) for tt in range(ntt)]
        st_ = {"tn": 0}

        def gu(i):
            e, half, tt = items[i]
            u = (e * 2 + half) % NU
            Ge = G[e % 2]
            if tt == 0:
                if half == 0:
                    P.dma("pool", Ge.ap[:, 0:Tl], self.gscr[e:e + 1, 0:Tl].to_broadcast([128, Tl]),
                          reads=[self.gscr_buf.v()], writes=[Ge.v()])
                fsl = slice(half * 512, (half + 1) * 512)
                P.dma("pool", wg[u].ap, self.w_gate[self.lmap[l], e].rearrange("(kc p) f -> p kc f", p=128)[:, :, fsl],
                      writes=[wg[u].v()])
                P.dma("pool", wu[u].ap, self.w_up[self.lmap[l], e].rearrange("(kc p) f -> p kc f", p=128)[:, :, fsl],
                      writes=[wu[u].v()])
                P.dma("pool", wd[u].ap, self.w_down[self.lmap[l], e].rearrange("(fc p) d -> p fc d", p=128)[:, half * 4:half * 4 + 4, :],
                      writes=[wd[u].v()])
            t0, n = TT[tt]
            A = aT[i % 2]
            sn = slice(0, n)
            for fc in range(4):
                fcg = half * 4 + fc
                tn = st_["tn"]
                st_["tn"] += 1
                pg_ = self.ps[2 + (tn % 2)]
                pu_ = self.ps[4 + (tn % 2)]
                sil, uvt = silb[tn % 2], uvb[tn % 2]
                for kc in range(KC):
                    P.mm(pg_.v((slice(None), sn)), wg[u].v((slice(None), kc, slice(fc * 128, (fc + 1) * 128))),
                         self.hT.v((slice(None), kc, slice(t0, t0 + n)), subs=self.xsub(kc, tt)),
                         start=(kc == 0), stop=(kc == KC - 1))
                for kc in range(KC):
                    P.mm(pu_.v((slice(None), sn)), wu[u].v((slice(None), kc, slice(fc * 128, (fc + 1) * 128))),
                         self.hT.v((slice(None), kc, slice(t0, t0 + n)), subs=self.xsub(kc, tt)),
                         start=(kc == 0), stop=(kc == KC - 1))
                sv = sil.v((slice(None), sn))
                uv = uvt.v((slice(None), sn))
                P.act(sv, pg_.v((slice(None), sn)), AF.Silu, bias=bgc.v((slice(None), fcg, slice(e, e + 1))), scale=ALPHA)
                P.ts("dve", uv, pu_.v((slice(None), sn)), buc.v((slice(None), fcg, slice(e, e + 1))), -6.0, ALU.add, ALU.max)
                P.stt("dve", uv, uv, 8.0, Ge.v((slice(None), slice(t0, t0 + n))), ALU.min, ALU.mult)
                P.stt("dve", A.v((slice(None), fc, sn), subs=fc), sv, C7, uv, ALU.min, ALU.mult)

        def down(i):
            e, half, tt = items[i]
            u = (e * 2 + half) % NU
            t0, n = TT[tt]
            typ = 0 if tt < 4 else 1
            A = aT[i % 2]
            sn = slice(0, n)
            for dc in range(KC):
                py = self.ps[dc % 2]
                for fc in range(4):
                    P.mm(py.v((slice(None), sn)), wd[u].v((slice(None), fc, slice(dc * 128, (dc + 1) * 128))),
                         A.v((slice(None), fc, sn), subs=fc), start=(fc == 0), stop=(fc == 3))
                xv = self.xT.v((slice(None), dc, slice(t0, t0 + n)), subs=self.xsub(dc, tt))
                P.stt("dve", xv, py.v((slice(None), sn)), g2a.v((slice(None), dc, slice(typ, typ + 1))), xv, ALU.mult, ALU.add)

        if items:
            gu(0)
        for i in range(len(items)):
            if i + 1 < len(items):
                gu(i + 1)
            down(i)

    def norm1(self, l):
        P = self.P
        self.new_phase()
        self.prep_gm(l, 0)
        sq = [self.alloc(f"n1sq{i}", [128, 512], F32) for i in range(2)]
        rstd = self.alloc("n1rstd", [128, 512], F32)
        tmul = [self.alloc(f"n1tmul{i}", [128, 512], F32) for i in range(2)]
        for tt in range(5):
            t0, n = TT[tt]
            typ = 0 if tt < 4 else 1
            self.norm_tile(
                tt,
                gain_fn=lambda kc: self.gm.v((slice(None), 0, kc, slice(typ, typ + 1)), subs=0),
                shift_fn=lambda kc: self.modc.v((slice(None), l, 0, kc, slice(typ, typ + 1)), subs=l),
                out_fn=lambda kc: [self.hT.v((slice(None), kc, slice(t0, t0 + n)), subs=self.xsub(kc, tt))],
                tmp=(sq, rstd, tmul), ps_i=0 + (tt % 2))

    def hTv(self, kc, tt, lo=None, n=None):
        t0, nn = TT[tt]
        if lo is None:
            lo, n = t0, nn
        return self.hT.v((slice(None), kc, slice(lo, lo + n)), subs=self.xsub(kc, tt))

    def attn_setup(self):
        self.Pt = [self.alloc(f"Pt{i}", [128, 512], BF16) for i in range(4)]
        self.St = [self.alloc(f"St{i}", [128, 512], F32) for i in range(4)]
        self.rd = [self.alloc(f"rd{i}", [64, 512], F32) for i in range(2)]
        self.sbanks = [0, 1, 6, 7]
        self.pending = []
        self.a_n = 0
        self.s_n = 0

    def attn_flush(self):
        while self.pending:
            self.pending.pop(0)()

    def attend(self, qparts, chunks, QW, out_v, sink_v, scale, SKEW=2):
        P = self.P
        G = 512 // QW
        ob = self.ps[2 + self.a_n % 2]
        db = self.ps[4 + self.a_n % 2]
        rd = self.rd[self.a_n % 2]
        self.a_n += 1
        ng = (len(chunks) + G - 1) // G
        sl64 = slice(0, 64)
        for gi in range(ng):
            cs = chunks[gi * G:(gi + 1) * G]
            sb = self.ps[self.sbanks[self.s_n % 4]]
            pt = self.Pt[self.s_n % 4]
            st = self.St[self.s_n % 4]
            self.s_n += 1
            for j, ch in enumerate(cs):
                cols = slice(j * QW, (j + 1) * QW)
                for pi, (kv, qv) in enumerate(zip(ch["k"], qparts)):
                    P.mm(sb.v((slice(None), cols)), kv, qv, start=(pi == 0), stop=(pi == len(qparts) - 1))
            j = 0
            while j < len(cs):
                if cs[j]["mask"] is None:
                    j2 = j
                    while j2 < len(cs) and cs[j2]["mask"] is None:
                        j2 += 1
                    cols = slice(j * QW, j2 * QW)
                    P.act(pt.v((slice(None), cols)), sb.v((slice(None), cols)), AF.Exp, scale=scale)
                    j = j2
                else:
                    cols = slice(j * QW, (j + 1) * QW)
                    P.stt("dve", st.v((slice(None), cols)), sb.v((slice(None), cols)), scale, cs[j]["mask"],
                          ALU.mult, ALU.add)
                    P.act(pt.v((slice(None), cols)), st.v((slice(None), cols)), AF.Exp)
                    j += 1

            def pv(gi=gi, cs=cs, pt=pt):
                for j, ch in enumerate(cs):
                    cols = slice(j * QW, (j + 1) * QW)
                    first = (gi == 0 and j == 0)
                    lastc = (gi == ng - 1 and j == len(cs) - 1)
                    P.mm(ob.v((sl64, slice(0, QW))), ch["v"], pt.v((slice(None), cols)), start=first, stop=lastc)
                    P.mm(db.v((sl64, slice(0, QW))), self.ones16.v(), pt.v((slice(None), cols)), start=first, stop=lastc)
                if gi == ng - 1:
                    rv = rd.v((sl64, slice(0, QW)))
                    if sink_v is not None:
                        P.ts("dve", rv, db.v((sl64, slice(0, QW))), sink_v, None, ALU.add)
                        P.recip(rv, rv)
                    else:
                        P.recip(rv, db.v((sl64, slice(0, QW))))
                    P.tt("dve", out_v, ob.v((sl64, slice(0, QW))), rv, ALU.mult)

            self.pending.append(pv)
            while len(self.pending) > SKEW:
                self.pending.pop(0)()

    def out_proj(self, l, wo, OTb, nh, ntt):
        P = self.P
        self.attn_flush()
        for tt in range(ntt):
            t0, n = TT[tt]
            typ = 0 if tt < 4 else 1
            for dc in range(KC):
                pb = self.ps[6 + dc % 2]
                for hh in range(nh):
                    P.mm(pb.v((slice(None), slice(0, n))), wo.v((slice(None), hh, slice(dc * 128, (dc + 1) * 128))),
                         OTb.v((slice(None), hh, slice(t0, t0 + n)), subs=hh), start=(hh == 0), stop=(hh == nh - 1))
                xv = self.xT.v((slice(None), dc, slice(t0, t0 + n)), subs=self.xsub(dc, tt))
                P.stt("dve", xv, pb.v((slice(None), slice(0, n))),
                      self.modc.v((slice(None), l, 2, dc, slice(typ, typ + 1)), subs=l), xv, ALU.mult, ALU.add)

    def rope_proj(self, dst_v, wA_fn, wB_fn, rhs_fn, nkc, n, t0, cosT, sinT, prow, M, rt):
        P = self.P
        pa, pb = self.ps[6], self.ps[7]
        for kc in range(nkc):
            P.mm(pa.v((slice(0, M), slice(0, n))), wA_fn(kc), rhs_fn(kc), start=(kc == 0), stop=(kc == nkc - 1))
        for kc in range(nkc):
            P.mm(pb.v((slice(0, M), slice(0, n))), wB_fn(kc), rhs_fn(kc), start=(kc == 0), stop=(kc == nkc - 1))
        t1, t2 = rt
        P.tt("dve", t1.v((prow, slice(0, n))), pa.v((prow, slice(0, n))), cosT.v((prow, slice(t0, t0 + n))), ALU.mult)
        P.tt("dve", t2.v((prow, slice(0, n))), pb.v((prow, slice(0, n))), sinT.v((prow, slice(t0, t0 + n))), ALU.mult)
        P.tt("pool", dst_v, t1.v((prow, slice(0, n))), t2.v((prow, slice(0, n))), ALU.add)
        return pa

    def mixer_a(self, l, last):
        P = self.P
        slot = l // 3
        self.new_phase()
        nttq = 4 if last else 5
        scale = 64 ** -0.5
        cosT = self.alloc("cosA", [64, T], F32)
        sinT = self.alloc("sinA", [64, T], F32)
        P.dma("sp", cosT.ap, self.ropeA[0], writes=[cosT.v()])
        P.dma("sp", sinT.ap, self.ropeA[1], writes=[sinT.v()])
        mprev = self.alloc("mprev", [128, 128], F32)
        mnext = self.alloc("mnext", [128, 128], F32)
        P.dma("sp", mprev.ap, self.maskA[0], writes=[mprev.v()])
        P.dma("sp", mnext.ap, self.maskA[1], writes=[mnext.v()])
        sinkx = self.alloc("sinkx", [64, 16], F32)
        P.dma("sp", sinkx.ap, self.a_sink[slot:slot + 1, :].to_broadcast([64, 16]), writes=[sinkx.v()])
        P.act(sinkx.v(), sinkx.v(), AF.Exp)
        self.attn_setup()
        KTb = self.alloc("KT", [64, T], BF16)
        Vb = self.alloc("V", [128, 18, 64], BF16)
        QTb = self.alloc("QT", [64, 2, T], BF16, nsub=2)
        OTb = self.alloc("OT", [64, 2, T], BF16, nsub=2)
        wk = self.alloc("wk", [128, KC, 128], BF16)
        wv = self.alloc("wv", [128, KC, 64], BF16)
        wq = self.alloc("wq", [128, KC, 256], BF16)
        wo = self.alloc("wo", [64, 2, D], BF16)
        rt = [self.alloc(f"rt{i}", [64, 512], F32) for i in range(2)]
        win = self.a_w_in[slot].rearrange("(kc p) c -> p kc c", p=128)
        wsw = self.a_w_in_sw[slot].rearrange("(kc p) c -> p kc c", p=128)
        sl64 = slice(0, 64)
        for g in range(4):
            kc0 = 1024 + g * 64
            P.dma("pool", wk.ap[:, :, 0:64], win[:, :, kc0:kc0 + 64], writes=[wk.v()])
            P.dma("pool", wk.ap[:, :, 64:128], wsw[:, :, kc0:kc0 + 64], writes=[wk.v()])
            P.dma("pool", wv.ap, win[:, :, 1280 + g * 64:1280 + (g + 1) * 64], writes=[wv.v()])
            for tt in range(5):
                t0, n = TT[tt]
                self.rope_proj(KTb.v((sl64, slice(t0, t0 + n))),
                               lambda kc: wk.v((slice(None), kc, slice(0, 64))),
                               lambda kc: wk.v((slice(None), kc, slice(64, 128))),
                               lambda kc: self.hTv(kc, tt), KC, n, t0, cosT, sinT, sl64, 64, rt)
            for tci in range(18):
                tt = min(tci // 4, 4)
                pb = self.ps[6 + tci % 2]
                for kc in range(KC):
                    P.mm(pb.v((slice(None), slice(0, 64))), self.hTv(kc, tt, tci * 128, 128), wv.v((slice(None), kc, slice(None))),
                         start=(kc == 0), stop=(kc == KC - 1))
                P.copy("act", Vb.v((slice(None), tci, slice(None))), pb.v((slice(None), slice(0, 64))))
            kch = lambda ci, m: dict(k=[KTb.v((sl64, slice(ci * 128, (ci + 1) * 128)))],
                                     v=Vb.v((slice(None), ci, slice(None))), mask=m)
            for half in range(2):
                h0 = g * 4 + half * 2
                P.dma("pool", wq.ap[:, :, 0:128], win[:, :, h0 * 64:(h0 + 2) * 64], writes=[wq.v()])
                P.dma("pool", wq.ap[:, :, 128:256], wsw[:, :, h0 * 64:(h0 + 2) * 64], writes=[wq.v()])
                P.dma("pool", wo.ap, self.a_w_out[slot][h0 * 64:(h0 + 2) * 64, :].rearrange("(h d) n -> d h n", d=64),
                      writes=[wo.v()])
                for hh in range(2):
                    for tt in range(nttq):
                        t0, n = TT[tt]
                        self.rope_proj(QTb.v((sl64, hh, slice(t0, t0 + n)), subs=hh),
                                       lambda kc: wq.v((slice(None), kc, slice(hh * 64, (hh + 1) * 64))),
                                       lambda kc: wq.v((slice(None), kc, slice(128 + hh * 64, 128 + (hh + 1) * 64))),
                                       lambda kc: self.hTv(kc, tt), KC, n, t0, cosT, sinT, sl64, 64, rt)
                for hh in range(2):
                    h = h0 + hh
                    sv = sinkx.v((slice(None), slice(h, h + 1)))
                    for qb in range(16):
                        chunks = [kch(qb, None), kch(16, None), kch(17, None)]
                        if qb > 0:
                            chunks.append(kch(qb - 1, mprev.v()))
                        if qb < 15:
                            chunks.append(kch(qb + 1, mnext.v()))
                        qs = slice(qb * 128, (qb + 1) * 128)
                        self.attend([QTb.v((sl64, hh, qs), subs=hh)], chunks, 128, OTb.v((sl64, hh, qs), subs=hh), sv, scale)
                    if not last:
                        for qb in (16, 17):
                            qs = slice(qb * 128, (qb + 1) * 128)
                            self.attend([QTb.v((sl64, hh, qs), subs=hh)], [kch(16, None), kch(17, None)], 128,
                                        OTb.v((sl64, hh, qs), subs=hh), sv, scale)
                self.out_proj(l, wo, OTb, 2, nttq)

    def mixer_b(self, l):
        P = self.P
        self.new_phase()
        scale = 64 ** -0.5
        NEG = -30000.0
        self.attn_setup()
        QTb = self.alloc("QT", [64, 2, T], BF16, nsub=2)
        KTb = self.alloc("KT", [64, 2, T], BF16, nsub=2)
        Vb = self.alloc("V", [128, 18, 128], BF16)
        OTb = self.alloc("OT", [64, 2, T], BF16, nsub=2)
        wqkv = self.alloc("wqkv", [128, KC, 384], BF16)
        wo = self.alloc("wo", [64, 2, D], BF16)
        bm2 = [[[self.alloc(f"bm{s_}_{a_}_{b_}", [128, 128], F32, nsub=4) for b_ in range(5)] for a_ in range(5)] for s_ in range(2)]
        win = self.b_w_in.rearrange("(kc p) c -> p kc c", p=128)
        sl64 = slice(0, 64)
        reps = [0, 1, 2, 14, 15]
        for hp in range(8):
            h0 = hp * 2
            for j in range(3):
                P.dma("pool", wqkv.ap[:, :, j * 128:(j + 1) * 128], win[:, :, j * 1024 + h0 * 64:j * 1024 + (h0 + 2) * 64],
                      writes=[wqkv.v()])
            P.dma("pool", wo.ap, self.b_w_out[h0 * 64:(h0 + 2) * 64, :].rearrange("(h d) n -> d h n", d=64), writes=[wo.v()])
            n_ = 0
            for hh in range(2):
                for j, dstb in ((0, QTb), (1, KTb)):
                    for tt in range(5):
                        t0, n = TT[tt]
                        pb = self.ps[6 + n_ % 2]
                        n_ += 1
                        for kc in range(KC):
                            P.mm(pb.v((sl64, slice(0, n))), wqkv.v((slice(None), kc, slice(j * 128 + hh * 64, j * 128 + (hh + 1) * 64))),
                                 self.hTv(kc, tt), start=(kc == 0), stop=(kc == KC - 1))
                        P.copy("act", dstb.v((sl64, hh, slice(t0, t0 + n)), subs=hh), pb.v((sl64, slice(0, n))))
            for tci in range(18):
                tt = min(tci // 4, 4)
                pb = self.ps[6 + tci % 2]
                for kc in range(KC):
                    P.mm(pb.v((slice(None), slice(0, 128))), self.hTv(kc, tt, tci * 128, 128), wqkv.v((slice(None), kc, slice(256, 384))),
                         start=(kc == 0), stop=(kc == KC - 1))
                P.copy("act", Vb.v((slice(None), tci, slice(None))), pb.v((slice(None), slice(0, 128))))
            for hh in range(2):
                h = h0 + hh
                bm = bm2[h % 2]
                import os
                for cls, rp in enumerate(reps):
                    cs = min(max(2 * rp - 4, 0), 22)
                    if os.environ.get("B_MODE", "dma") in ("nolocal", "nomask", "nofill"):
                        break
                    for c in range(5):
                        for a in range(2):
                            for b in range(2):
                                kr = cs + 2 * c + a
                                qr = 2 * rp + b
                                r0 = min(max(qr - 4, 0), 24)
                                dst = bm[cls][c].v((slice(a * 64, (a + 1) * 64), slice(b * 64, (b + 1) * 64)), subs=a * 2 + b)
                                dr = (kr - qr + 7) if (r0 <= kr < r0 + 8) else 15
                                P.dma("sp", dst.ap, self.b_tm[h, dr], writes=[dst])
                kch = lambda ci, m: dict(k=[KTb.v((sl64, hh, slice(ci * 128, (ci + 1) * 128)), subs=hh)],
                                         v=Vb.v((slice(None), ci, slice(hh * 64, (hh + 1) * 64))), mask=m)
                for rp in range(16):
                    cls = {0: 0, 1: 1, 14: 3, 15: 4}.get(rp, 2)
                    cs = min(max(2 * rp - 4, 0), 22)
                    chunks = [kch(16, None), kch(17, None)]
                    import os
                    for c in range(5):
                        if os.environ.get("B_MODE", "dma") == "nolocal":
                            break
                        if os.environ.get("B_MODE", "dma") == "nomask":
                            chunks.append(kch(cs // 2 + c, None))
                            continue
                        chunks.append(kch(cs // 2 + c, bm[cls][c].v()))
                    qs = slice(rp * 128, (rp + 1) * 128)
                    self.attend([QTb.v((sl64, hh, qs), subs=hh)], chunks, 128, OTb.v((sl64, hh, qs), subs=hh), None, scale)
                for qb in (16, 17):
                    qs = slice(qb * 128, (qb + 1) * 128)
                    self.attend([QTb.v((sl64, hh, qs), subs=hh)], [kch(16, None), kch(17, None)], 128,
                                OTb.v((sl64, hh, qs), subs=hh), None, scale)
            self.out_proj(l, wo, OTb, 2, 5)

    def mixer_c(self, l):
        P = self.P
        self.new_phase()
        scale = 96 ** -0.5
        cosT = self.alloc("cosC", [96, T], F32)
        sinT = self.alloc("sinC", [96, T], F32)
        P.dma("sp", cosT.ap, self.ropeC[0], writes=[cosT.v()])
        P.dma("sp", sinT.ap, self.ropeC[1], writes=[sinT.v()])
        cqT = self.alloc("cqT", [128, 2, T], BF16)
        ckvT = self.alloc("ckvT", [128, T], BF16)
        krT = self.alloc("krT", [96, T], BF16)
        ncol = self.alloc("cnorm", [128, 3], F32)
        rt = [self.alloc(f"rt{i}", [96, 512], F32) for i in range(2)]
        mark = self.top
        w1 = self.alloc("w1", [128, KC, 416], BF16)
        w1s = self.alloc("w1s", [128, KC, 96], BF16)
        P.dma("pool", w1.ap, self.c_w_in.rearrange("(kc p) c -> p kc c", p=128), writes=[w1.v()])
        P.dma("pool", w1s.ap, self.c_w_in_sw.rearrange("(kc p) c -> p kc c", p=128), writes=[w1s.v()])
        nst = self.alloc("nst", [3, 128], F32)
        P.dma("sp", nst.ap, self.c_norms, writes=[nst.v()])
        pv = self.ps[5].v((slice(None), slice(0, 3)))
        P.transpose(pv, nst.v(), self.ident.v((slice(0, 3), slice(0, 3))))
        P.copy("dve", ncol.v(), pv)
        c32 = self.alloc("c32", [128, 3, 512], F32, nsub=3)
        sq = [self.alloc(f"csq{i}", [128, 512], F32) for i in range(2)]
        rs = [self.alloc(f"crs{i}", [128, 512], F32) for i in range(2)]
        r96 = slice(64, 96)
        for tt in range(5):
            t0, n = TT[tt]
            sn = slice(0, n)
            for s3 in range(3):
                pb = self.ps[6 + s3 % 2]
                for kc in range(KC):
                    P.mm(pb.v((slice(None), sn)), w1.v((slice(None), kc, slice(s3 * 128, (s3 + 1) * 128))), self.hTv(kc, tt),
                         start=(kc == 0), stop=(kc == KC - 1))
                P.copy("act", c32.v((slice(None), s3, sn), subs=s3), pb.v((slice(None), sn)))
            for which, slots, nf in ((0, (0, 1), 256.0), (1, (2,), 128.0)):
                pr = self.ps[0 + which]
                for i, s3 in enumerate(slots):
                    P.act(sq[i % 2].v((slice(None), sn)), c32.v((slice(None), s3, sn), subs=s3), AF.Square)
                    P.mm(pr.v((slice(None), sn)), self.ones32.v(), sq[i % 2].v((slice(None), sn)),
                         start=(i == 0), stop=(i == len(slots) - 1))
                r = rs[which]
                P.ts("dve", r.v((slice(None), sn)), pr.v((slice(None), sn)), 1.0 / nf, EPS, ALU.mult, ALU.add)
                P.act(r.v((slice(None), sn)), r.v((slice(None), sn)), AF.Sqrt)
                P.recip(r.v((slice(None), sn)), r.v((slice(None), sn)))
                for s3 in slots:
                    P.tt("dve", c32.v((slice(None), s3, sn), subs=s3), c32.v((slice(None), s3, sn), subs=s3),
                         r.v((slice(None), sn)), ALU.mult)
                    dst = cqT.v((slice(None), s3, slice(t0, t0 + n))) if which == 0 else ckvT.v((slice(None), slice(t0, t0 + n)))
                    P.act(dst, c32.v((slice(None), s3, sn), subs=s3), AF.Identity, scale=ncol.v((slice(None), slice(s3, s3 + 1))))
            self.rope_proj(krT.v((r96, slice(t0, t0 + n))),
                           lambda kc: w1.v((slice(None), kc, slice(320, 416))),
                           lambda kc: w1s.v((slice(None), kc, slice(0, 96))),
                           lambda kc: self.hTv(kc, tt), KC, n, t0, cosT, sinT, r96, 96, rt)
        self.release(mark)
        self.attn_setup()
        q96 = self.alloc("q96", [96, 2, T], BF16, nsub=2)
        k96 = self.alloc("k96", [96, 2, T], BF16, nsub=2)
        Vb = self.alloc("V", [128, 18, 128], BF16)
        OTb = self.alloc("OT", [64, 2, T], BF16, nsub=2)
        wuq = self.alloc("wuq", [128, 2, 192], BF16)
        wuqs = self.alloc("wuqs", [128, 2, 192], BF16)
        wukv = self.alloc("wukv", [128, 256], BF16)
        wo = self.alloc("wo", [64, 2, D], BF16)
        uq = self.c_w_uq.rearrange("(kc p) c -> p kc c", p=128)
        uqs = self.c_w_uq_sw.rearrange("(kc p) c -> p kc c", p=128)
        sl64 = slice(0, 64)
        for hp in range(8):
            h0 = hp * 2
            P.dma("pool", wuq.ap, uq[:, :, h0 * 96:(h0 + 2) * 96], writes=[wuq.v()])
            P.dma("pool", wuqs.ap, uqs[:, :, h0 * 96:(h0 + 2) * 96], writes=[wuqs.v()])
            P.dma("pool", wukv.ap, self.c_w_ukv[:, h0 * 128:(h0 + 2) * 128], writes=[wukv.v()])
            P.dma("pool", wo.ap, self.c_w_out[h0 * 64:(h0 + 2) * 64, :].rearrange("(h d) n -> d h n", d=64), writes=[wo.v()])
            for hh in range(2):
                for tt in range(5):
                    t0, n = TT[tt]
                    pa = self.rope_proj(q96.v((r96, hh, slice(t0, t0 + n)), subs=hh),
                                        lambda kc: wuq.v((slice(None), kc, slice(hh * 96, (hh + 1) * 96))),
                                        lambda kc: wuqs.v((slice(None), kc, slice(hh * 96, (hh + 1) * 96))),
                                        lambda kc: cqT.v((slice(None), kc, slice(t0, t0 + n))), 2, n, t0, cosT, sinT, r96, 96, rt)
                    P.copy("act", q96.v((sl64, hh, slice(t0, t0 + n)), subs=hh), pa.v((sl64, slice(0, n))))
                    pk = self.ps[6 + 0]
                    P.mm(pk.v((sl64, slice(0, n))), wukv.v((slice(None), slice(hh * 128, hh * 128 + 64))),
                         ckvT.v((slice(None), slice(t0, t0 + n))))
                    P.copy("act", k96.v((sl64, hh, slice(t0, t0 + n)), subs=hh), pk.v((sl64, slice(0, n))))
                P.copy("pool", k96.v((r96, hh, slice(None)), subs=hh), krT.v((r96, slice(None))))
            for tci in range(18):
                pb = self.ps[6 + tci % 2]
                for hh in range(2):
                    P.mm(pb.v((slice(None), slice(hh * 64, (hh + 1) * 64))), ckvT.v((slice(None), slice(tci * 128, (tci + 1) * 128))),
                         wukv.v((slice(None), slice(hh * 128 + 64, (hh + 1) * 128))))
                P.copy("act", Vb.v((slice(None), tci, slice(None))), pb.v((slice(None), slice(0, 128))))
            r0_96 = slice(0, 96)
            for hh in range(2):
                kch = lambda ci: dict(k=[k96.v((r0_96, hh, slice(ci * 128, (ci + 1) * 128)), subs=hh)],
                                      v=Vb.v((slice(None), ci, slice(hh * 64, (hh + 1) * 64))), mask=None)
                for qt in range(4):
                    t0, n = TT[qt]
                    self.attend([q96.v((r0_96, hh, slice(t0, t0 + n)), subs=hh)], [kch(ci) for ci in range(18)], 512,
                                OTb.v((sl64, hh, slice(t0, t0 + n)), subs=hh), None, scale)
                t0, n = TT[4]
                self.attend([q96.v((r0_96, hh, slice(t0, t0 + n)), subs=hh)], [kch(16), kch(17)], 256,
                            OTb.v((sl64, hh, slice(t0, t0 + n)), subs=hh), None, scale)
            self.out_proj(l, wo, OTb, 2, 5)

    def epilogue(self, do_norm=True):
        P = self.P
        self.new_phase()
        sq = [self.alloc(f"esq{i}", [128, 512], F32) for i in range(2)]
        rstd = self.alloc("erstd", [128, 512], F32)
        tmul = [self.alloc(f"etmul{i}", [128, 512], F32) for i in range(2)]
        hn = self.alloc("hn", [128, KC, 512], F32, nsub=KC)
        ost = [self.alloc(f"ost{i}", [128, D], F32) for i in range(2)]
        outbuf = Buf("outdram", self.out, 16)
        n = 0
        for tt in range(4):
            t0, _ = TT[tt]
            if do_norm:
                self.norm_tile(tt, gain_fn=lambda kc: self.normc.v((slice(None), kc, slice(8, 9))),
                               shift_fn=lambda kc: None,
                               out_fn=lambda kc: [hn.v((slice(None), kc, slice(None)), subs=kc)],
                               tmp=(sq, rstd, tmul), ps_i=0 + (tt % 2))
            else:
                for kc in range(KC):
                    P.copy("pool", hn.v((slice(None), kc, slice(None)), subs=kc),
                           self.xT.v((slice(None), kc, slice(t0, t0 + 512)), subs=self.xsub(kc, tt)))
            for sub in range(4):
                o = ost[n % 2]
                for half in range(2):
                    pb = self.ps[2 + (n * 2 + half) % 4]
                    for j in range(4):
                        kc = half * 4 + j
                        P.transpose(pb.v((slice(None), slice(j * 128, (j + 1) * 128)), subs=j),
                                    hn.v((slice(None), kc, slice(sub * 128, (sub + 1) * 128)), subs=kc), self.ident.v())
                    P.copy("dve" if half == 0 else "act", o.v((slice(None), slice(half * 512, (half + 1) * 512))), pb.v())
                tok = t0 + sub * 128
                P.dma("sp", self.out[tok:tok + 128, :], o.ap, reads=[o.v()], writes=[outbuf.v(subs=tok // 128)])
                n += 1
        P.op("sp", lambda e: e.nop(), reads=[outbuf.v()])

    def build(self, final_norm=True):
        self.consts()
        self.load_small()
        self.load_x()
        for l in self.layers:
            last = (l == DEPTH - 1)
            if self.do_mixer:
                self.norm1(l)
                kind = l % 3
                if kind == 0:
                    self.mixer_a(l, last)
                elif kind == 1:
                    self.mixer_b(l)
                else:
                    self.mixer_c(l)
            if self.do_moe:
                self.ffn_phase(l, last)
        self.epilogue(final_norm)
        return self.P.emit()


def rope_tables(rot_dim, nrows, row_off):
    f32 = np.float32
    t = np.arange(S)
    row = (t // 64).astype(f32)
    col = (t % 64).astype(f32)
    nf = rot_dim // 4
    inv = (f32(10000.0) ** (-np.arange(nf, dtype=f32) / f32(nf))).astype(f32)
    ang = np.concatenate([row[:, None] * inv, col[:, None] * inv], axis=-1).astype(f32)
    cos = np.cos(ang).astype(f32)
    sin = np.sin(ang).astype(f32)
    out = np.zeros((2, nrows, T), f32)
    out[0] = 1.0
    for d in range(rot_dim):
        i = d // 2
        out[0, row_off + d, :S] = cos[:, i]
        out[1, row_off + d, :S] = -sin[:, i] if d % 2 == 0 else sin[:, i]
    return out


def mixer_host_inputs(inp):
    f = lambda a: np.ascontiguousarray(np.asarray(a, dtype=np.float32))
    NEG = np.float32(-30000.0)
    a_w_in = f(inp["a_w_in"])
    perm = np.arange(1280) ^ 1
    a_sw = a_w_in[:, :, perm]
    j = np.arange(128)[:, None]
    i = np.arange(128)[None, :]
    maskA = np.stack([np.where(j >= i, 0.0, NEG), np.where(j <= i, 0.0, NEG)]).astype(np.float32)
    rpb = f(inp["b_rpb"])[0]
    kc = np.arange(64)[:, None]
    qc = np.arange(64)[None, :]
    ws = np.clip(qc - 8, 0, 48)
    ok = (kc >= ws) & (kc < ws + 16)
    idx = np.clip(kc - qc + 15, 0, 30)
    tm = np.full((16, 16, 64, 64), NEG, np.float32)
    tm[:, :15] = np.where(ok[None, None], rpb[:, :, idx], NEG)
    c_w_in = f(inp["c_w_in"])[0]
    c_sw = c_w_in[:, 320:416].copy()
    c_sw[:, 64:96] = c_w_in[:, 384:416][:, np.arange(32) ^ 1]
    uq = f(inp["c_w_uq"])[0]
    uqs = uq.copy()
    for h in range(16):
        uqs[:, h * 96 + 64:h * 96 + 96] = uq[:, h * 96 + 64:h * 96 + 96][:, np.arange(32) ^ 1]
    c_norms = np.stack([inp["c_q_norm"][0][:128], inp["c_q_norm"][0][128:], inp["c_kv_norm"][0]])
    return {
        "a_w_in": a_w_in, "a_w_in_sw": f(a_sw), "a_w_out": f(inp["a_w_out"]), "a_sink": f(inp["a_sink"]),
        "ropeA": rope_tables(64, 64, 0), "maskA": maskA,
        "b_w_in": f(inp["b_w_in"])[0], "b_w_out": f(inp["b_w_out"])[0], "b_tm": f(tm),
        "c_w_in": c_w_in, "c_w_in_sw": f(c_sw), "c_norms": f(c_norms),
        "c_w_uq": uq, "c_w_uq_sw": f(uqs), "c_w_ukv": f(inp["c_w_ukv"])[0], "c_w_out": f(inp["c_w_out"])[0],
        "ropeC": rope_tables(32, 96, 64),
    }


def make_in_maps(inp, ncores=8, mk=None):
    f = lambda a: np.ascontiguousarray(np.asarray(a, dtype=np.float32))
    ml = mk.moe_layers if (mk is not None and mk.moe_layers) else [0]
    nx = max(1, mk.n_experts) if (mk is not None and mk.do_moe) else 1
    if mk is None:
        ml, nx = list(range(DEPTH)), NE
    msl = lambda a: f(np.asarray(a)[ml][:, :nx])
    norms = f(np.concatenate([inp["norm_mix"], inp["norm_ffn"], inp["norm_out"][None, :]], axis=0))
    shared = {
        "cst_ident": np.eye(128, dtype=np.float32),
        "ada_w": f(inp["ada_w"]), "ada_b": f(inp["ada_b"]), "norms": norms,
        "router_w": f(inp["router_w"]), "router_b": f(inp["router_b"]),
        "moe_w_gate": msl(inp["moe_w_gate"]), "moe_w_up": msl(inp["moe_w_up"]), "moe_w_down": msl(inp["moe_w_down"]),
        "moe_b_gate": f(inp["moe_b_gate"]), "moe_b_up": f(inp["moe_b_up"]), "moe_b_down": f(inp["moe_b_down"]),
    }
    if mk is None or mk.do_mixer:
        shared.update(mixer_host_inputs(inp))
    maps = []
    for b in range(ncores):
        m = dict(shared)
        m["x"] = f(inp["x"][b])
        m["ctx"] = f(inp["ctx"][b])
        m["c2"] = f(np.stack([inp["c"][b], inp["c_ctx"]], axis=0))
        maps.append(m)
    return maps


def kernel(**inputs):
    mk = MK()
    nc = mk.build()
    in_maps = make_in_maps(inputs, 8, mk)
    res = run_bass_kernel_spmd(nc, in_maps, core_ids=list(range(8)))
    return np.stack([np.asarray(r["out"], dtype=np.float32) for r in res.results], axis=0)
```

```python
from contextlib import ExitStack
import numpy as np
import concourse.bass as bass
import concourse.mybir as mybir
from concourse.bass_utils import run_bass_kernel_spmd

F32 = mybir.dt.float32
BF16 = mybir.dt.bfloat16
AF = mybir.ActivationFunctionType
ALU = mybir.AluOpType
AX = mybir.AxisListType

ENG = ["pe", "act", "dve", "pool", "sp"]
N_DMA_SEMS = 12

D = 1024
KC = 8
S = 2048
C = 256
T = S + C
DEPTH = 4
NE = 32
EPS = 1e-6
TT = [(0, 512), (512, 512), (1024, 512), (1536, 512), (2048, 256)]
ARENA_WORDS = 53000


class V:
    __slots__ = ("ap", "keys")

    def __init__(self, ap, keys):
        self.ap = ap
        self.keys = keys


class Buf:
    def __init__(self, name, ap, nsub=1):
        self.name = name
        self.ap = ap
        self.nsub = nsub

    def v(self, idx=None, subs=None):
        ap = self.ap[idx] if idx is not None else self.ap
        if subs is None:
            keys = [(self.name, i) for i in range(self.nsub)]
        elif isinstance(subs, int):
            keys = [(self.name, subs)]
        else:
            keys = [(self.name, i) for i in subs]
        return V(ap, keys)


class Op:
    __slots__ = ("fn", "deps", "dma", "signal", "sem", "val", "sigcount")

    def __init__(self, fn, deps, dma):
        self.fn = fn
        self.deps = deps
        self.dma = dma
        self.signal = False
        self.sem = None
        self.val = 0
        self.sigcount = 0


class Prog:
    def __init__(self):
        self.nc = bass.Bass("TRN2", target_bir_lowering=False)
        self.ops = {e: [] for e in ENG}
        self.res_w = {}
        self.res_r = {}
        self.es = ExitStack()
        self.dma_since_barrier = []
        self.nbuf = 0

    def op(self, eng, fn, reads=(), writes=(), dma=False, extra_deps=()):
        deps = {}

        def add(tok):
            e, i = tok
            if self.ops[e][i].dma:
                deps[("dma", e, i)] = tok
            else:
                if e == "pe" and eng == "pe":
                    return
                k = ("eng", e)
                if k not in deps or deps[k][1] < i:
                    deps[k] = tok

        for v in reads:
            for k in v.keys:
                w = self.res_w.get(k)
                if w is not None:
                    add(w)
                if k[0].startswith("ps"):
                    for r in self.res_r.get(k, ()):
                        if r[0] != eng:
                            add(r)
        for v in writes:
            for k in v.keys:
                w = self.res_w.get(k)
                if w is not None:
                    add(w)
                for r in self.res_r.get(k, ()):
                    add(r)
        for tok in extra_deps:
            add(tok)
        idx = len(self.ops[eng])
        tok = (eng, idx)
        self.ops[eng].append(Op(fn, list(deps.values()), dma))
        if dma:
            self.dma_since_barrier.append(tok)
        for v in reads:
            for k in v.keys:
                self.res_r.setdefault(k, []).append(tok)
        for v in writes:
            for k in v.keys:
                self.res_w[k] = tok
                self.res_r[k] = []
        return tok

    def barrier(self):
        last = [(e, len(self.ops[e]) - 1) for e in ENG if self.ops[e]]
        deps = last + list(self.dma_since_barrier)
        self.dma_since_barrier = []
        for e in ENG:
            self.op(e, lambda eng: eng.nop(), extra_deps=deps)
        self.res_w = {}
        self.res_r = {}

    def mm(self, out, lhsT, rhs, start=True, stop=True):
        return self.op("pe", lambda e: e.matmul(out.ap, lhsT.ap, rhs.ap, start=start, stop=stop),
                       reads=[lhsT, rhs], writes=[out])

    def transpose(self, out, in_, ident):
        return self.op("pe", lambda e: e.transpose(out.ap, in_.ap, ident.ap),
                       reads=[in_, ident], writes=[out])

    def act(self, out, in_, func, bias=None, scale=1.0, accum=None):
        reads = [in_]
        kw = {}
        if isinstance(bias, V):
            reads.append(bias)
            kw["bias"] = bias.ap
        elif bias is not None:
            kw["bias"] = float(bias)
        if isinstance(scale, V):
            reads.append(scale)
            kw["scale"] = scale.ap
        else:
            kw["scale"] = float(scale)
        writes = [out]
        if accum is not None:
            writes.append(accum)
            kw["accum_out"] = accum.ap
        return self.op("act", lambda e: e.activation(out.ap, in_.ap, func, **kw), reads=reads, writes=writes)

    def ts(self, eng, out, in0, s1, s2, op0, op1=None, accum=None):
        reads = [in0]
        a1 = s1
        a2 = s2
        if isinstance(s1, V):
            reads.append(s1)
            a1 = s1.ap
        if isinstance(s2, V):
            reads.append(s2)
            a2 = s2.ap
        kw = {}
        if op1 is not None:
            kw["op1"] = op1
        writes = [out]
        if accum is not None:
            writes.append(accum)
            kw["accum_out"] = accum.ap
        return self.op(eng, lambda e: e.tensor_scalar(out.ap, in0.ap, a1, a2, op0, **kw), reads=reads, writes=writes)

    def tt(self, eng, out, in0, in1, op):
        return self.op(eng, lambda e: e.tensor_tensor(out.ap, in0.ap, in1.ap, op), reads=[in0, in1], writes=[out])

    def stt(self, eng, out, in0, scalar, in1, op0, op1):
        reads = [in0, in1]
        a = scalar
        if isinstance(scalar, V):
            reads.append(scalar)
            a = scalar.ap
        return self.op(eng, lambda e: e.scalar_tensor_tensor(out.ap, in0.ap, a, in1.ap, op0, op1),
                       reads=reads, writes=[out])

    def copy(self, eng, out, in_):
        if eng == "act":
            return self.op(eng, lambda e: e.copy(out.ap, in_.ap), reads=[in_], writes=[out])
        return self.op(eng, lambda e: e.tensor_copy(out.ap, in_.ap), reads=[in_], writes=[out])

    def recip(self, out, in_):
        return self.op("dve", lambda e: e.reciprocal(out.ap, in_.ap), reads=[in_], writes=[out])

    def memset(self, eng, out, val):
        return self.op(eng, lambda e: e.memset(out.ap, val), writes=[out])

    def dma(self, eng, out_ap, in_ap, reads=(), writes=(), **kw):
        return self.op(eng, lambda e: e.dma_start(out_ap, in_ap, **kw), reads=reads, writes=writes, dma=True)

    def emit(self):
        nc = self.nc
        es = self.es
        for e in ENG:
            for op in self.ops[e]:
                for (pe_, pi) in op.deps:
                    p = self.ops[pe_][pi]
                    if not p.dma:
                        p.signal = True
        EPOCH = 8000
        eng_sem = {}
        for e in ENG:
            c = 0
            for op in self.ops[e]:
                if op.dma:
                    continue
                if op.signal:
                    c += 1
                op.sigcount = c
            nep = max(1, (c + EPOCH - 1) // EPOCH)
            eng_sem[e] = [es.enter_context(nc.semaphore(f"s_{e}_{i}")) for i in range(nep)]

        def sig_of(e, sigcount):
            ep = (sigcount - 1) // EPOCH
            return eng_sem[e][ep], sigcount - ep * EPOCH

        for e in ENG:
            dl = [op for op in self.ops[e] if op.dma]
            if not dl:
                continue
            sems = [es.enter_context(nc.semaphore(f"d_{e}_{i}")) for i in range(N_DMA_SEMS)]
            uses = [0] * N_DMA_SEMS
            for j, op in enumerate(dl):
                s = j % N_DMA_SEMS
                uses[s] += 1
                op.sem = sems[s]
                op.val = 16 * uses[s]
        self.stats = {e: (len(self.ops[e]), max([o.sigcount for o in self.ops[e]] + [0])) for e in ENG}

        def run(e, eng):
            seen = {}

            def wait(sem, val):
                k = sem.num
                if seen.get(k, 0) < val:
                    eng.wait_ge(sem, val)
                    seen[k] = val

            for op in self.ops[e]:
                for (pe_, pi) in op.deps:
                    p = self.ops[pe_][pi]
                    if p.dma:
                        wait(p.sem, p.val)
                    else:
                        wait(*sig_of(pe_, p.sigcount))
                if op.dma:
                    if op.val > 16:
                        wait(op.sem, op.val - 16)
                    ins = op.fn(eng)
                    ins.then_inc(op.sem, 16)
                else:
                    ins = op.fn(eng)
                    if op.signal:
                        ins.then_inc(sig_of(e, op.sigcount)[0], 1)

        block = es.enter_context(nc.Block())

        @block.tensor
        def _(eng):
            run("pe", eng)

        @block.scalar
        def _(eng):
            run("act", eng)

        @block.vector
        def _(eng):
            run("dve", eng)

        @block.gpsimd
        def _(eng):
            run("pool", eng)

        @block.sync
        def _(eng):
            run("sp", eng)

        es.close()
        return nc


class MK:
    def __init__(self, layers=(0, 1, 2, 3), do_mixer=True, do_moe=True, n_experts=NE):
        self.layers = list(layers)
        self.do_mixer = do_mixer
        self.do_moe = do_moe
        self.n_experts = n_experts
        self.P = Prog()
        P = self.P
        nc = P.nc
        di = lambda n, s: nc.dram_tensor(n, list(s), F32, kind="ExternalInput").ap()
        self.x = di("x", [S, D])
        self.ctx = di("ctx", [C, D])
        self.c2 = di("c2", [2, D])
        self.ada_w = di("ada_w", [DEPTH, D, 6 * D])
        self.ada_b = di("ada_b", [DEPTH, 6 * D])
        self.norms = di("norms", [9, D])
        self.router_w = di("router_w", [DEPTH, D, NE])
        self.router_b = di("router_b", [DEPTH, NE])
        self.moe_layers = [l for l in self.layers] if do_moe else []
        self.lmap = {l: i for i, l in enumerate(self.moe_layers)}
        nl = max(1, len(self.moe_layers))
        nx = max(1, n_experts) if do_moe else 1
        self.w_gate = di("moe_w_gate", [nl, nx, D, D])
        self.w_up = di("moe_w_up", [nl, nx, D, D])
        self.w_down = di("moe_w_down", [nl, nx, D, D])
        self.b_gate = di("moe_b_gate", [DEPTH, NE, D])
        self.b_up = di("moe_b_up", [DEPTH, NE, D])
        self.b_down = di("moe_b_down", [DEPTH, NE, D])
        self.cst_ident = di("cst_ident", [128, 128])
        if do_mixer:
            self.a_w_in = di("a_w_in", [2, D, 1536])
            self.a_w_in_sw = di("a_w_in_sw", [2, D, 1280])
            self.a_w_out = di("a_w_out", [2, D, D])
            self.a_sink = di("a_sink", [2, 16])
            self.ropeA = di("ropeA", [2, 64, T])
            self.maskA = di("maskA", [2, 128, 128])
            self.b_w_in = di("b_w_in", [D, 3072])
            self.b_w_out = di("b_w_out", [D, D])
            self.b_tm = di("b_tm", [16, 16, 64, 64])
            self.c_w_in = di("c_w_in", [D, 416])
            self.c_w_in_sw = di("c_w_in_sw", [D, 96])
            self.c_norms = di("c_norms", [3, 128])
            self.c_w_uq = di("c_w_uq", [256, 1536])
            self.c_w_uq_sw = di("c_w_uq_sw", [256, 1536])
            self.c_w_ukv = di("c_w_ukv", [128, 2048])
            self.c_w_out = di("c_w_out", [D, D])
            self.ropeC = di("ropeC", [2, 96, T])
        self.out = nc.dram_tensor("out", [S, D], F32, kind="ExternalOutput").ap()
        self.gscr = nc.dram_tensor("gscr", [NE, T], F32, kind="Internal").ap()
        self.gscr_buf = Buf("gscr", self.gscr, 1)

        self.arena = P.es.enter_context(nc.sbuf_tensor("arena", [128, ARENA_WORDS], F32))
        self.ps = []
        for i in range(8):
            h = P.es.enter_context(nc.psum_tensor(f"ps{i}", [128, 512], F32))
            self.ps.append(Buf(f"ps{i}", h[:], 4))
        self.top = 0
        self.xT = self.alloc("xT", [128, KC, T], F32, nsub=KC * 5)
        self.hT = self.alloc("hT", [128, KC, T], BF16, nsub=KC * 5)
        self.ident = self.alloc("ident", [128, 128], F32)
        self.ones32 = self.alloc("ones32", [128, 128], F32)
        self.ones16 = self.alloc("ones16", [128, 64], BF16)
        self.modc = self.alloc("modc", [128, DEPTH, 6, KC, 2], F32, nsub=DEPTH)
        self.normc = self.alloc("normc", [128, KC, 9], F32)
        self.gm = self.alloc("gm", [128, 2, KC, 2], F32, nsub=2)
        self.phase_base = self.top

    def alloc(self, name, shape, dtype, nsub=1):
        n = int(np.prod(shape[1:]))
        words = n if dtype == F32 else (n + 1) // 2
        words = (words + 7) // 8 * 8
        lo = self.top
        self.top += words
        assert self.top <= ARENA_WORDS, (name, self.top)
        ap = self.arena[0:shape[0], lo:lo + words]
        if dtype != F32:
            ap = ap.bitcast(dtype)
        ap = ap[:, 0:n]
        if len(shape) == 3:
            ap = ap.rearrange("p (a b) -> p a b", a=shape[1])
        elif len(shape) == 4:
            ap = ap.rearrange("p (a b c) -> p a b c", a=shape[1], b=shape[2])
        elif len(shape) == 5:
            ap = ap.rearrange("p (a b c d) -> p a b c d", a=shape[1], b=shape[2], c=shape[3])
        self.P.nbuf += 1
        return Buf(f"{name}#{self.P.nbuf}", ap, nsub)

    def new_phase(self):
        self.P.barrier()
        self.top = self.phase_base

    def release(self, mark):
        self.P.barrier()
        self.top = mark

    def xsub(self, kc, tt):
        return kc * 5 + tt

    def psv(self, i, cols=slice(0, 512), parts=slice(0, 128), subs=None):
        return self.ps[i].v((parts, cols), subs=subs)

    def consts(self):
        P = self.P
        P.memset("dve", self.ones32.v(), 1.0)
        P.memset("dve", self.ones16.v(), 1.0)
        P.dma("sp", self.ident.ap, self.cst_ident, writes=[self.ident.v()])

    def rows_to_cols(self, dram_rows_ap, nrows, dst_fn, stage, ps_i):
        P = self.P
        P.dma("sp", stage.ap[0:nrows, :], dram_rows_ap, writes=[stage.v()])
        for kc in range(KC):
            pv = self.ps[ps_i].v((slice(0, 128), slice(kc * 32, kc * 32 + nrows)))
            P.transpose(pv, stage.v((slice(0, nrows), slice(kc * 128, (kc + 1) * 128))),
                        self.ident.v((slice(0, nrows), slice(0, nrows))))
            P.copy("dve", dst_fn(kc), pv)

    def load_x(self):
        P = self.P
        st = [self.alloc(f"xst{i}", [128, D], F32) for i in range(2)]
        n = 0
        for ti in range(T // 128):
            tok0 = ti * 128
            tt = min(tok0 // 512, 4)
            s = st[ti % 2]
            src = self.x[tok0:tok0 + 128, :] if tok0 < S else self.ctx[tok0 - S:tok0 - S + 128, :]
            P.dma("sp", s.ap, src, writes=[s.v()])
            for half in range(2):
                pb = self.ps[n % 4]
                n += 1
                for j in range(4):
                    kc = half * 4 + j
                    P.transpose(pb.v((slice(None), slice(j * 128, (j + 1) * 128)), subs=j),
                                s.v((slice(None), slice(kc * 128, (kc + 1) * 128))), self.ident.v())
                dst = self.xT.v((slice(None), slice(half * 4, half * 4 + 4), slice(tok0, tok0 + 128)),
                                subs=[self.xsub(half * 4 + j, tt) for j in range(4)])
                src_v = V(pb.ap.rearrange("p (a b) -> p a b", a=4), pb.v().keys)
                P.copy("dve" if half == 0 else "act", dst, src_v)

    def load_small(self):
        P = self.P
        stage = self.alloc("sm_stage", [128, D], F32)
        self.rows_to_cols(self.norms, 9, lambda kc: self.normc.v((slice(None), kc, slice(0, 9))), stage, 4)
        sc = self.alloc("siluc", [128, KC, 2], F32)
        self.rows_to_cols(self.c2, 2, lambda kc: sc.v((slice(None), kc, slice(0, 2))), stage, 4)
        sig = self.alloc("silu_sig", [128, KC, 2], F32)
        P.act(sig.v(), sc.v(), AF.Sigmoid)
        P.tt("dve", sc.v(), sc.v(), sig.v(), ALU.mult)
        adab = self.alloc("adab", [128, DEPTH, 48], F32)
        stage2 = self.alloc("sm_stage2", [48, 128], F32)
        for l in range(DEPTH):
            P.dma("sp", stage2.ap, self.ada_b[l].rearrange("(j p) -> j p", p=128), writes=[stage2.v()])
            pv = self.ps[5].v((slice(0, 128), slice(0, 48)))
            P.transpose(pv, stage2.v(), self.ident.v((slice(0, 48), slice(0, 48))))
            P.copy("dve", adab.v((slice(None), l, slice(None))), pv)
        wst = [self.alloc(f"adaw{i}", [128, KC, 512], F32) for i in range(2)]
        n = 0
        for l in range(DEPTH):
            for piece in range(12):
                w = wst[n % 2]
                n += 1
                src = self.ada_w[l].rearrange("(kc p) c -> p kc c", p=128)[:, :, piece * 512:(piece + 1) * 512]
                P.dma("sp", w.ap, src, writes=[w.v()])
                pb = self.ps[6 + (n % 2)]
                for q in range(4):
                    for kc in range(KC):
                        P.mm(pb.v((slice(None), slice(q * 2, q * 2 + 2))),
                             w.v((slice(None), kc, slice(q * 128, (q + 1) * 128))),
                             sc.v((slice(None), kc, slice(0, 2))), start=(kc == 0), stop=(kc == KC - 1))
                j, kc0 = divmod(piece * 4, 8)
                for q in range(4):
                    idx = piece * 4 + q
                    P.ts("dve", self.modc.v((slice(None), l, j, kc0 + q, slice(0, 2)), subs=l),
                         pb.v((slice(None), slice(q * 2, q * 2 + 2))),
                         adab.v((slice(None), l, slice(idx, idx + 1))), None, ALU.add)

    def prep_gm(self, l, which):
        P = self.P
        jsc = 1 if which == 0 else 4
        nidx = l if which == 0 else 4 + l
        for typ in range(2):
            P.ts("dve", self.gm.v((slice(None), which, slice(None), typ), subs=which),
                 self.modc.v((slice(None), l, jsc, slice(None), typ), subs=l), 1.0, None, ALU.add)
            P.tt("dve", self.gm.v((slice(None), which, slice(None), typ), subs=which),
                 self.gm.v((slice(None), which, slice(None), typ), subs=which),
                 self.normc.v((slice(None), slice(None), nidx)), ALU.mult)

    def norm_tile(self, tt, gain_fn, shift_fn, out_fn, tmp, ps_i, tmax=None):
        P = self.P
        t0, n = TT[tt]
        if tmax is not None:
            n = min(n, tmax)
        sq, rstd, tmul = tmp
        pb = self.ps[ps_i]
        for kc in range(KC):
            s = sq[kc % 2]
            P.act(s.v((slice(None), slice(0, n))), self.xT.v((slice(None), kc, slice(t0, t0 + n)), subs=self.xsub(kc, tt)),
                  AF.Square)
            P.mm(pb.v((slice(None), slice(0, n))), self.ones32.v(), s.v((slice(None), slice(0, n))),
                 start=(kc == 0), stop=(kc == KC - 1))
        P.ts("dve", rstd.v((slice(None), slice(0, n))), pb.v((slice(None), slice(0, n))), 1.0 / D, EPS, ALU.mult, ALU.add)
        P.act(rstd.v((slice(None), slice(0, n))), rstd.v((slice(None), slice(0, n))), AF.Sqrt)
        P.recip(rstd.v((slice(None), slice(0, n))), rstd.v((slice(None), slice(0, n))))
        for kc in range(KC):
            tm = tmul[kc % 2]
            P.tt("dve", tm.v((slice(None), slice(0, n))),
                 self.xT.v((slice(None), kc, slice(t0, t0 + n)), subs=self.xsub(kc, tt)),
                 rstd.v((slice(None), slice(0, n))), ALU.mult)
            sh = shift_fn(kc)
            outs = out_fn(kc)
            first = outs[0]
            P.act(first, tm.v((slice(None), slice(0, n))), AF.Identity, bias=sh if sh is not None else 0.0,
                  scale=gain_fn(kc))
            for o in outs[1:]:
                P.copy("pool", o, first)

    def ffn_phase(self, l, last):
        P = self.P
        ntt = 4 if last else 5
        Tl = S if last else T
        self.new_phase()
        self.prep_gm(l, 1)
        h32 = self.alloc("h32", [128, KC, 512], F32, nsub=KC)
        sq = [self.alloc(f"sq{i}", [128, 512], F32) for i in range(2)]
        rstd = self.alloc("rstd", [128, 512], F32)
        tmul = [self.alloc(f"tmul{i}", [128, 512], F32) for i in range(2)]
        rw = self.alloc("rw", [128, KC, NE], F32)
        rb = self.alloc("rb", [128, NE], F32)
        gatesT = self.alloc("gatesT", [NE, T], F32, nsub=5)
        lg = [self.alloc(f"lg{i}", [128, NE], F32) for i in range(2)]
        ex = [self.alloc(f"ex{i}", [128, NE], F32) for i in range(2)]
        mk = [self.alloc(f"mk{i}", [128, NE], F32) for i in range(2)]
        top8 = [self.alloc(f"top8{i}", [128, 8], F32) for i in range(2)]
        sm = [self.alloc(f"sm{i}", [128, 4], F32) for i in range(2)]
        P.dma("sp", rw.ap, self.router_w[l].rearrange("(kc p) e -> p kc e", p=128), writes=[rw.v()])
        P.dma("sp", rb.ap, self.router_b[l:l + 1, :].to_broadcast([128, NE]), writes=[rb.v()])
        it = 0
        for tt in range(ntt):
            t0, n = TT[tt]
            typ = 0 if tt < 4 else 1
            self.norm_tile(
                tt,
                gain_fn=lambda kc: self.gm.v((slice(None), 1, kc, slice(typ, typ + 1)), subs=1),
                shift_fn=lambda kc: self.modc.v((slice(None), l, 3, kc, slice(typ, typ + 1)), subs=l),
                out_fn=lambda kc: [h32.v((slice(None), kc, slice(0, n)), subs=kc),
                                   self.hT.v((slice(None), kc, slice(t0, t0 + n)), subs=self.xsub(kc, tt))],
                tmp=(sq, rstd, tmul), ps_i=0 + (tt % 2))
            for sub in range(n // 128):
                i2 = it % 2
                it += 1
                pl = self.ps[2 + i2]
                for kc in range(KC):
                    P.mm(pl.v((slice(None), slice(0, NE)), subs=0),
                         h32.v((slice(None), kc, slice(sub * 128, (sub + 1) * 128)), subs=kc),
                         rw.v((slice(None), kc, slice(None))), start=(kc == 0), stop=(kc == KC - 1))
                L = lg[i2]
                P.tt("dve", L.v(), pl.v((slice(None), slice(0, NE)), subs=0), rb.v(), ALU.add)
                P.op("dve", lambda e, o=top8[i2], i=L: e.max(o.ap, i.ap), reads=[L.v()], writes=[top8[i2].v()])
                P.ts("dve", mk[i2].v(), L.v(), top8[i2].v((slice(None), slice(3, 4))), None, ALU.is_ge)
                P.ts("dve", sm[i2].v((slice(None), slice(0, 1))), top8[i2].v((slice(None), slice(0, 1))), -1.0, None, ALU.mult)
                P.act(ex[i2].v(), L.v(), AF.Exp, bias=sm[i2].v((slice(None), slice(0, 1))))
                P.tt("dve", ex[i2].v(), ex[i2].v(), mk[i2].v(), ALU.mult)
                P.op("dve", lambda e, o=sm[i2], i=ex[i2]: e.reduce_sum(o.ap[:, 1:2], i.ap, axis=AX.X),
                     reads=[ex[i2].v()], writes=[sm[i2].v()])
                P.recip(sm[i2].v((slice(None), slice(2, 3))), sm[i2].v((slice(None), slice(1, 2))))
                P.ts("dve", ex[i2].v(), ex[i2].v(), sm[i2].v((slice(None), slice(2, 3))), None, ALU.mult)
                pg = self.ps[4 + i2]
                P.transpose(pg.v((slice(0, NE), slice(0, 128)), subs=0), ex[i2].v(), self.ident.v())
                tok = t0 + sub * 128
                P.copy("act", gatesT.v((slice(None), slice(tok, tok + 128)), subs=tt),
                       pg.v((slice(0, NE), slice(0, 128)), subs=0))
        P.dma("sp", self.gscr[:, 0:Tl], gatesT.ap[:, 0:Tl], reads=[gatesT.v()], writes=[self.gscr_buf.v()])

        self.new_phase()
        bgc = self.alloc("bgc", [128, KC, NE], F32)
        buc = self.alloc("buc", [128, KC, NE], F32)
        mark = self.top
        bstage = self.alloc("bstage", [NE, D], F32)
        bd = self.alloc("bd", [NE, D], F32)
        gT32 = self.alloc("gT32", [NE, T], F32)
        self.rows_to_cols(self.b_gate[l], NE, lambda kc: bgc.v((slice(None), kc, slice(None))), bstage, 7)
        self.rows_to_cols(self.b_up[l], NE, lambda kc: buc.v((slice(None), kc, slice(None))), bstage, 7)
        P.dma("sp", bd.ap, self.b_down[l], writes=[bd.v()])
        P.dma("sp", gT32.ap[:, 0:Tl], self.gscr[:, 0:Tl], reads=[self.gscr_buf.v()], writes=[gT32.v()])
        g2 = lambda kc, typ: self.modc.v((slice(None), l, 5, kc, slice(typ, typ + 1)), subs=l)
        for tt in range(ntt):
            t0, n = TT[tt]
            typ = 0 if tt < 4 else 1
            for dc in range(KC):
                pb = self.ps[dc % 2]
                P.mm(pb.v((slice(None), slice(0, n))), bd.v((slice(None), slice(dc * 128, (dc + 1) * 128))),
                     gT32.v((slice(None), slice(t0, t0 + n))))
                xv = self.xT.v((slice(None), dc, slice(t0, t0 + n)), subs=self.xsub(dc, tt))
                P.stt("dve", xv, pb.v((slice(None), slice(0, n))), g2(dc, typ), xv, ALU.mult, ALU.add)
        self.release(mark)
        NU = 2
        ALPHA = 1.702
        C7 = float(ALPHA * 7.0 / (1.0 + np.exp(-7.0 * ALPHA)))
        wg = [self.alloc(f"wg{i}", [128, KC, 512], BF16) for i in range(NU)]
        wu = [self.alloc(f"wu{i}", [128, KC, 512], BF16) for i in range(NU)]
        wd = [self.alloc(f"wd{i}", [128, 4, D], BF16) for i in range(NU)]
        G = [self.alloc(f"G{i}", [128, T], BF16) for i in range(2)]
        aT = [self.alloc(f"aT{i}", [128, 4, 512], BF16, nsub=4) for i in range(2)]
        silb = [self.alloc(f"silb{i}", [128, 512], F32) for i in range(2)]
        uvb = [self.alloc(f"uvb{i}", [128, 512], F32) for i in range(2)]
        g2a = self.alloc("g2a", [128, KC, 2], F32)
        P.ts("dve", bgc.v(), bgc.v(), ALPHA, None, ALU.mult)
        P.ts("dve", buc.v(), buc.v(), 1.0, None, ALU.add)
        P.ts("dve", g2a.v(), self.modc.v((slice(None), l, 5, slice(None), slice(None)), subs=l), 1.0 / ALPHA, None, ALU.mult)
        items = [(e, half, tt) for e in range(self.n_experts) for half in range(2) for tt in range(ntt)]
        st_ = {"tn": 0}

        def gu(i):
            e, half, tt = items[i]
            u = (e * 2 + half) % NU
            Ge = G[e % 2]
            if tt == 0:
                if half == 0:
                    P.dma("pool", Ge.ap[:, 0:Tl], self.gscr[e:e + 1, 0:Tl].to_broadcast([128, Tl]),
                          reads=[self.gscr_buf.v()], writes=[Ge.v()])
                fsl = slice(half * 512, (half + 1) * 512)
                P.dma("pool", wg[u].ap, self.w_gate[self.lmap[l], e].rearrange("(kc p) f -> p kc f", p=128)[:, :, fsl],
                      writes=[wg[u].v()])
                P.dma("pool", wu[u].ap, self.w_up[self.lmap[l], e].rearrange("(kc p) f -> p kc f", p=128)[:, :, fsl],
                      writes=[wu[u].v()])
                P.dma("pool", wd[u].ap, self.w_down[self.lmap[l], e].rearrange("(fc p) d -> p fc d", p=128)[:, half * 4:half * 4 + 4, :],
                      writes=[wd[u].v()])
            t0, n = TT[tt]
            A = aT[i % 2]
            sn = slice(0, n)
            for fc in range(4):
                fcg = half * 4 + fc
                tn = st_["tn"]
                st_["tn"] += 1
                pg_ = self.ps[2 + (tn % 2)]
                pu_ = self.ps[4 + (tn % 2)]
                sil, uvt = silb[tn % 2], uvb[tn % 2]
                for kc in range(KC):
                    P.mm(pg_.v((slice(None), sn)), wg[u].v((slice(None), kc, slice(fc * 128, (fc + 1) * 128))),
                         self.hT.v((slice(None), kc, slice(t0, t0 + n)), subs=self.xsub(kc, tt)),
                         start=(kc == 0), stop=(kc == KC - 1))
                for kc in range(KC):
                    P.mm(pu_.v((slice(None), sn)), wu[u].v((slice(None), kc, slice(fc * 128, (fc + 1) * 128))),
                         self.hT.v((slice(None), kc, slice(t0, t0 + n)), subs=self.xsub(kc, tt)),
                         start=(kc == 0), stop=(kc == KC - 1))
                sv = sil.v((slice(None), sn))
                uv = uvt.v((slice(None), sn))
                P.act(sv, pg_.v((slice(None), sn)), AF.Silu, bias=bgc.v((slice(None), fcg, slice(e, e + 1))), scale=ALPHA)
                P.ts("dve", uv, pu_.v((slice(None), sn)), buc.v((slice(None), fcg, slice(e, e + 1))), -6.0, ALU.add, ALU.max)
                P.stt("dve", uv, uv, 8.0, Ge.v((slice(None), slice(t0, t0 + n))), ALU.min, ALU.mult)
                P.stt("dve", A.v((slice(None), fc, sn), subs=fc), sv, C7, uv, ALU.min, ALU.mult)

        def down(i):
            e, half, tt = items[i]
            u = (e * 2 + half) % NU
            t0, n = TT[tt]
            typ = 0 if tt < 4 else 1
            A = aT[i % 2]
            sn = slice(0, n)
            for dc in range(KC):
                py = self.ps[(0, 1, 6, 7)[dc % 4]]
                for fc in range(4):
                    P.mm(py.v((slice(None), sn)), wd[u].v((slice(None), fc, slice(dc * 128, (dc + 1) * 128))),
                         A.v((slice(None), fc, sn), subs=fc), start=(fc == 0), stop=(fc == 3))
                xv = self.xT.v((slice(None), dc, slice(t0, t0 + n)), subs=self.xsub(dc, tt))
                P.stt("dve", xv, py.v((slice(None), sn)), g2a.v((slice(None), dc, slice(typ, typ + 1))), xv, ALU.mult, ALU.add)

        if items:
            gu(0)
        for i in range(len(items)):
            if i + 1 < len(items):
                gu(i + 1)
            down(i)

    def norm1(self, l):
        P = self.P
        self.new_phase()
        self.prep_gm(l, 0)
        sq = [self.alloc(f"n1sq{i}", [128, 512], F32) for i in range(2)]
        rstd = self.alloc("n1rstd", [128, 512], F32)
        tmul = [self.alloc(f"n1tmul{i}", [128, 512], F32) for i in range(2)]
        for tt in range(5):
            t0, n = TT[tt]
            typ = 0 if tt < 4 else 1
            self.norm_tile(
                tt,
                gain_fn=lambda kc: self.gm.v((slice(None), 0, kc, slice(typ, typ + 1)), subs=0),
                shift_fn=lambda kc: self.modc.v((slice(None), l, 0, kc, slice(typ, typ + 1)), subs=l),
                out_fn=lambda kc: [self.hT.v((slice(None), kc, slice(t0, t0 + n)), subs=self.xsub(kc, tt))],
                tmp=(sq, rstd, tmul), ps_i=0 + (tt % 2))

    def hTv(self, kc, tt, lo=None, n=None):
        t0, nn = TT[tt]
        if lo is None:
            lo, n = t0, nn
        return self.hT.v((slice(None), kc, slice(lo, lo + n)), subs=self.xsub(kc, tt))

    def attn_setup(self):
        self.Pt = [self.alloc(f"Pt{i}", [128, 512], BF16) for i in range(4)]
        self.St = [self.alloc(f"St{i}", [128, 512], F32) for i in range(4)]
        self.rd = [self.alloc(f"rd{i}", [64, 512], F32) for i in range(2)]
        self.sbanks = [0, 1, 6, 7]
        self.pending = []
        self.a_n = 0
        self.s_n = 0

    def attn_flush(self):
        while self.pending:
            self.pending.pop(0)()

    def attend(self, qparts, chunks, QW, out_v, sink_v, scale, SKEW=3):
        P = self.P
        G = 512 // QW
        ob = self.ps[2 + self.a_n % 2]
        db = self.ps[4 + self.a_n % 2]
        rd = self.rd[self.a_n % 2]
        self.a_n += 1
        ng = (len(chunks) + G - 1) // G
        sl64 = slice(0, 64)
        for gi in range(ng):
            cs = chunks[gi * G:(gi + 1) * G]
            sb = self.ps[self.sbanks[self.s_n % 4]]
            pt = self.Pt[self.s_n % 4]
            st = self.St[self.s_n % 4]
            self.s_n += 1
            for j, ch in enumerate(cs):
                cols = slice(j * QW, (j + 1) * QW)
                for pi, (kv, qv) in enumerate(zip(ch["k"], qparts)):
                    P.mm(sb.v((slice(None), cols)), kv, qv, start=(pi == 0), stop=(pi == len(qparts) - 1))
            j = 0
            while j < len(cs):
                if cs[j]["mask"] is None:
                    j2 = j
                    while j2 < len(cs) and cs[j2]["mask"] is None:
                        j2 += 1
                    cols = slice(j * QW, j2 * QW)
                    P.act(pt.v((slice(None), cols)), sb.v((slice(None), cols)), AF.Exp, scale=scale)
                    j = j2
                else:
                    cols = slice(j * QW, (j + 1) * QW)
                    P.stt("dve", st.v((slice(None), cols)), sb.v((slice(None), cols)), scale, cs[j]["mask"],
                          ALU.mult, ALU.add)
                    P.act(pt.v((slice(None), cols)), st.v((slice(None), cols)), AF.Exp)
                    j += 1

            def pv(gi=gi, cs=cs, pt=pt):
                for j, ch in enumerate(cs):
                    cols = slice(j * QW, (j + 1) * QW)
                    first = (gi == 0 and j == 0)
                    lastc = (gi == ng - 1 and j == len(cs) - 1)
                    P.mm(ob.v((sl64, slice(0, QW))), ch["v"], pt.v((slice(None), cols)), start=first, stop=lastc)
                    P.mm(db.v((sl64, slice(0, QW))), self.ones16.v(), pt.v((slice(None), cols)), start=first, stop=lastc)
                if gi == ng - 1:
                    rv = rd.v((sl64, slice(0, QW)))
                    if sink_v is not None:
                        P.ts("dve", rv, db.v((sl64, slice(0, QW))), sink_v, None, ALU.add)
                        P.recip(rv, rv)
                    else:
                        P.recip(rv, db.v((sl64, slice(0, QW))))
                    P.tt("dve", out_v, ob.v((sl64, slice(0, QW))), rv, ALU.mult)

            self.pending.append(pv)
            while len(self.pending) > SKEW:
                self.pending.pop(0)()

    def out_proj(self, l, wo, OTb, nh, ntt):
        P = self.P
        self.attn_flush()
        for tt in range(ntt):
            t0, n = TT[tt]
            typ = 0 if tt < 4 else 1
            for dc in range(KC):
                pb = self.ps[6 + dc % 2]
                for hh in range(nh):
                    P.mm(pb.v((slice(None), slice(0, n))), wo.v((slice(None), hh, slice(dc * 128, (dc + 1) * 128))),
                         OTb.v((slice(None), hh, slice(t0, t0 + n)), subs=hh), start=(hh == 0), stop=(hh == nh - 1))
                xv = self.xT.v((slice(None), dc, slice(t0, t0 + n)), subs=self.xsub(dc, tt))
                P.stt("dve", xv, pb.v((slice(None), slice(0, n))),
                      self.modc.v((slice(None), l, 2, dc, slice(typ, typ + 1)), subs=l), xv, ALU.mult, ALU.add)

    def rope_proj(self, dst_v, wA_fn, wB_fn, rhs_fn, nkc, n, t0, cosT, sinT, prow, M, rt):
        P = self.P
        pa, pb = self.ps[6], self.ps[7]
        for kc in range(nkc):
            P.mm(pa.v((slice(0, M), slice(0, n))), wA_fn(kc), rhs_fn(kc), start=(kc == 0), stop=(kc == nkc - 1))
        for kc in range(nkc):
            P.mm(pb.v((slice(0, M), slice(0, n))), wB_fn(kc), rhs_fn(kc), start=(kc == 0), stop=(kc == nkc - 1))
        t1, t2 = rt
        P.tt("dve", t1.v((prow, slice(0, n))), pa.v((prow, slice(0, n))), cosT.v((prow, slice(t0, t0 + n))), ALU.mult)
        P.tt("dve", t2.v((prow, slice(0, n))), pb.v((prow, slice(0, n))), sinT.v((prow, slice(t0, t0 + n))), ALU.mult)
        P.tt("pool", dst_v, t1.v((prow, slice(0, n))), t2.v((prow, slice(0, n))), ALU.add)
        return pa

    def mixer_a(self, l, last):
        P = self.P
        slot = l // 3
        self.new_phase()
        nttq = 4 if last else 5
        scale = 64 ** -0.5
        cosT = self.alloc("cosA", [64, T], F32)
        sinT = self.alloc("sinA", [64, T], F32)
        P.dma("sp", cosT.ap, self.ropeA[0], writes=[cosT.v()])
        P.dma("sp", sinT.ap, self.ropeA[1], writes=[sinT.v()])
        mprev = self.alloc("mprev", [128, 128], F32)
        mnext = self.alloc("mnext", [128, 128], F32)
        P.dma("sp", mprev.ap, self.maskA[0], writes=[mprev.v()])
        P.dma("sp", mnext.ap, self.maskA[1], writes=[mnext.v()])
        sinkx = self.alloc("sinkx", [64, 16], F32)
        P.dma("sp", sinkx.ap, self.a_sink[slot:slot + 1, :].to_broadcast([64, 16]), writes=[sinkx.v()])
        P.act(sinkx.v(), sinkx.v(), AF.Exp)
        self.attn_setup()
        KTb = self.alloc("KT", [64, T], BF16)
        Vb = self.alloc("V", [128, 18, 64], BF16)
        QTb = self.alloc("QT", [64, 2, T], BF16, nsub=2)
        OTb = self.alloc("OT", [64, 2, T], BF16, nsub=2)
        wk = self.alloc("wk", [128, KC, 128], BF16)
        wv = self.alloc("wv", [128, KC, 64], BF16)
        wq = self.alloc("wq", [128, KC, 256], BF16)
        wo = self.alloc("wo", [64, 2, D], BF16)
        rt = [self.alloc(f"rt{i}", [64, 512], F32) for i in range(2)]
        win = self.a_w_in[slot].rearrange("(kc p) c -> p kc c", p=128)
        wsw = self.a_w_in_sw[slot].rearrange("(kc p) c -> p kc c", p=128)
        sl64 = slice(0, 64)
        for g in range(4):
            kc0 = 1024 + g * 64
            P.dma("pool", wk.ap[:, :, 0:64], win[:, :, kc0:kc0 + 64], writes=[wk.v()])
            P.dma("pool", wk.ap[:, :, 64:128], wsw[:, :, kc0:kc0 + 64], writes=[wk.v()])
            P.dma("pool", wv.ap, win[:, :, 1280 + g * 64:1280 + (g + 1) * 64], writes=[wv.v()])
            for tt in range(5):
                t0, n = TT[tt]
                self.rope_proj(KTb.v((sl64, slice(t0, t0 + n))),
                               lambda kc: wk.v((slice(None), kc, slice(0, 64))),
                               lambda kc: wk.v((slice(None), kc, slice(64, 128))),
                               lambda kc: self.hTv(kc, tt), KC, n, t0, cosT, sinT, sl64, 64, rt)
            for tci in range(18):
                tt = min(tci // 4, 4)
                pb = self.ps[6 + tci % 2]
                for kc in range(KC):
                    P.mm(pb.v((slice(None), slice(0, 64))), self.hTv(kc, tt, tci * 128, 128), wv.v((slice(None), kc, slice(None))),
                         start=(kc == 0), stop=(kc == KC - 1))
                P.copy("act", Vb.v((slice(None), tci, slice(None))), pb.v((slice(None), slice(0, 64))))
            kch = lambda ci, m: dict(k=[KTb.v((sl64, slice(ci * 128, (ci + 1) * 128)))],
                                     v=Vb.v((slice(None), ci, slice(None))), mask=m)
            for half in range(2):
                h0 = g * 4 + half * 2
                P.dma("pool", wq.ap[:, :, 0:128], win[:, :, h0 * 64:(h0 + 2) * 64], writes=[wq.v()])
                P.dma("pool", wq.ap[:, :, 128:256], wsw[:, :, h0 * 64:(h0 + 2) * 64], writes=[wq.v()])
                P.dma("pool", wo.ap, self.a_w_out[slot][h0 * 64:(h0 + 2) * 64, :].rearrange("(h d) n -> d h n", d=64),
                      writes=[wo.v()])
                for hh in range(2):
                    for tt in range(nttq):
                        t0, n = TT[tt]
                        self.rope_proj(QTb.v((sl64, hh, slice(t0, t0 + n)), subs=hh),
                                       lambda kc: wq.v((slice(None), kc, slice(hh * 64, (hh + 1) * 64))),
                                       lambda kc: wq.v((slice(None), kc, slice(128 + hh * 64, 128 + (hh + 1) * 64))),
                                       lambda kc: self.hTv(kc, tt), KC, n, t0, cosT, sinT, sl64, 64, rt)
                for hh in range(2):
                    h = h0 + hh
                    sv = sinkx.v((slice(None), slice(h, h + 1)))
                    for qb in range(16):
                        chunks = [kch(qb, None), kch(16, None), kch(17, None)]
                        if qb > 0:
                            chunks.append(kch(qb - 1, mprev.v()))
                        if qb < 15:
                            chunks.append(kch(qb + 1, mnext.v()))
                        qs = slice(qb * 128, (qb + 1) * 128)
                        self.attend([QTb.v((sl64, hh, qs), subs=hh)], chunks, 128, OTb.v((sl64, hh, qs), subs=hh), sv, scale)
                    if not last:
                        for qb in (16, 17):
                            qs = slice(qb * 128, (qb + 1) * 128)
                            self.attend([QTb.v((sl64, hh, qs), subs=hh)], [kch(16, None), kch(17, None)], 128,
                                        OTb.v((sl64, hh, qs), subs=hh), sv, scale)
                self.out_proj(l, wo, OTb, 2, nttq)

    def mixer_b(self, l):
        P = self.P
        self.new_phase()
        scale = 64 ** -0.5
        NEG = -30000.0
        self.attn_setup()
        QTb = self.alloc("QT", [64, 2, T], BF16, nsub=2)
        KTb = self.alloc("KT", [64, 2, T], BF16, nsub=2)
        Vb = self.alloc("V", [128, 18, 128], BF16)
        OTb = self.alloc("OT", [64, 2, T], BF16, nsub=2)
        wqkv = self.alloc("wqkv", [128, KC, 384], BF16)
        wo = self.alloc("wo", [64, 2, D], BF16)
        bm2 = [[[self.alloc(f"bm{s_}_{a_}_{b_}", [128, 128], F32, nsub=4) for b_ in range(5)] for a_ in range(5)] for s_ in range(2)]
        win = self.b_w_in.rearrange("(kc p) c -> p kc c", p=128)
        sl64 = slice(0, 64)
        reps = [0, 1, 2, 14, 15]
        for hp in range(8):
            h0 = hp * 2
            for j in range(3):
                P.dma("pool", wqkv.ap[:, :, j * 128:(j + 1) * 128], win[:, :, j * 1024 + h0 * 64:j * 1024 + (h0 + 2) * 64],
                      writes=[wqkv.v()])
            P.dma("pool", wo.ap, self.b_w_out[h0 * 64:(h0 + 2) * 64, :].rearrange("(h d) n -> d h n", d=64), writes=[wo.v()])
            n_ = 0
            for hh in range(2):
                for j, dstb in ((0, QTb), (1, KTb)):
                    for tt in range(5):
                        t0, n = TT[tt]
                        pb = self.ps[6 + n_ % 2]
                        n_ += 1
                        for kc in range(KC):
                            P.mm(pb.v((sl64, slice(0, n))), wqkv.v((slice(None), kc, slice(j * 128 + hh * 64, j * 128 + (hh + 1) * 64))),
                                 self.hTv(kc, tt), start=(kc == 0), stop=(kc == KC - 1))
                        P.copy("act", dstb.v((sl64, hh, slice(t0, t0 + n)), subs=hh), pb.v((sl64, slice(0, n))))
            for tci in range(18):
                tt = min(tci // 4, 4)
                pb = self.ps[6 + tci % 2]
                for kc in range(KC):
                    P.mm(pb.v((slice(None), slice(0, 128))), self.hTv(kc, tt, tci * 128, 128), wqkv.v((slice(None), kc, slice(256, 384))),
                         start=(kc == 0), stop=(kc == KC - 1))
                P.copy("act", Vb.v((slice(None), tci, slice(None))), pb.v((slice(None), slice(0, 128))))
            for hh in range(2):
                h = h0 + hh
                bm = bm2[h % 2]
                import os
                for cls, rp in enumerate(reps):
                    cs = min(max(2 * rp - 4, 0), 22)
                    if os.environ.get("B_MODE", "dma") in ("nolocal", "nomask", "nofill"):
                        break
                    for c in range(5):
                        for a in range(2):
                            for b in range(2):
                                kr = cs + 2 * c + a
                                qr = 2 * rp + b
                                r0 = min(max(qr - 4, 0), 24)
                                dst = bm[cls][c].v((slice(a * 64, (a + 1) * 64), slice(b * 64, (b + 1) * 64)), subs=a * 2 + b)
                                dr = (kr - qr + 7) if (r0 <= kr < r0 + 8) else 15
                                P.dma("sp", dst.ap, self.b_tm[h, dr], writes=[dst])
                kch = lambda ci, m: dict(k=[KTb.v((sl64, hh, slice(ci * 128, (ci + 1) * 128)), subs=hh)],
                                         v=Vb.v((slice(None), ci, slice(hh * 64, (hh + 1) * 64))), mask=m)
                for rp in range(16):
                    cls = {0: 0, 1: 1, 14: 3, 15: 4}.get(rp, 2)
                    cs = min(max(2 * rp - 4, 0), 22)
                    chunks = [kch(16, None), kch(17, None)]
                    import os
                    for c in range(5):
                        if os.environ.get("B_MODE", "dma") == "nolocal":
                            break
                        if os.environ.get("B_MODE", "dma") == "nomask":
                            chunks.append(kch(cs // 2 + c, None))
                            continue
                        chunks.append(kch(cs // 2 + c, bm[cls][c].v()))
                    qs = slice(rp * 128, (rp + 1) * 128)
                    self.attend([QTb.v((sl64, hh, qs), subs=hh)], chunks, 128, OTb.v((sl64, hh, qs), subs=hh), None, scale)
                for qb in (16, 17):
                    qs = slice(qb * 128, (qb + 1) * 128)
                    self.attend([QTb.v((sl64, hh, qs), subs=hh)], [kch(16, None), kch(17, None)], 128,
                                OTb.v((sl64, hh, qs), subs=hh), None, scale)
            self.out_proj(l, wo, OTb, 2, 5)

    def mixer_c(self, l):
        P = self.P
        self.new_phase()
        scale = 96 ** -0.5
        cosT = self.alloc("cosC", [96, T], F32)
        sinT = self.alloc("sinC", [96, T], F32)
        P.dma("sp", cosT.ap, self.ropeC[0], writes=[cosT.v()])
        P.dma("sp", sinT.ap, self.ropeC[1], writes=[sinT.v()])
        cqT = self.alloc("cqT", [128, 2, T], BF16)
        ckvT = self.alloc("ckvT", [128, T], BF16)
        krT = self.alloc("krT", [96, T], BF16)
        ncol = self.alloc("cnorm", [128, 3], F32)
        rt = [self.alloc(f"rt{i}", [96, 512], F32) for i in range(2)]
        mark = self.top
        w1 = self.alloc("w1", [128, KC, 416], BF16)
        w1s = self.alloc("w1s", [128, KC, 96], BF16)
        P.dma("pool", w1.ap, self.c_w_in.rearrange("(kc p) c -> p kc c", p=128), writes=[w1.v()])
        P.dma("pool", w1s.ap, self.c_w_in_sw.rearrange("(kc p) c -> p kc c", p=128), writes=[w1s.v()])
        nst = self.alloc("nst", [3, 128], F32)
        P.dma("sp", nst.ap, self.c_norms, writes=[nst.v()])
        pv = self.ps[5].v((slice(None), slice(0, 3)))
        P.transpose(pv, nst.v(), self.ident.v((slice(0, 3), slice(0, 3))))
        P.copy("dve", ncol.v(), pv)
        c32 = self.alloc("c32", [128, 3, 512], F32, nsub=3)
        sq = [self.alloc(f"csq{i}", [128, 512], F32) for i in range(2)]
        rs = [self.alloc(f"crs{i}", [128, 512], F32) for i in range(2)]
        r96 = slice(64, 96)
        for tt in range(5):
            t0, n = TT[tt]
            sn = slice(0, n)
            for s3 in range(3):
                pb = self.ps[6 + s3 % 2]
                for kc in range(KC):
                    P.mm(pb.v((slice(None), sn)), w1.v((slice(None), kc, slice(s3 * 128, (s3 + 1) * 128))), self.hTv(kc, tt),
                         start=(kc == 0), stop=(kc == KC - 1))
                P.copy("act", c32.v((slice(None), s3, sn), subs=s3), pb.v((slice(None), sn)))
            for which, slots, nf in ((0, (0, 1), 256.0), (1, (2,), 128.0)):
                pr = self.ps[0 + which]
                for i, s3 in enumerate(slots):
                    P.act(sq[i % 2].v((slice(None), sn)), c32.v((slice(None), s3, sn), subs=s3), AF.Square)
                    P.mm(pr.v((slice(None), sn)), self.ones32.v(), sq[i % 2].v((slice(None), sn)),
                         start=(i == 0), stop=(i == len(slots) - 1))
                r = rs[which]
                P.ts("dve", r.v((slice(None), sn)), pr.v((slice(None), sn)), 1.0 / nf, EPS, ALU.mult, ALU.add)
                P.act(r.v((slice(None), sn)), r.v((slice(None), sn)), AF.Sqrt)
                P.recip(r.v((slice(None), sn)), r.v((slice(None), sn)))
                for s3 in slots:
                    P.tt("dve", c32.v((slice(None), s3, sn), subs=s3), c32.v((slice(None), s3, sn), subs=s3),
                         r.v((slice(None), sn)), ALU.mult)
                    dst = cqT.v((slice(None), s3, slice(t0, t0 + n))) if which == 0 else ckvT.v((slice(None), slice(t0, t0 + n)))
                    P.act(dst, c32.v((slice(None), s3, sn), subs=s3), AF.Identity, scale=ncol.v((slice(None), slice(s3, s3 + 1))))
            self.rope_proj(krT.v((r96, slice(t0, t0 + n))),
                           lambda kc: w1.v((slice(None), kc, slice(320, 416))),
                           lambda kc: w1s.v((slice(None), kc, slice(0, 96))),
                           lambda kc: self.hTv(kc, tt), KC, n, t0, cosT, sinT, r96, 96, rt)
        self.release(mark)
        self.attn_setup()
        q96 = self.alloc("q96", [96, 2, T], BF16, nsub=2)
        k96 = self.alloc("k96", [96, 2, T], BF16, nsub=2)
        Vb = self.alloc("V", [128, 18, 128], BF16)
        OTb = self.alloc("OT", [64, 2, T], BF16, nsub=2)
        wuq = self.alloc("wuq", [128, 2, 192], BF16)
        wuqs = self.alloc("wuqs", [128, 2, 192], BF16)
        wukv = self.alloc("wukv", [128, 256], BF16)
        wo = self.alloc("wo", [64, 2, D], BF16)
        uq = self.c_w_uq.rearrange("(kc p) c -> p kc c", p=128)
        uqs = self.c_w_uq_sw.rearrange("(kc p) c -> p kc c", p=128)
        sl64 = slice(0, 64)
        for hp in range(8):
            h0 = hp * 2
            P.dma("pool", wuq.ap, uq[:, :, h0 * 96:(h0 + 2) * 96], writes=[wuq.v()])
            P.dma("pool", wuqs.ap, uqs[:, :, h0 * 96:(h0 + 2) * 96], writes=[wuqs.v()])
            P.dma("pool", wukv.ap, self.c_w_ukv[:, h0 * 128:(h0 + 2) * 128], writes=[wukv.v()])
            P.dma("pool", wo.ap, self.c_w_out[h0 * 64:(h0 + 2) * 64, :].rearrange("(h d) n -> d h n", d=64), writes=[wo.v()])
            for hh in range(2):
                for tt in range(5):
                    t0, n = TT[tt]
                    pa = self.rope_proj(q96.v((r96, hh, slice(t0, t0 + n)), subs=hh),
                                        lambda kc: wuq.v((slice(None), kc, slice(hh * 96, (hh + 1) * 96))),
                                        lambda kc: wuqs.v((slice(None), kc, slice(hh * 96, (hh + 1) * 96))),
                                        lambda kc: cqT.v((slice(None), kc, slice(t0, t0 + n))), 2, n, t0, cosT, sinT, r96, 96, rt)
                    P.copy("act", q96.v((sl64, hh, slice(t0, t0 + n)), subs=hh), pa.v((sl64, slice(0, n))))
                    pk = self.ps[6 + 0]
                    P.mm(pk.v((sl64, slice(0, n))), wukv.v((slice(None), slice(hh * 128, hh * 128 + 64))),
                         ckvT.v((slice(None), slice(t0, t0 + n))))
                    P.copy("act", k96.v((sl64, hh, slice(t0, t0 + n)), subs=hh), pk.v((sl64, slice(0, n))))
                P.copy("pool", k96.v((r96, hh, slice(None)), subs=hh), krT.v((r96, slice(None))))
            for tci in range(18):
                pb = self.ps[6 + tci % 2]
                for hh in range(2):
                    P.mm(pb.v((slice(None), slice(hh * 64, (hh + 1) * 64))), ckvT.v((slice(None), slice(tci * 128, (tci + 1) * 128))),
                         wukv.v((slice(None), slice(hh * 128 + 64, (hh + 1) * 128))))
                P.copy("act", Vb.v((slice(None), tci, slice(None))), pb.v((slice(None), slice(0, 128))))
            r0_96 = slice(0, 96)
            for hh in range(2):
                kch = lambda ci: dict(k=[k96.v((r0_96, hh, slice(ci * 128, (ci + 1) * 128)), subs=hh)],
                                      v=Vb.v((slice(None), ci, slice(hh * 64, (hh + 1) * 64))), mask=None)
                for qt in range(4):
                    t0, n = TT[qt]
                    self.attend([q96.v((r0_96, hh, slice(t0, t0 + n)), subs=hh)], [kch(ci) for ci in range(18)], 512,
                                OTb.v((sl64, hh, slice(t0, t0 + n)), subs=hh), None, scale)
                t0, n = TT[4]
                self.attend([q96.v((r0_96, hh, slice(t0, t0 + n)), subs=hh)], [kch(16), kch(17)], 256,
                            OTb.v((sl64, hh, slice(t0, t0 + n)), subs=hh), None, scale)
            self.out_proj(l, wo, OTb, 2, 5)

    def epilogue(self, do_norm=True):
        P = self.P
        self.new_phase()
        sq = [self.alloc(f"esq{i}", [128, 512], F32) for i in range(2)]
        rstd = self.alloc("erstd", [128, 512], F32)
        tmul = [self.alloc(f"etmul{i}", [128, 512], F32) for i in range(2)]
        hn = self.alloc("hn", [128, KC, 512], F32, nsub=KC)
        ost = [self.alloc(f"ost{i}", [128, D], F32) for i in range(2)]
        outbuf = Buf("outdram", self.out, 16)
        n = 0
        for tt in range(4):
            t0, _ = TT[tt]
            if do_norm:
                self.norm_tile(tt, gain_fn=lambda kc: self.normc.v((slice(None), kc, slice(8, 9))),
                               shift_fn=lambda kc: None,
                               out_fn=lambda kc: [hn.v((slice(None), kc, slice(None)), subs=kc)],
                               tmp=(sq, rstd, tmul), ps_i=0 + (tt % 2))
            else:
                for kc in range(KC):
                    P.copy("pool", hn.v((slice(None), kc, slice(None)), subs=kc),
                           self.xT.v((slice(None), kc, slice(t0, t0 + 512)), subs=self.xsub(kc, tt)))
            for sub in range(4):
                o = ost[n % 2]
                for half in range(2):
                    pb = self.ps[2 + (n * 2 + half) % 4]
                    for j in range(4):
                        kc = half * 4 + j
                        P.transpose(pb.v((slice(None), slice(j * 128, (j + 1) * 128)), subs=j),
                                    hn.v((slice(None), kc, slice(sub * 128, (sub + 1) * 128)), subs=kc), self.ident.v())
                    P.copy("dve" if half == 0 else "act", o.v((slice(None), slice(half * 512, (half + 1) * 512))), pb.v())
                tok = t0 + sub * 128
                P.dma("sp", self.out[tok:tok + 128, :], o.ap, reads=[o.v()], writes=[outbuf.v(subs=tok // 128)])
                n += 1
        P.op("sp", lambda e: e.nop(), reads=[outbuf.v()])

    def build(self, final_norm=True):
        self.consts()
        self.load_small()
        self.load_x()
        for l in self.layers:
            last = (l == DEPTH - 1)
            if self.do_mixer:
                self.norm1(l)
                kind = l % 3
                if kind == 0:
                    self.mixer_a(l, last)
                elif kind == 1:
                    self.mixer_b(l)
                else:
                    self.mixer_c(l)
            if self.do_moe:
                self.ffn_phase(l, last)
        self.epilogue(final_norm)
        return self.P.emit()


def rope_tables(rot_dim, nrows, row_off):
    f32 = np.float32
    t = np.arange(S)
    row = (t // 64).astype(f32)
    col = (t % 64).astype(f32)
    nf = rot_dim // 4
    inv = (f32(10000.0) ** (-np.arange(nf, dtype=f32) / f32(nf))).astype(f32)
    ang = np.concatenate([row[:, None] * inv, col[:, None] * inv], axis=-1).astype(f32)
    cos = np.cos(ang).astype(f32)
    sin = np.sin(ang).astype(f32)
    out = np.zeros((2, nrows, T), f32)
    out[0] = 1.0
    for d in range(rot_dim):
        i = d // 2
        out[0, row_off + d, :S] = cos[:, i]
        out[1, row_off + d, :S] = -sin[:, i] if d % 2 == 0 else sin[:, i]
    return out


def mixer_host_inputs(inp):
    f = lambda a: np.ascontiguousarray(np.asarray(a, dtype=np.float32))
    NEG = np.float32(-30000.0)
    a_w_in = f(inp["a_w_in"])
    perm = np.arange(1280) ^ 1
    a_sw = a_w_in[:, :, perm]
    j = np.arange(128)[:, None]
    i = np.arange(128)[None, :]
    maskA = np.stack([np.where(j >= i, 0.0, NEG), np.where(j <= i, 0.0, NEG)]).astype(np.float32)
    rpb = f(inp["b_rpb"])[0]
    kc = np.arange(64)[:, None]
    qc = np.arange(64)[None, :]
    ws = np.clip(qc - 8, 0, 48)
    ok = (kc >= ws) & (kc < ws + 16)
    idx = np.clip(kc - qc + 15, 0, 30)
    tm = np.full((16, 16, 64, 64), NEG, np.float32)
    tm[:, :15] = np.where(ok[None, None], rpb[:, :, idx], NEG)
    c_w_in = f(inp["c_w_in"])[0]
    c_sw = c_w_in[:, 320:416].copy()
    c_sw[:, 64:96] = c_w_in[:, 384:416][:, np.arange(32) ^ 1]
    uq = f(inp["c_w_uq"])[0]
    uqs = uq.copy()
    for h in range(16):
        uqs[:, h * 96 + 64:h * 96 + 96] = uq[:, h * 96 + 64:h * 96 + 96][:, np.arange(32) ^ 1]
    c_norms = np.stack([inp["c_q_norm"][0][:128], inp["c_q_norm"][0][128:], inp["c_kv_norm"][0]])
    return {
        "a_w_in": a_w_in, "a_w_in_sw": f(a_sw), "a_w_out": f(inp["a_w_out"]), "a_sink": f(inp["a_sink"]),
        "ropeA": rope_tables(64, 64, 0), "maskA": maskA,
        "b_w_in": f(inp["b_w_in"])[0], "b_w_out": f(inp["b_w_out"])[0], "b_tm": f(tm),
        "c_w_in": c_w_in, "c_w_in_sw": f(c_sw), "c_norms": f(c_norms),
        "c_w_uq": uq, "c_w_uq_sw": f(uqs), "c_w_ukv": f(inp["c_w_ukv"])[0], "c_w_out": f(inp["c_w_out"])[0],
        "ropeC": rope_tables(32, 96, 64),
    }


def make_in_maps(inp, ncores=8, mk=None):
    f = lambda a: np.ascontiguousarray(np.asarray(a, dtype=np.float32))
    ml = mk.moe_layers if (mk is not None and mk.moe_layers) else [0]
    nx = max(1, mk.n_experts) if (mk is not None and mk.do_moe) else 1
    if mk is None:
        ml, nx = list(range(DEPTH)), NE
    msl = lambda a: f(np.asarray(a)[ml][:, :nx])
    norms = f(np.concatenate([inp["norm_mix"], inp["norm_ffn"], inp["norm_out"][None, :]], axis=0))
    shared = {
        "cst_ident": np.eye(128, dtype=np.float32),
        "ada_w": f(inp["ada_w"]), "ada_b": f(inp["ada_b"]), "norms": norms,
        "router_w": f(inp["router_w"]), "router_b": f(inp["router_b"]),
        "moe_w_gate": msl(inp["moe_w_gate"]), "moe_w_up": msl(inp["moe_w_up"]), "moe_w_down": msl(inp["moe_w_down"]),
        "moe_b_gate": f(inp["moe_b_gate"]), "moe_b_up": f(inp["moe_b_up"]), "moe_b_down": f(inp["moe_b_down"]),
    }
    if mk is None or mk.do_mixer:
        shared.update(mixer_host_inputs(inp))
    maps = []
    for b in range(ncores):
        m = dict(shared)
        m["x"] = f(inp["x"][b])
        m["ctx"] = f(inp["ctx"][b])
        m["c2"] = f(np.stack([inp["c"][b], inp["c_ctx"]], axis=0))
        maps.append(m)
    return maps


def kernel(**inputs):
    mk = MK()
    nc = mk.build()
    in_maps = make_in_maps(inputs, 8, mk)
    res = run_bass_kernel_spmd(nc, in_maps, core_ids=list(range(8)))
    return np.stack([np.asarray(r["out"], dtype=np.float32) for r in res.results], axis=0)
```
